# Optimizing a Trainium2 kernel written in Bass

```python
import math
import jax, jax.numpy as jnp
from jax import lax
import numpy as np

D_MODEL = 2048
BATCH = 4
SEQ = 4096
DEPTH = 1

ATT_HEADS = 16
ATT_KV_HEADS = 2
ATT_HEAD_DIM = 64
WINDOW = 128
ATT_BLOCK = WINDOW
ATT_Q_WIDTH = ATT_HEADS * ATT_HEAD_DIM
ATT_KV_WIDTH = ATT_KV_HEADS * ATT_HEAD_DIM
GLA_HEADS = 4
GLA_DK = D_MODEL // 2
GLA_DV = D_MODEL
GLA_HEAD_K = GLA_DK // GLA_HEADS
GLA_HEAD_V = GLA_DV // GLA_HEADS
GLA_GATE_RANK = 16
GLA_GATE_TEMP = 16.0
GLA_CHUNK = 64
N_EXPERTS = 32
TOP_K = 4
D_FF = D_MODEL
SWIGLU_LIMIT = 7.0
SWIGLU_ALPHA = 1.702
MOE_BLOCK = 256
LN_EPS = 1e-5
DN_ALPHA = (2 * DEPTH) ** 0.25
DN_BETA = (8 * DEPTH) ** -0.25
IN_SPLITS = (ATT_Q_WIDTH, ATT_KV_WIDTH, ATT_KV_WIDTH,
             GLA_DK, GLA_DK, GLA_DV, GLA_DV, GLA_GATE_RANK,
             D_MODEL, D_MODEL)
IN_IS_VALUE = (False, False, True, False, False, True, False, False, False, False)
IN_COLS = sum(IN_SPLITS)

kernel_name = "hybrid_swa_gla_moe_deepnorm_adaln"


def _layernorm(x):
    xf = x.astype(jnp.float32)
    mu = jnp.mean(xf, -1, keepdims=True)
    var = jnp.mean(jnp.square(xf - mu), -1, keepdims=True)
    return ((xf - mu) * lax.rsqrt(var + LN_EPS)).astype(x.dtype)


def _layernorm_affine(x, gain, bias):
    xf = x.astype(jnp.float32)
    mu = jnp.mean(xf, -1, keepdims=True)
    var = jnp.mean(jnp.square(xf - mu), -1, keepdims=True)
    y = (xf - mu) * lax.rsqrt(var + LN_EPS) * gain.astype(jnp.float32) + bias.astype(jnp.float32)
    return y.astype(x.dtype)


def _sliding_window_attention(q, k, v, sinks):
    B, T, Hq, Dh = q.shape
    Hkv = k.shape[2]
    G = Hq // Hkv
    nb = T // ATT_BLOCK
    qb = q.reshape(B, nb, ATT_BLOCK, Hkv, G, Dh)

    def band(t):
        tb = t.reshape(B, nb, ATT_BLOCK, Hkv, Dh)
        prev = jnp.pad(tb[:, :-1], ((0, 0), (1, 0), (0, 0), (0, 0), (0, 0)))
        return jnp.concatenate([prev, tb], axis=2)

    kb, vb = band(k), band(v)
    s = jnp.einsum('bnqkgd,bnskd->bnkgqs', qb, kb,
                   preferred_element_type=jnp.float32) * (Dh ** -0.5)
    qi = jnp.arange(ATT_BLOCK)[:, None]
    si = jnp.arange(2 * ATT_BLOCK)[None, :]
    dist = qi + ATT_BLOCK - si
    blk = jnp.arange(nb)[:, None, None]
    valid = (dist >= 0) & (dist < WINDOW)
    valid = valid[None] & ((blk > 0) | (si[None] >= ATT_BLOCK))
    slopes = (2.0 ** (-8.0 * (jnp.arange(Hq, dtype=jnp.float32) + 1.0) / Hq)).reshape(Hkv, G)
    s = s - slopes[None, None, :, :, None, None] * dist.astype(jnp.float32)
    s = jnp.where(valid[None, :, None, None], s, -jnp.inf)
    sink = sinks.astype(jnp.float32).reshape(Hkv, G)[None, None, :, :, None, None]
    m = jnp.maximum(jnp.max(s, -1, keepdims=True), sink)
    p = jnp.exp(s - m)
    p = p / (jnp.sum(p, -1, keepdims=True) + jnp.exp(sink - m))
    o = jnp.einsum('bnkgqs,bnskd->bnqkgd', p.astype(v.dtype), vb)
    return o.reshape(B, T, Hq * Dh)


def _gla(q, k, v, log_a):
    B, T, H, dk = q.shape
    dv = v.shape[-1]
    C = GLA_CHUNK
    N = T // C

    def chunks(t):
        return t.astype(jnp.float32).reshape(B, N, C, H, t.shape[-1]).transpose(1, 0, 3, 2, 4)

    q = chunks(q) * (dk ** -0.5)
    k, v, g = chunks(k), chunks(v), chunks(log_a)
    b = jnp.cumsum(g, axis=3)
    b_last = b[:, :, :, -1:, :]
    q_e = q * jnp.exp(b)
    k_e = k * jnp.exp(-b)
    k_end = k * jnp.exp(b_last - b)
    causal = jnp.tril(jnp.ones((C, C), dtype=bool))
    A = jnp.where(causal, jnp.einsum('nbhid,nbhjd->nbhij', q_e, k_e), 0.0)
    o_intra = jnp.einsum('nbhij,nbhjv->nbhiv', A, v)

    def step(S, inp):
        qe, ke, vc, dl = inp
        o = jnp.einsum('bhid,bhdv->bhiv', qe, S)
        S = jnp.exp(dl)[..., None] * S + jnp.einsum('bhjd,bhjv->bhdv', ke, vc)
        return S, o

    S0 = jnp.zeros((B, H, dk, dv), jnp.float32)
    _, o_inter = lax.scan(step, S0, (q_e, k_end, v, b_last[:, :, :, 0, :]))
    o = o_intra + o_inter
    return o.transpose(1, 0, 3, 2, 4).reshape(B, T, H, dv)


def _head_rmsnorm(o, gain):
    return o * lax.rsqrt(jnp.mean(jnp.square(o), -1, keepdims=True) + LN_EPS) * gain.astype(jnp.float32)


def _moe(h, w_router, b_router, w_gate_up, b_gate_up, w_down, b_down):
    B, T, D = h.shape
    xt = h.reshape(-1, D)
    n_tok = xt.shape[0]
    logits = (xt @ w_router).astype(jnp.float32) + b_router.astype(jnp.float32)
    top_val, top_idx = lax.top_k(logits, TOP_K)
    top_w = jax.nn.softmax(top_val, axis=-1)
    n_rows = n_tok * TOP_K
    e_flat = top_idx.reshape(-1)
    w_flat = top_w.reshape(-1)
    tok_flat = jnp.arange(n_rows) // TOP_K
    order = jnp.argsort(e_flat)
    e_s, tok_s, w_s = e_flat[order], tok_flat[order], w_flat[order]
    counts = jnp.bincount(e_flat, length=N_EXPERTS)
    starts = jnp.cumsum(counts) - counts
    padded = (counts + MOE_BLOCK - 1) // MOE_BLOCK * MOE_BLOCK
    pad_end = jnp.cumsum(padded)
    pad_start = pad_end - padded
    dest = pad_start[e_s] + jnp.arange(n_rows) - starts[e_s]
    cap = -(-n_rows // MOE_BLOCK) * MOE_BLOCK + N_EXPERTS * MOE_BLOCK
    n_blocks = cap // MOE_BLOCK
    xs = jnp.zeros((cap, D), h.dtype).at[dest].set(xt[tok_s])
    blk_e = jnp.minimum(jnp.searchsorted(pad_end, jnp.arange(n_blocks) * MOE_BLOCK, side='right'),
                        N_EXPERTS - 1)

    def expert_block(args):
        xb, e = args
        gu = xb @ w_gate_up[e] + b_gate_up[e]
        gate = jnp.minimum(gu[:, 0::2], SWIGLU_LIMIT)
        up = jnp.clip(gu[:, 1::2], -SWIGLU_LIMIT, SWIGLU_LIMIT)
        act = (up + 1.0) * gate * jax.nn.sigmoid(SWIGLU_ALPHA * gate)
        return act @ w_down[e] + b_down[e]

    ys = lax.map(expert_block, (xs.reshape(n_blocks, MOE_BLOCK, D), blk_e))
    y_rows = ys.reshape(cap, D)[dest].astype(jnp.float32) * w_s[:, None]
    out = jax.ops.segment_sum(y_rows, tok_s, num_segments=n_tok)
    return out.astype(h.dtype).reshape(B, T, D)


def setup_inputs(seed: int = 0) -> dict:
    key = jax.random.key(seed)
    ks = iter(jax.random.split(key, 48))

    def nrm(shape, scale):
        return jax.random.normal(next(ks), shape, jnp.float32) * scale

    L, D, E, F = DEPTH, D_MODEL, N_EXPERTS, D_FF
    x = nrm((BATCH, SEQ, D), 1.0)
    c = nrm((BATCH, D), 1.0)
    w_ada = nrm((L, D, 6 * D), 0.5 * D ** -0.5)
    b_ada = nrm((L, 6 * D), 0.02)
    w_in = jnp.concatenate(
        [nrm((L, D, w), D ** -0.5 * (DN_BETA if is_v else 1.0))
         for w, is_v in zip(IN_SPLITS, IN_IS_VALUE)], axis=-1)
    w_gla_gate_up = nrm((L, GLA_GATE_RANK, GLA_DK), GLA_GATE_RANK ** -0.5)
    b_gla_gate = nrm((L, GLA_DK), 0.1)
    attn_sinks = nrm((L, ATT_HEADS), 1.0)
    gla_norm_gain = 1.0 + nrm((L, GLA_DV), 0.02)
    w_branch_att = nrm((L, ATT_Q_WIDTH, D), ATT_Q_WIDTH ** -0.5)
    w_branch_gla = nrm((L, GLA_DV, D), GLA_DV ** -0.5)
    w_out = nrm((L, D, D), D ** -0.5 * DN_BETA)
    ln1_gain = 1.0 + nrm((L, D), 0.02)
    ln1_bias = nrm((L, D), 0.02)
    w_router = nrm((L, D, E), D ** -0.5)
    b_router = nrm((L, E), 0.01)
    w_gate_up = nrm((L, E, D, 2 * F), D ** -0.5)
    b_gate_up = nrm((L, E, 2 * F), 0.02)
    w_down = nrm((L, E, F, D), F ** -0.5 * DN_BETA)
    b_down = nrm((L, E, D), 0.02)
    ln2_gain = 1.0 + nrm((L, D), 0.02)
    ln2_bias = nrm((L, D), 0.02)
    return {"x": x, "c": c, "w_ada": w_ada, "b_ada": b_ada, "w_in": w_in,
            "w_gla_gate_up": w_gla_gate_up, "b_gla_gate": b_gla_gate,
            "attn_sinks": attn_sinks, "gla_norm_gain": gla_norm_gain,
            "w_branch_att": w_branch_att, "w_branch_gla": w_branch_gla, "w_out": w_out,
            "ln1_gain": ln1_gain, "ln1_bias": ln1_bias,
            "w_router": w_router, "b_router": b_router,
            "w_gate_up": w_gate_up, "b_gate_up": b_gate_up,
            "w_down": w_down, "b_down": b_down,
            "ln2_gain": ln2_gain, "ln2_bias": ln2_bias}


def reference(x, c, w_ada, b_ada, w_in, w_gla_gate_up, b_gla_gate, attn_sinks, gla_norm_gain,
              w_branch_att, w_branch_gla, w_out, ln1_gain, ln1_bias, w_router, b_router,
              w_gate_up, b_gate_up, w_down, b_down, ln2_gain, ln2_bias):
    B, T, D = x.shape
    offs = np.cumsum(IN_SPLITS)[:-1].tolist()
    for l in range(DEPTH):
        ada = (jax.nn.silu(c) @ w_ada[l] + b_ada[l])[:, None, :]
        sh1, sc1, gt1, sh2, sc2, gt2 = jnp.split(ada, 6, axis=-1)

        h = _layernorm(x) * (1.0 + sc1) + sh1
        proj = h @ w_in[l]
        q_a, k_a, v_a, q_g, k_g, v_g, r_g, a_lr, g_a, g_g = jnp.split(proj, offs, axis=-1)
        y_att = _sliding_window_attention(
            q_a.reshape(B, T, ATT_HEADS, ATT_HEAD_DIM),
            k_a.reshape(B, T, ATT_KV_HEADS, ATT_HEAD_DIM),
            v_a.reshape(B, T, ATT_KV_HEADS, ATT_HEAD_DIM),
            attn_sinks[l])
        log_a = jax.nn.log_sigmoid((a_lr @ w_gla_gate_up[l] + b_gla_gate[l]).astype(jnp.float32)) / GLA_GATE_TEMP
        o_g = _gla(q_g.reshape(B, T, GLA_HEADS, GLA_HEAD_K),
                   k_g.reshape(B, T, GLA_HEADS, GLA_HEAD_K),
                   v_g.reshape(B, T, GLA_HEADS, GLA_HEAD_V),
                   log_a.reshape(B, T, GLA_HEADS, GLA_HEAD_K))
        o_g = _head_rmsnorm(o_g, gla_norm_gain[l].reshape(GLA_HEADS, GLA_HEAD_V))
        y_gla = o_g.reshape(B, T, GLA_DV).astype(x.dtype) * jax.nn.silu(r_g)
        merged = (jax.nn.sigmoid(g_a) * (y_att @ w_branch_att[l])
                  + jax.nn.sigmoid(g_g) * (y_gla @ w_branch_gla[l]))
        mix = merged @ w_out[l]
        x = _layernorm_affine(DN_ALPHA * x + gt1 * mix, ln1_gain[l], ln1_bias[l])

        h = _layernorm(x) * (1.0 + sc2) + sh2
        ffn = _moe(h, w_router[l], b_router[l], w_gate_up[l], b_gate_up[l], w_down[l], b_down[l])
        x = _layernorm_affine(DN_ALPHA * x + gt2 * ffn, ln2_gain[l], ln2_bias[l])
    return x
```

```python
import numpy as np
import concourse.bass as bass
import concourse.mybir as mybir
from concourse.bass_utils import run_bass_kernel_spmd

F32 = mybir.dt.float32
BF16 = mybir.dt.bfloat16
AF = mybir.ActivationFunctionType
ALU = mybir.AluOpType
AX = mybir.AxisListType
ISZ = {F32: 4, BF16: 2}

UNIT = 256
SB_BASE = 16512
SB_END = 229376
EPOCH = 30000
ENGS = ['pe', 'act', 'dve', 'pool', 'sp']


class Tile:
    def __init__(self, h, space, addr, shape, dtype):
        self.h, self.space, self.addr, self.shape, self.dtype = h, space, addr, shape, dtype
        self.isz = ISZ[dtype]
        n = 1
        for s in shape[1:]:
            n *= s
        self.nbytes = n * self.isz

    def __getitem__(self, k):
        return self.h[k]

    def rng(self, lo=None, hi=None):
        lo = 0 if lo is None else lo * self.isz
        hi = self.nbytes if hi is None else hi * self.isz
        return (self.space, self.addr + lo, self.addr + hi)


def _rng(x):
    return x.rng() if isinstance(x, Tile) else x


class Sched:
    def __init__(self, nc, n_dma_slots=24):
        self.nc = nc
        self.eng = {'pe': nc.tensor, 'act': nc.scalar, 'dve': nc.vector, 'pool': nc.gpsimd, 'sp': nc.sync}
        self.q = {e: [] for e in ENGS}
        self.sem = {}
        self.epoch = {e: 0 for e in ENGS}
        self.cnt = {e: 0 for e in ENGS}
        for e in ENGS:
            self.sem[(e, 0)] = nc.alloc_semaphore(f"tl_{e}_0")
        self.known = {e: {} for e in ENGS}
        self.mem_w = {'sb': {}, 'ps': {}}
        self.mem_r = {'sb': {}, 'ps': {}}
        self.sb_off = SB_BASE
        self.ps_n = 0
        self.nslots = n_dma_slots
        self.dslot = {}
        self.dnext = {e: 0 for e in ENGS}
        self.ninstr = 0

    def sb(self, name, shape, dtype, off=None):
        t = Tile(None, 'sb', 0, shape, dtype)
        if off is None:
            off = self.sb_off
            self.sb_off = off + ((t.nbytes + 63) // 64) * 64
        t.addr = off
        assert off >= SB_BASE and off + t.nbytes <= SB_END, (name, off, t.nbytes)
        t.h = self.nc.alloc_sbuf_tensor_at(name, list(shape), dtype, offset=off)
        return t

    def ps(self, name, shape=(128, 512), dtype=F32):
        t = Tile(None, 'ps', self.ps_n * 2048, shape, dtype)
        assert t.nbytes <= 2048
        self.ps_n += 1
        t.h = self.nc.alloc_psum_tensor(name, list(shape), dtype)
        return t

    def _units(self, r):
        sp, lo, hi = r
        if sp == 'ps':
            return sp, range((lo // 2048) * (2048 // UNIT), ((hi - 1) // 2048 + 1) * (2048 // UNIT))
        return sp, range(lo // UNIT, (hi - 1) // UNIT + 1)

    @staticmethod
    def _ps_as_writes(reads, writes):
        extra = [r for r in reads if _rng(r)[0] == 'ps']
        return (list(writes) + extra) if extra else writes

    def _deps(self, eng, reads, writes, extra):
        deps = {}

        def add(tok):
            if tok is None:
                return
            k, v = tok
            if eng == 'pe' and k[0] == 'pe':
                return
            if deps.get(k, 0) < v:
                deps[k] = v
        for r in reads:
            sp, us = self._units(_rng(r))
            W = self.mem_w[sp]
            for u in us:
                add(W.get(u))
        for r in writes:
            sp, us = self._units(_rng(r))
            W, Rr = self.mem_w[sp], self.mem_r[sp]
            for u in us:
                add(W.get(u))
                d = Rr.get(u)
                if d:
                    for k, v in d.items():
                        add((k, v))
        for tok in extra:
            add(tok)
        return deps

    def _emit_waits(self, eng, deps):
        kn = self.known[eng]
        for k, v in deps.items():
            if kn.get(k, 0) >= v:
                continue
            kn[k] = v
            sem = self.sem[k]
            self.q[eng].append(lambda E, sem=sem, v=v: E.wait_ge(sem, v))

    def _commit(self, tok, reads, writes):
        for r in writes:
            sp, us = self._units(_rng(r))
            W, Rr = self.mem_w[sp], self.mem_r[sp]
            for u in us:
                W[u] = tok
                Rr[u] = {}
        k, v = tok
        for r in reads:
            sp, us = self._units(_rng(r))
            Rr = self.mem_r[sp]
            for u in us:
                d = Rr.get(u)
                if d is None:
                    d = Rr[u] = {}
                d[k] = v

    def op(self, eng, fn, reads=(), writes=(), extra=()):
        writes = self._ps_as_writes(reads, writes)
        deps = self._deps(eng, reads, writes, extra)
        self._emit_waits(eng, deps)
        if self.cnt[eng] >= EPOCH:
            self.epoch[eng] += 1
            self.cnt[eng] = 0
            self.sem[(eng, self.epoch[eng])] = self.nc.alloc_semaphore(f"tl_{eng}_{self.epoch[eng]}")
        self.cnt[eng] += 1
        k = (eng, self.epoch[eng])
        sem = self.sem[k]
        self.q[eng].append(lambda E, fn=fn, sem=sem: fn(E).then_inc(sem, 1))
        tok = (k, self.cnt[eng])
        self._commit(tok, reads, writes)
        self.ninstr += 1
        return tok

    def dma(self, fn, reads=(), writes=(), extra=(), q='sp'):
        i = self.dnext[q]
        self.dnext[q] = (i + 1) % self.nslots
        k = ('d', q, i)
        if k not in self.sem:
            self.sem[k] = self.nc.alloc_semaphore(f"dma_{q}_{i}")
            self.dslot[k] = 0
        deps = self._deps(q, reads, writes, extra)
        if self.dslot[k] > 0:
            deps[k] = max(deps.get(k, 0), self.dslot[k])
        self._emit_waits(q, deps)
        self.dslot[k] += 16
        sem = self.sem[k]
        self.q[q].append(lambda E, fn=fn, sem=sem: fn(E).then_inc(sem, 16))
        tok = (k, self.dslot[k])
        self._commit(tok, reads, writes)
        self.ninstr += 1
        return tok

    def finish(self, q='sp'):
        for e in ENGS:
            deps = {k: v for k, v in self.dslot.items() if k[1] == e and v > 0}
            self._emit_waits(e, deps)

    def run(self):
        nc = self.nc
        with nc.Block() as block:
            @block.tensor
            def _(E):
                for f in self.q['pe']:
                    f(E)

            @block.scalar
            def _(E):
                for f in self.q['act']:
                    f(E)

            @block.vector
            def _(E):
                for f in self.q['dve']:
                    f(E)

            @block.gpsimd
            def _(E):
                for f in self.q['pool']:
                    f(E)

            @block.sync
            def _(E):
                for f in self.q['sp']:
                    f(E)


D = 2048
KT = 16
SBT = 512
TT = SBT // 128
NEXP = 32
ALPHA = float(2 ** 0.25)
EPS = 1e-5
OFF_QA, OFF_KA, OFF_VA, OFF_QG, OFF_KG, OFF_VG, OFF_RG, OFF_AL, OFF_GA, OFF_GG = (
    0, 1024, 1152, 1280, 2304, 3328, 5376, 7424, 7440, 9488)
NEG = -30000.0


class _Stop(Exception):
    pass


def build_nc(T, dbg=False, do_moe=True, stage=99):
    NSB = T // SBT
    nc = bass.Bass("TRN2", target_bir_lowering=False)

    def din(name, shape):
        return nc.dram_tensor(name, list(shape), F32, kind="ExternalInput").ap()

    x_d = din("x", (T, D))
    cfm_d = din("c_fm", (128, KT))
    wada_d = din("w_ada", (D, 6 * D))
    bada_d = din("b_ada", (6 * D,))
    win_d = din("w_in", (D, 11536))
    wup_d = din("w_gla_up", (16, 1024))
    bgate_d = din("b_gate_fm", (128, 8))
    sinks_d = din("attn_sinks", (16,))
    ggain_d = din("gla_gain", (D,))
    wpa_d = din("w_pa", (1024, D))
    wpg_d = din("w_pg", (D, D))
    wo_d = din("w_out", (D, D))
    ln1g_d, ln1b_d = din("ln1_gain", (D,)), din("ln1_bias", (D,))
    ln2g_d, ln2b_d = din("ln2_gain", (D,)), din("ln2_bias", (D,))
    wr_d = din("w_router", (D, NEXP))
    br_d = din("b_router", (NEXP,))
    if do_moe:
        wgu_d = din("w_gate_up", (NEXP, D, 2 * D))
        bgu_d = din("b_gu_fm", (128, NEXP * 16 * 2))
        wd_d = din("w_down", (NEXP, D, D))
        bd_d = din("b_down", (NEXP, D))
    ident_d = din("ident", (128, 128))
    swab_d = din("swa_bias", (128, 16 * 256))
    gmask_d = din("gla_mask", (64, 64))
    out_d = nc.dram_tensor("out", [T, D], F32, kind="ExternalOutput").ap()
    dbg_d = {}
    if dbg:
        for nm, w in (("d_h", D), ("d_yatt", 1024), ("d_ygla", D), ("d_x1", D), ("d_lg", 32 * 2)):
            dbg_d[nm] = nc.dram_tensor(nm, [T, w], F32, kind="ExternalOutput").ap()
    ada_row = nc.dram_tensor("ada_row", [6 * D], F32).ap()
    S_dram = nc.dram_tensor("S_dram", [4, 128, 1024], F32).ap()

    S = Sched(nc)
    xres = [S.sb(f"xres{t}", (128, D), F32) for t in range(TT)]
    hT = S.sb("hT", (128, KT, SBT), BF16)
    yattT = S.sb("yattT", (128, 8, SBT), BF16)
    yglaT = S.sb("yglaT", (128, KT, SBT), BF16)
    wst = [S.sb(f"wst{i}", (128, 4096), F32) for i in range(2)]
    wbf = [S.sb(f"wbf{i}", (128, 4096), BF16) for i in range(2)]
    bc = [S.sb(f"bc{i}", (128, D), F32) for i in range(2)]
    tmpA = S.sb("tmpA", (128, D), F32)
    ident = S.sb("ident", (128, 128), F32)
    silu_c = S.sb("silu_c", (128, KT), F32)
    sinks = S.sb("sinks", (128, 16), F32)
    nbgate = S.sb("nbgate", (128, 8), F32)
    wr_sb = S.sb("wr_sb", (128, KT, NEXP), F32)
    br_bc = S.sb("br_bc", (128, NEXP), F32)
    halo_k = [S.sb(f"halo_k{g}", (128, 128), BF16) for g in range(2)]
    halo_v = [S.sb(f"halo_v{g}", (128, 64), BF16) for g in range(2)]
    gmask = S.sb("gmask", (64, 64), F32)
    if do_moe:
        bgu = S.sb("bgu", (128, NEXP * 32), F32)
    NST = 6
    st_bn = [S.sb(f"st_bn{i}", (128, 4, 6), F32) for i in range(NST)]
    st_mv = [S.sb(f"st_mv{i}", (128, 2), F32) for i in range(NST)]
    st_a = [S.sb(f"st_a{i}", (128, 1), F32) for i in range(NST)]
    st_b = [S.sb(f"st_b{i}", (128, 1), F32) for i in range(NST)]
    st_c = [S.sb(f"st_c{i}", (128, 1), F32) for i in range(NST)]
    RA = S.sb_off
    PS = [S.ps(f"ps{i}") for i in range(8)]
    cnt = {'ps': 0, 'w': 0, 'st': 0, 'cast': 0, 'bc': 0}

    def nps():
        cnt['ps'] += 1
        return PS[cnt['ps'] % 8]

    def nst():
        cnt['st'] += 1
        return cnt['st'] % NST

    def MM(out, lhsT, rhs, start, stop, R, W):
        S.op('pe', lambda E: E.matmul(out, lhsT=lhsT, rhs=rhs, start=start, stop=stop), R, W)

    def TR(out, in_, R, W):
        n = in_.shape[0]
        S.op('pe', lambda E: E.transpose(out=out, in_=in_, identity=ident[0:n, 0:n]), list(R) + [ident], W)

    def ACT(out, in_, func, R, W, bias=0.0, scale=1.0, accum=None):
        S.op('act', lambda E: E.activation(out=out, in_=in_, func=func, bias=bias, scale=scale, accum_out=accum), R, W)

    def TS(eng, out, in0, s1, s2, op0, op1, R, W):
        if op1 is None:
            S.op(eng, lambda E: E.tensor_scalar(out=out, in0=in0, scalar1=s1, scalar2=None, op0=op0), R, W)
        else:
            S.op(eng, lambda E: E.tensor_scalar(out=out, in0=in0, scalar1=s1, scalar2=s2, op0=op0, op1=op1), R, W)

    def TTo(eng, out, in0, in1, op, R, W):
        S.op(eng, lambda E: E.tensor_tensor(out=out, in0=in0, in1=in1, op=op), R, W)

    def STT(eng, out, in0, scalar, in1, op0, op1, R, W):
        S.op(eng, lambda E: E.scalar_tensor_tensor(out=out, in0=in0, scalar=scalar, in1=in1, op0=op0, op1=op1), R, W)

    def CP(eng, out, in_, R, W):
        if eng == 'act':
            S.op('act', lambda E: E.copy(out=out, in_=in_), R, W)
        else:
            S.op(eng, lambda E: E.tensor_copy(out=out, in_=in_), R, W)

    def LD(out, in_, W, extra=()):
        return S.dma(lambda E: E.dma_start(out=out, in_=in_), (), W, extra)

    def ST(out, in_, R, extra=()):
        return S.dma(lambda E: E.dma_start(out=out, in_=in_), R, (), extra)

    CAST_ENGS = ['dve', 'pool', 'act']

    def wload(srcs, kt, n, deint=False):
        cnt['w'] += 1
        i = cnt['w'] % 2
        sv = wst[i][:, 0:kt * n].rearrange("p (k n) -> p k n", k=kt)
        for (ap, c0, w) in srcs:
            LD(sv[:, :, c0:c0 + w], ap.rearrange("(k p) n -> p k n", p=128), [wst[i].rng(0, kt * n)])
        cnt['cast'] += 1
        eng = CAST_ENGS[cnt['cast'] % 3]
        if deint:
            ov = wbf[i][:, 0:kt * n].rearrange("p (k g f) -> p k g f", k=kt, g=2)
            iv = wst[i][:, 0:kt * n].rearrange("p (k f g) -> p k g f", k=kt, g=2)
            CP(eng, ov, iv, [wst[i].rng(0, kt * n)], [wbf[i].rng(0, kt * n)])
            return wbf[i], ov
        bv = wbf[i][:, 0:kt * n].rearrange("p (k n) -> p k n", k=kt)
        CP(eng, bv, sv, [wst[i].rng(0, kt * n)], [wbf[i].rng(0, kt * n)])
        return wbf[i], bv

    def bcload(src_ap, extra=(), plus1=False):
        cnt['bc'] += 1
        b = bc[cnt['bc'] % 2]
        LD(b[:], src_ap.partition_broadcast(128), [b], extra)
        if plus1:
            TS('pool', b[:], b[:], 1.0, None, ALU.add, None, [b], [b])
        return b

    def layernorm(src, dst_tile, dst_ap):
        i = nst()
        for c in range(4):
            S.op('dve', lambda E, c=c: E.bn_stats(out=st_bn[i][:, c, :], in_=src[:, c * 512:(c + 1) * 512]), [src], [st_bn[i]])
        S.op('dve', lambda E: E.bn_aggr(out=st_mv[i][:], in_=st_bn[i][:]), [st_bn[i]], [st_mv[i]])
        ACT(st_a[i][:], st_mv[i][:, 1:2], AF.Sqrt, [st_mv[i]], [st_a[i]], bias=EPS, scale=1.0)
        S.op('dve', lambda E: E.reciprocal(out=st_a[i][:], in_=st_a[i][:]), [st_a[i]], [st_a[i]])
        TS('dve', dst_ap, src[:], st_mv[i][:, 0:1], st_a[i][:, 0:1], ALU.subtract, ALU.mult, [src, st_mv[i], st_a[i]], [dst_tile])

    def transpose_tile(src, t, want_f32=None):
        for g in range(4):
            p = nps()
            for j in range(4):
                k = g * 4 + j
                TR(p[:, j * 128:(j + 1) * 128], src[:, k * 128:(k + 1) * 128], [src], [p])
            pv = p[:].rearrange("p (a b) -> p a b", a=4)
            CP('act', hT[:, g * 4:(g + 1) * 4, t * 128:(t + 1) * 128], pv, [p], [hT])
            if want_f32 is not None:
                CP('dve', want_f32[:, g * 4:(g + 1) * 4, :], pv, [p], [want_f32])

    LD(ident[:], ident_d, [ident])
    LD(silu_c[:], cfm_d, [silu_c])
    LD(sinks[:], sinks_d.partition_broadcast(128), [sinks])
    LD(nbgate[:], bgate_d, [nbgate])
    LD(wr_sb[:], wr_d.rearrange("(k p) n -> p k n", p=128), [wr_sb])
    LD(br_bc[:], br_d.partition_broadcast(128), [br_bc])
    LD(gmask[:], gmask_d, [gmask])
    if do_moe:
        LD(bgu[:], bgu_d, [bgu])
    for g in range(2):
        S.op('pool', lambda E, g=g: E.memset(halo_k[g][:], 0.0), [], [halo_k[g]])
        S.op('pool', lambda E, g=g: E.memset(halo_v[g][:], 0.0), [], [halo_v[g]])
    TS('dve', nbgate[:], nbgate[:], -1.0, None, ALU.mult, None, [nbgate], [nbgate])
    ACT(silu_c[:], silu_c[:], AF.Silu, [silu_c], [silu_c])
    ada_tok = []
    arow = [S.sb(f"arow{i}", (1, 256), F32, off=RA + i * 1024) for i in range(2)]
    brow = [S.sb(f"brow{i}", (1, 256), F32, off=RA + 2048 + i * 1024) for i in range(2)]
    for gi in range(6 * D // 256):
        i = gi % 2
        c0 = gi * 256
        sv = wst[i][:].rearrange("p (k n) -> p k n", k=KT)
        LD(sv, wada_d[:, c0:c0 + 256].rearrange("(k p) n -> p k n", p=128), [wst[i]])
        LD(brow[i][:], bada_d[c0:c0 + 256].rearrange("(o n) -> o n", o=1), [brow[i]])
        p = nps()
        for k in range(KT):
            MM(p[0:1, 0:256], silu_c[:, k:k + 1], sv[:, k, :], k == 0, k == KT - 1, [silu_c, wst[i]], [p.rng(0, 256)])
        TTo('dve', arow[i][:], p[0:1, 0:256], brow[i][:], ALU.add, [p.rng(0, 256), brow[i]], [arow[i]])
        ada_tok.append(ST(ada_row[c0:c0 + 256].rearrange("(o n) -> o n", o=1), arow[i][:], [arow[i]]))

    def ada_bc(idx, plus1=False):
        return bcload(ada_row[idx * D:(idx + 1) * D], extra=ada_tok, plus1=plus1)

    S_tok = [None] * 4

    def chk(k):
        if stage <= k:
            raise _Stop()

    def sb_body(s):
        t0 = s * SBT
        sc1p = ada_bc(1, plus1=True)
        sh1 = ada_bc(0)
        for t in range(TT):
            LD(xres[t][:], x_d[t0 + t * 128:t0 + (t + 1) * 128, :], [xres[t]])
            layernorm(xres[t], tmpA, tmpA[:])
            TTo('pool', tmpA[:], tmpA[:], sc1p[:], ALU.mult, [tmpA, sc1p], [tmpA])
            TTo('dve', tmpA[:], tmpA[:], sh1[:], ALU.add, [tmpA, sh1], [tmpA])
            if dbg:
                ST(dbg_d["d_h"][t0 + t * 128:t0 + (t + 1) * 128, :], tmpA[:], [tmpA])
            transpose_tile(tmpA, t)

        chk(1)
        o = RA
        swab = S.sb(f"swab_{s}", (128, 16, 256), F32, off=o); o += 16384
        qT = S.sb(f"qT_{s}", (128, 4, SBT), BF16, off=o); o += 4096
        kT = S.sb(f"kT_{s}", (128, 128 + SBT), BF16, off=o); o += 1280
        vv = S.sb(f"vv_{s}", (128, 5, 64), BF16, off=o); o += 640
        s_sb = [S.sb(f"s_sb{i}_{s}", (128, 256), F32, off=o + i * 1024) for i in range(2)]; o += 2048
        p_sb = [S.sb(f"p_sb{i}_{s}", (128, 256), F32, off=o + i * 1024) for i in range(2)]; o += 2048
        pT = [S.sb(f"pT{i}_{s}", (128, 2, 128), BF16, off=o + i * 512) for i in range(2)]; o += 1024
        yatt = S.sb(f"yatt_{s}", (128, 4, 1024), F32, off=o); o += 16384
        LD(swab[:], swab_d.rearrange("p (h n) -> p h n", h=16), [swab])
        for g in range(2):
            wt, wv = wload([(win_d[:, OFF_KA + g * 64:OFF_KA + g * 64 + 64], 0, 64),
                            (win_d[:, OFF_KA + g * 64:OFF_KA + g * 64 + 64], 64, 64),
                            (win_d[:, OFF_VA + g * 64:OFF_VA + g * 64 + 64], 128, 64)], KT, 192)
            p = nps()
            for k in range(KT):
                MM(p[:, :], wv[:, k, 0:128], hT[:, k, :], k == 0, k == KT - 1, [wt, hT], [p])
            CP('dve', kT[:, 0:128], halo_k[g][:], [halo_k[g]], [kT.rng(0, 128)])
            CP('act', kT[:, 128:128 + SBT], p[:, :], [p], [kT.rng(128, 128 + SBT)])
            CP('pool', vv[:, 0, :], halo_v[g][:], [halo_v[g]], [vv.rng(0, 64)])
            for t in range(TT):
                p = nps()
                for k in range(KT):
                    MM(p[:, 0:64], hT[:, k, t * 128:(t + 1) * 128], wv[:, k, 128:192], k == 0, k == KT - 1, [wt, hT], [p.rng(0, 64)])
                CP('dve', vv[:, t + 1, :], p[:, 0:64], [p.rng(0, 64)], [vv.rng((t + 1) * 64, (t + 2) * 64)])
            CP('pool', halo_k[g][:], kT[:, SBT:SBT + 128], [kT], [halo_k[g]])
            CP('pool', halo_v[g][:], vv[:, 4, :], [vv], [halo_v[g]])
            for half in range(2):
                c0 = OFF_QA + g * 512 + half * 256
                wt, wv = wload([(win_d[:, c0:c0 + 256], 0, 256)], KT, 256)
                for jb in range(2):
                    p = nps()
                    for k in range(KT):
                        MM(p[:, :], wv[:, k, jb * 128:(jb + 1) * 128], hT[:, k, :], k == 0, k == KT - 1, [wt, hT], [p])
                    ACT(qT[:, half * 2 + jb, :], p[:, :], AF.Copy, [p], [qT], scale=0.125)
            for hl in range(8):
                h = g * 8 + hl
                jb, hf = hl // 2, hl % 2
                r0, r1 = hf * 64, hf * 64 + 64
                for n in range(TT):
                    first = (s == 0 and n == 0)
                    NS = 128 if first else 256
                    kc0 = 128 + n * 128 if first else n * 128
                    i2 = (hl * TT + n) % 2
                    ii = nst()
                    p = nps()
                    MM(p[:, 0:NS], qT[r0:r1, jb, n * 128:(n + 1) * 128], kT[r0:r1, kc0:kc0 + NS], True, True, [qT, kT], [p.rng(0, NS)])
                    TTo('dve', s_sb[i2][:, 0:NS], p[:, 0:NS], swab[:, h, 256 - NS:256], ALU.add, [p.rng(0, NS), swab], [s_sb[i2]])
                    S.op('dve', lambda E, i2=i2, NS=NS, ii=ii: E.tensor_reduce(out=st_a[ii][:], in_=s_sb[i2][:, 0:NS], axis=AX.X, op=ALU.max), [s_sb[i2]], [st_a[ii]])
                    TS('dve', st_a[ii][:], st_a[ii][:], sinks[:, h:h + 1], -1.0, ALU.max, ALU.mult, [st_a[ii], sinks], [st_a[ii]])
                    ACT(p_sb[i2][:, 0:NS], s_sb[i2][:, 0:NS], AF.Exp, [s_sb[i2], st_a[ii]], [p_sb[i2], st_b[ii]], bias=st_a[ii][:, 0:1], accum=st_b[ii][:, 0:1])
                    ACT(st_c[ii][:], sinks[:, h:h + 1], AF.Exp, [sinks, st_a[ii]], [st_c[ii]], bias=st_a[ii][:, 0:1])
                    TTo('dve', st_b[ii][:], st_b[ii][:], st_c[ii][:], ALU.add, [st_b[ii], st_c[ii]], [st_b[ii]])
                    S.op('dve', lambda E, ii=ii: E.reciprocal(out=st_b[ii][:], in_=st_b[ii][:]), [st_b[ii]], [st_b[ii]])
                    p2 = nps()
                    nsb = NS // 128
                    for sbk in range(nsb):
                        TR(p2[:, sbk * 128:(sbk + 1) * 128], p_sb[i2][:, sbk * 128:(sbk + 1) * 128], [p_sb[i2]], [p2.rng(0, NS)])
                    CP('act', pT[i2][:, 0:nsb, :], p2[:, 0:NS].rearrange("p (a b) -> p a b", a=nsb), [p2.rng(0, NS)], [pT[i2]])
                    p3 = nps()
                    for sbk in range(nsb):
                        vt = n + 1 if first else n + sbk
                        MM(p3[:, 0:64], pT[i2][:, sbk, :], vv[:, vt, :], sbk == 0, sbk == nsb - 1, [pT[i2], vv], [p3.rng(0, 64)])
                    TS('dve', yatt[:, n, h * 64:(h + 1) * 64], p3[:, 0:64], st_b[ii][:, 0:1], None, ALU.mult, None, [p3.rng(0, 64), st_b[ii]], [yatt.rng(n * 1024 + h * 64, n * 1024 + h * 64 + 64)])
        for n in range(TT):
            if dbg:
                ST(dbg_d["d_yatt"][t0 + n * 128:t0 + (n + 1) * 128, :], yatt[:, n, :], [yatt])
            for g2 in range(2):
                p = nps()
                for j in range(4):
                    k = g2 * 4 + j
                    TR(p[:, j * 128:(j + 1) * 128], yatt[:, n, k * 128:(k + 1) * 128], [yatt], [p])
                CP('act', yattT[:, g2 * 4:(g2 + 1) * 4, n * 128:(n + 1) * 128], p[:].rearrange("p (a b) -> p a b", a=4), [p], [yattT])

        chk(2)
        o = RA
        Ebuf = S.sb(f"Ebuf_{s}", (128, 2, SBT), F32, off=o); o += 4096
        L0 = S.sb(f"L0_{s}", (128, 2, SBT), F32, off=o); o += 4096
        L1 = S.sb(f"L1_{s}", (128, 2, SBT), F32, off=o); o += 4096
        qe = S.sb(f"qe_{s}", (128, 2, SBT), BF16, off=o); o += 2048
        ke = S.sb(f"ke_{s}", (128, 2, SBT), BF16, off=o); o += 2048
        kendT = S.sb(f"kendT_{s}", (128, 2, SBT), F32, off=o); o += 4096
        kend = S.sb(f"kend_{s}", (64, 8, 256), BF16, off=o); o += 4096
        vg = S.sb(f"vg_{s}", (64, 8, 512), BF16, off=o); o += 8192
        rs = S.sb(f"rs_{s}", (64, 8, 512), BF16, off=o); o += 8192
        Sst = S.sb(f"Sst_{s}", (128, 2, 512), F32, off=o); o += 4096
        Sb = S.sb(f"Sb_{s}", (128, 2, 512), BF16, off=o); o += 2048
        ytmp1 = S.sb(f"ytmp_{s}", (64, 512), F32, off=o); o += 2048
        ytmp = [ytmp1, ytmp1]
        gain_h = S.sb(f"gain_h_{s}", (64, 512), F32, off=o); o += 2048
        alrT = S.sb(f"alrT_{s}", (16, SBT), F32, off=o); o += 2048
        wup = S.sb(f"wup_{s}", (16, 256), F32, off=o); o += 1024
        AT = [S.sb(f"AT{i}_{s}", (64, 64), BF16, off=o + i * 128) for i in range(2)]; o += 256
        nll = S.sb(f"nll_{s}", (128, 2, 8), F32, off=o); o += 64
        dec = S.sb(f"dec_{s}", (128, 2, 8), F32, off=o); o += 64
        assert o <= SB_END, (o, RA)
        wt, wv = wload([(win_d[:, OFF_AL:OFF_AL + 16], 0, 16)], KT, 16)
        p = nps()
        for k in range(KT):
            MM(p[0:16, :], wv[:, k, :], hT[:, k, :], k == 0, k == KT - 1, [wt, hT], [p])
        CP('act', alrT[:], p[0:16, :], [p], [alrT])
        for hd in range(4):
            LD(wup[:], wup_d[:, hd * 256:(hd + 1) * 256], [wup])
            for dc in range(2):
                p = nps()
                MM(p[:, :], wup[0:16, dc * 128:(dc + 1) * 128], alrT[0:16, :], True, True, [wup, alrT], [p])
                ACT(Ebuf[:, dc, :], p[:, :], AF.Exp, [p, nbgate], [Ebuf.rng(dc * SBT, (dc + 1) * SBT)], bias=nbgate[:, hd * 2 + dc:hd * 2 + dc + 1], scale=-1.0)
                ACT(L0[:, dc, :], Ebuf[:, dc, :], AF.Ln, [Ebuf.rng(dc * SBT, (dc + 1) * SBT)], [L0.rng(dc * SBT, (dc + 1) * SBT)], bias=1.0, scale=1.0)
            src, dst = L0, L1
            for sh in (1, 2, 4, 8, 16, 32):
                sv4 = src[:].rearrange("p d (c w) -> p d c w", w=64)
                dv4 = dst[:].rearrange("p d (c w) -> p d c w", w=64)
                TTo('dve', dv4[:, :, :, sh:64], sv4[:, :, :, sh:64], sv4[:, :, :, 0:64 - sh], ALU.add, [src], [dst])
                CP('pool', dv4[:, :, :, 0:sh], sv4[:, :, :, 0:sh], [src], [dst])
                src, dst = dst, src
            Lc = src
            Lc4 = Lc[:].rearrange("p d (c w) -> p d c w", w=64)
            TS('dve', nll[:], Lc4[:, :, :, 63], -1.0 / 16.0, None, ALU.mult, None, [Lc], [nll])
            ACT(dec[:], nll[:], AF.Exp, [nll], [dec])
            wt, wv = wload([(win_d[:, OFF_QG + hd * 256:OFF_QG + hd * 256 + 256], 0, 256)], KT, 256)
            for dc in range(2):
                p = nps()
                for k in range(KT):
                    MM(p[:, :], wv[:, k, dc * 128:(dc + 1) * 128], hT[:, k, :], k == 0, k == KT - 1, [wt, hT], [p])
                ACT(Ebuf[:, dc, :], Lc[:, dc, :], AF.Exp, [Lc], [Ebuf.rng(dc * SBT, (dc + 1) * SBT)], scale=-1.0 / 16.0)
                STT('dve', qe[:, dc, :], p[:, :], 1.0 / 16.0, Ebuf[:, dc, :], ALU.mult, ALU.mult, [p, Ebuf.rng(dc * SBT, (dc + 1) * SBT)], [qe.rng(dc * SBT, (dc + 1) * SBT)])
            wt, wv = wload([(win_d[:, OFF_KG + hd * 256:OFF_KG + hd * 256 + 256], 0, 256)], KT, 256)
            for dc in range(2):
                p = nps()
                for k in range(KT):
                    MM(p[:, :], wv[:, k, dc * 128:(dc + 1) * 128], hT[:, k, :], k == 0, k == KT - 1, [wt, hT], [p])
                er = Ebuf.rng(dc * SBT, (dc + 1) * SBT)
                ACT(Ebuf[:, dc, :], Lc[:, dc, :], AF.Exp, [Lc], [er], scale=1.0 / 16.0)
                TTo('dve', ke[:, dc, :], p[:, :], Ebuf[:, dc, :], ALU.mult, [p, er], [ke.rng(dc * SBT, (dc + 1) * SBT)])
                for c in range(8):
                    ACT(Ebuf[:, dc, c * 64:(c + 1) * 64], Lc[:, dc, c * 64:(c + 1) * 64], AF.Exp, [Lc, nll], [er], bias=nll[:, dc, c:c + 1], scale=1.0 / 16.0)
                TTo('dve', kendT[:, dc, :], p[:, :], Ebuf[:, dc, :], ALU.mult, [p, er], [kendT.rng(dc * SBT, (dc + 1) * SBT)])
            for c in range(8):
                p = nps()
                for dc in range(2):
                    TR(p[0:64, dc * 128:(dc + 1) * 128], kendT[:, dc, c * 64:(c + 1) * 64], [kendT], [p.rng(0, 256)])
                CP('act', kend[:, c, :], p[0:64, 0:256], [p.rng(0, 256)], [kend.rng(c * 256, (c + 1) * 256)])
            for (OFFX, dstt, is_r) in ((OFF_VG, vg, False), (OFF_RG, rs, True)):
                for half in range(2):
                    c0 = OFFX + hd * 512 + half * 256
                    wt, wv = wload([(win_d[:, c0:c0 + 256], 0, 256)], KT, 256)
                    for c in range(8):
                        p = nps()
                        for k in range(KT):
                            MM(p[0:64, 0:256], hT[:, k, c * 64:(c + 1) * 64], wv[:, k, :], k == 0, k == KT - 1, [wt, hT], [p.rng(0, 256)])
                        wr_ = dstt.rng(c * 512 + half * 256, c * 512 + half * 256 + 256)
                        if is_r:
                            ACT(dstt[:, c, half * 256:(half + 1) * 256], p[0:64, 0:256], AF.Silu, [p.rng(0, 256)], [wr_])
                        else:
                            CP('dve', dstt[:, c, half * 256:(half + 1) * 256], p[0:64, 0:256], [p.rng(0, 256)], [wr_])
            LD(gain_h[:], ggain_d[hd * 512:(hd + 1) * 512].partition_broadcast(64), [gain_h])
            if s == 0:
                S.op('pool', lambda E: E.memset(Sst[:], 0.0), [], [Sst])
            else:
                LD(Sst[:], S_dram[hd].rearrange("p (d v) -> p d v", d=2), [Sst], extra=[S_tok[hd]])
            CP('act', Sb[:], Sst[:], [Sst], [Sb])
            for c in range(8):
                cs = slice(c * 64, (c + 1) * 64)
                i2 = c % 2
                ii = nst()
                pa = nps()
                for dc in range(2):
                    MM(pa[0:64, 0:64], ke[:, dc, cs], qe[:, dc, cs], dc == 0, dc == 1, [ke, qe], [pa.rng(0, 64)])
                TTo('dve', AT[i2][:], pa[0:64, 0:64], gmask[:], ALU.mult, [pa.rng(0, 64), gmask], [AT[i2]])
                po = nps()
                MM(po[0:64, :], AT[i2][:], vg[:, c, :], True, False, [AT[i2], vg], [po])
                MM(po[0:64, :], qe[:, 0, cs], Sb[:, 0, :], False, False, [qe, Sb], [po])
                MM(po[0:64, :], qe[:, 1, cs], Sb[:, 1, :], False, True, [qe, Sb], [po])
                for dc in range(2):
                    pS = nps()
                    MM(pS[:, :], kend[:, c, dc * 128:(dc + 1) * 128], vg[:, c, :], True, True, [kend, vg], [pS])
                    sr = Sst.rng(dc * 512, (dc + 1) * 512)
                    STT('dve', Sst[:, dc, :], Sst[:, dc, :], dec[:, dc, c:c + 1], pS[:, :], ALU.mult, ALU.add, [sr, dec, pS], [sr])
                    CP('pool', Sb[:, dc, :], Sst[:, dc, :], [sr], [Sb.rng(dc * 512, (dc + 1) * 512)])
                ACT(ytmp[i2][:], po[0:64, :], AF.Square, [po], [ytmp[i2], st_a[ii]], accum=st_a[ii][0:64, 0:1])
                ACT(st_a[ii][0:64, :], st_a[ii][0:64, :], AF.Sqrt, [st_a[ii]], [st_a[ii]], bias=EPS, scale=1.0 / 512.0)
                S.op('dve', lambda E, ii=ii: E.reciprocal(out=st_a[ii][0:64, :], in_=st_a[ii][0:64, :]), [st_a[ii]], [st_a[ii]])
                STT('dve', ytmp[i2][:], po[0:64, :], st_a[ii][0:64, 0:1], gain_h[:], ALU.mult, ALU.mult, [po, st_a[ii], gain_h], [ytmp[i2]])
                TTo('pool', ytmp[i2][:], ytmp[i2][:], rs[:, c, :], ALU.mult, [ytmp[i2], rs], [ytmp[i2]])
                if dbg:
                    ST(dbg_d["d_ygla"][t0 + c * 64:t0 + (c + 1) * 64, hd * 512:(hd + 1) * 512], ytmp[i2][:], [ytmp[i2]])
                pt = nps()
                for k4 in range(4):
                    TR(pt[:, k4 * 64:(k4 + 1) * 64], ytmp[i2][:, k4 * 128:(k4 + 1) * 128], [ytmp[i2]], [pt.rng(0, 256)])
                CP('act', yglaT[:, hd * 4:(hd + 1) * 4, cs], pt[:, 0:256].rearrange("p (a b) -> p a b", a=4), [pt.rng(0, 256)], [yglaT])
            S_tok[hd] = ST(S_dram[hd].rearrange("p (d v) -> p d v", d=2), Sst[:], [Sst])

        chk(3)
        o = RA
        G = S.sb(f"G_{s}", (128, TT, NEXP), F32, off=o); o += 512
        GT = S.sb(f"GT_{s}", (32, SBT), F32, off=o); o += 2048
        P5O2 = o
        mrgT = S.sb(f"mrgT_{s}", (128, KT, SBT), BF16, off=o); o += 16384
        sga = S.sb(f"sga_{s}", (128, SBT), F32, off=o); o += 2048
        sgg = S.sb(f"sgg_{s}", (128, SBT), F32, off=o); o += 2048
        mt = [S.sb(f"mt{i}_{s}", (128, SBT), F32, off=o + i * 2048) for i in range(2)]; o += 4096
        hTf = S.sb(f"hTf_{s}", (128, KT, 128), F32, off=o); o += 8192
        lg = S.sb(f"lg_{s}", (128, NEXP), F32, off=o); o += 128
        ex = S.sb(f"ex_{s}", (128, NEXP), F32, off=o); o += 128
        m8 = S.sb(f"m8_{s}", (128, 8), F32, off=o); o += 64
        assert o <= SB_END, (o, RA)
        for j in range(KT):
            cs = slice(j * 128, (j + 1) * 128)
            wt, wv = wload([(wpa_d[:, cs], 0, 128)], 8, 128)
            ppa = nps()
            for k in range(8):
                MM(ppa[:, :], wv[:, k, :], yattT[:, k, :], k == 0, k == 7, [wt, yattT], [ppa])
            wt, wv = wload([(wpg_d[:, cs], 0, 128)], KT, 128)
            ppg = nps()
            for k in range(KT):
                MM(ppg[:, :], wv[:, k, :], yglaT[:, k, :], k == 0, k == KT - 1, [wt, yglaT], [ppg])
            wt, wv = wload([(win_d[:, OFF_GA + j * 128:OFF_GA + (j + 1) * 128], 0, 128),
                            (win_d[:, OFF_GG + j * 128:OFF_GG + (j + 1) * 128], 128, 128)], KT, 256)
            pga = nps()
            for k in range(KT):
                MM(pga[:, :], wv[:, k, 0:128], hT[:, k, :], k == 0, k == KT - 1, [wt, hT], [pga])
            pgg = nps()
            for k in range(KT):
                MM(pgg[:, :], wv[:, k, 128:256], hT[:, k, :], k == 0, k == KT - 1, [wt, hT], [pgg])
            ACT(sga[:], pga[:, :], AF.Sigmoid, [pga], [sga])
            ACT(sgg[:], pgg[:, :], AF.Sigmoid, [pgg], [sgg])
            TTo('dve', mt[0][:], ppa[:, :], sga[:], ALU.mult, [ppa, sga], [mt[0]])
            TTo('dve', mt[1][:], ppg[:, :], sgg[:], ALU.mult, [ppg, sgg], [mt[1]])
            TTo('pool', mrgT[:, j, :], mt[0][:], mt[1][:], ALU.add, [mt[0], mt[1]], [mrgT.rng(j * SBT, (j + 1) * SBT)])
        chk(3.2)
        gt1 = ada_bc(2)
        for c8 in range(8):
            cs = slice(c8 * 256, (c8 + 1) * 256)
            wt, wv = wload([(wo_d[:, cs], 0, 256)], KT, 256)
            for t in range(TT):
                p = nps()
                for k in range(KT):
                    MM(p[:, 0:256], mrgT[:, k, t * 128:(t + 1) * 128], wv[:, k, :], k == 0, k == KT - 1, [wt, mrgT], [p.rng(0, 256)])
                i2 = (c8 * TT + t) % 2
                xr = xres[t].rng(c8 * 256, (c8 + 1) * 256)
                TTo('dve', mt[i2][:, 0:256], p[:, 0:256], gt1[:, cs], ALU.mult, [p.rng(0, 256), gt1], [mt[i2]])
                STT('dve', xres[t][:, cs], xres[t][:, cs], ALPHA, mt[i2][:, 0:256], ALU.mult, ALU.add, [xr, mt[i2]], [xr])
        g1 = bcload(ln1g_d)
        for t in range(TT):
            layernorm(xres[t], xres[t], xres[t][:])
            TTo('pool', xres[t][:], xres[t][:], g1[:], ALU.mult, [xres[t], g1], [xres[t]])
        b1 = bcload(ln1b_d)
        for t in range(TT):
            TTo('dve', xres[t][:], xres[t][:], b1[:], ALU.add, [xres[t], b1], [xres[t]])
            if dbg:
                ST(dbg_d["d_x1"][t0 + t * 128:t0 + (t + 1) * 128, :], xres[t][:], [xres[t]])
        chk(3.5)
        sc2p = ada_bc(4, plus1=True)
        sh2 = ada_bc(3)
        for t in range(TT):
            layernorm(xres[t], tmpA, tmpA[:])
            TTo('pool', tmpA[:], tmpA[:], sc2p[:], ALU.mult, [tmpA, sc2p], [tmpA])
            TTo('dve', tmpA[:], tmpA[:], sh2[:], ALU.add, [tmpA, sh2], [tmpA])
            transpose_tile(tmpA, t, want_f32=hTf)
            p = nps()
            for k in range(KT):
                MM(p[:, 0:NEXP], hTf[:, k, :], wr_sb[:, k, :], k == 0, k == KT - 1, [hTf, wr_sb], [p.rng(0, NEXP)])
            ii = nst()
            TTo('dve', lg[:], p[:, 0:NEXP], br_bc[:], ALU.add, [p.rng(0, NEXP), br_bc], [lg])
            S.op('dve', lambda E: E.max(out=m8[:], in_=lg[:]), [lg], [m8])
            TS('dve', st_a[ii][:], m8[:, 0:1], -1.0, None, ALU.mult, None, [m8], [st_a[ii]])
            ACT(ex[:], lg[:], AF.Exp, [lg, st_a[ii]], [ex], bias=st_a[ii][:, 0:1])
            TS('dve', lg[:], lg[:], m8[:, 3:4], None, ALU.is_ge, None, [lg, m8], [lg])
            TTo('dve', ex[:], ex[:], lg[:], ALU.mult, [ex, lg], [ex])
            S.op('dve', lambda E, ii=ii: E.tensor_reduce(out=st_b[ii][:], in_=ex[:], axis=AX.X, op=ALU.add), [ex], [st_b[ii]])
            S.op('dve', lambda E, ii=ii: E.reciprocal(out=st_b[ii][:], in_=st_b[ii][:]), [st_b[ii]], [st_b[ii]])
            TS('dve', G[:, t, :], ex[:], st_b[ii][:, 0:1], None, ALU.mult, None, [ex, st_b[ii]], [G.rng(t * NEXP, (t + 1) * NEXP)])
            if dbg:
                ST(dbg_d["d_lg"][t0 + t * 128:t0 + (t + 1) * 128, 0:32], G[:, t, :], [G])
            p = nps()
            TR(p[0:32, 0:128], G[:, t, :], [G], [p.rng(0, 128)])
            CP('act', GT[:, t * 128:(t + 1) * 128], p[0:32, 0:128], [p.rng(0, 128)], [GT.rng(t * 128, (t + 1) * 128)])

        chk(4)
        o = P5O2
        accf = [S.sb(f"accf{t}_{s}", (128, D), F32, off=o + t * 8192) for t in range(TT)]; o += 4 * 8192
        actT = S.sb(f"actT_{s}", (128, KT, SBT), BF16, off=o); o += 16384
        et = [S.sb(f"et{i}_{s}", (128, SBT), F32, off=tmpA.addr + i * 2048) for i in range(3)]
        assert o <= SB_END, o
        if do_moe:
            bdt = bc[(cnt['bc'] + 1) % 2]
            cnt['bc'] += 1
            LD(bdt[0:32, :], bd_d, [bdt])
            for t in range(TT):
                for c4 in range(4):
                    cs = slice(c4 * 512, (c4 + 1) * 512)
                    p = nps()
                    MM(p[:, :], GT[0:32, t * 128:(t + 1) * 128], bdt[0:32, cs], True, True, [GT, bdt], [p])
                    CP('act' if c4 % 2 else 'dve', accf[t][:, cs], p[:, :], [p], [accf[t].rng(c4 * 512, (c4 + 1) * 512)])
            for e in range(NEXP):
                for j in range(KT):
                    wt, wv = wload([(wgu_d[e][:, j * 256:(j + 1) * 256], 0, 256)], KT, 256, deint=True)
                    pg = nps()
                    for k in range(KT):
                        MM(pg[:, :], wv[:, k, 0, :], hT[:, k, :], k == 0, k == KT - 1, [wt, hT], [pg])
                    pu = nps()
                    for k in range(KT):
                        MM(pu[:, :], wv[:, k, 1, :], hT[:, k, :], k == 0, k == KT - 1, [wt, hT], [pu])
                    bcol = (e * 16 + j) * 2
                    TS('dve', et[0][:], pg[:, :], bgu[:, bcol:bcol + 1], 7.0, ALU.add, ALU.min, [pg, bgu], [et[0]])
                    ACT(et[1][:], et[0][:], AF.Sigmoid, [et[0]], [et[1]], scale=1.702)
                    TS('dve', et[2][:], pu[:, :], bgu[:, bcol + 1:bcol + 2], 7.0, ALU.add, ALU.min, [pu, bgu], [et[2]])
                    TS('pool', et[2][:], et[2][:], -7.0, 1.0, ALU.max, ALU.add, [et[2]], [et[2]])
                    TTo('pool', et[0][:], et[0][:], et[1][:], ALU.mult, [et[0], et[1]], [et[0]])
                    TTo('dve', actT[:, j, :], et[0][:], et[2][:], ALU.mult, [et[0], et[2]], [actT.rng(j * SBT, (j + 1) * SBT)])
                for c8 in range(8):
                    cs = slice(c8 * 256, (c8 + 1) * 256)
                    wt, wv = wload([(wd_d[e][:, cs], 0, 256)], KT, 256)
                    for t in range(TT):
                        p = nps()
                        for k in range(KT):
                            MM(p[:, 0:256], actT[:, k, t * 128:(t + 1) * 128], wv[:, k, :], k == 0, k == KT - 1, [wt, actT], [p.rng(0, 256)])
                        ar = accf[t].rng(c8 * 256, (c8 + 1) * 256)
                        STT('dve', accf[t][:, cs], p[:, 0:256], G[:, t, e:e + 1], accf[t][:, cs], ALU.mult, ALU.add, [p.rng(0, 256), G, ar], [ar])
        else:
            for t in range(TT):
                S.op('pool', lambda E, t=t: E.memset(accf[t][:], 0.0), [], [accf[t]])

        gt2 = ada_bc(5)
        for t in range(TT):
            TTo('pool', accf[t][:], accf[t][:], gt2[:], ALU.mult, [accf[t], gt2], [accf[t]])
            STT('dve', xres[t][:], xres[t][:], ALPHA, accf[t][:], ALU.mult, ALU.add, [xres[t], accf[t]], [xres[t]])
        g2 = bcload(ln2g_d)
        for t in range(TT):
            layernorm(xres[t], xres[t], xres[t][:])
            TTo('pool', xres[t][:], xres[t][:], g2[:], ALU.mult, [xres[t], g2], [xres[t]])
        b2 = bcload(ln2b_d)
        for t in range(TT):
            TTo('dve', xres[t][:], xres[t][:], b2[:], ALU.add, [xres[t], b2], [xres[t]])
            ST(out_d[t0 + t * 128:t0 + (t + 1) * 128, :], xres[t][:], [xres[t]])

    try:
        for s in range(NSB if stage > 0 else 0):
            sb_body(s)
    except _Stop:
        pass
    S.finish()
    S.run()
    return nc, S


def _consts():
    ident = np.eye(128, dtype=np.float32)
    q = np.arange(128)[:, None]
    s_ = np.arange(256)[None, :]
    dist = (q + 128 - s_).astype(np.float32)
    valid = (dist >= 0) & (dist < 128)
    slopes = (2.0 ** (-8.0 * (np.arange(16, dtype=np.float32) + 1.0) / 16)).astype(np.float32)
    swab = np.where(valid[:, None, :], -slopes[None, :, None] * dist[:, None, :], np.float32(NEG)).astype(np.float32)
    gmask = (np.arange(64)[:, None] <= np.arange(64)[None, :]).astype(np.float32)
    return ident, np.ascontiguousarray(swab.reshape(128, 16 * 256)), gmask


def make_in_maps(inputs, batches, T, do_moe=True):
    f = lambda a: np.ascontiguousarray(np.asarray(a, dtype=np.float32))
    ident, swab, gmask = _consts()
    shared = dict(
        w_ada=f(inputs["w_ada"][0]), b_ada=f(inputs["b_ada"][0]), w_in=f(inputs["w_in"][0]),
        w_gla_up=f(inputs["w_gla_gate_up"][0]),
        b_gate_fm=f(np.asarray(inputs["b_gla_gate"][0]).reshape(8, 128).T),
        attn_sinks=f(inputs["attn_sinks"][0]), gla_gain=f(inputs["gla_norm_gain"][0]),
        w_pa=f(inputs["w_branch_att"][0]), w_pg=f(inputs["w_branch_gla"][0]), w_out=f(inputs["w_out"][0]),
        ln1_gain=f(inputs["ln1_gain"][0]), ln1_bias=f(inputs["ln1_bias"][0]),
        ln2_gain=f(inputs["ln2_gain"][0]), ln2_bias=f(inputs["ln2_bias"][0]),
        w_router=f(inputs["w_router"][0]), b_router=f(inputs["b_router"][0]),
        ident=ident, swa_bias=swab, gla_mask=gmask)
    if do_moe:
        bgu = np.asarray(inputs["b_gate_up"][0]).reshape(NEXP, 16, 128, 2)
        shared.update(
            w_gate_up=f(inputs["w_gate_up"][0]), w_down=f(inputs["w_down"][0]), b_down=f(inputs["b_down"][0]),
            b_gu_fm=f(bgu.transpose(2, 0, 1, 3).reshape(128, NEXP * 32)))
    maps = []
    for b in batches:
        m = dict(shared)
        m["x"] = f(inputs["x"][b, :T])
        m["c_fm"] = f(np.asarray(inputs["c"][b]).reshape(KT, 128).T)
        maps.append(m)
    return maps


N_CORES = 4
SEQ = 4096


def kernel(**inputs):
    nc, _ = build_nc(SEQ)
    maps = make_in_maps(inputs, list(range(N_CORES)), SEQ)
    res = run_bass_kernel_spmd(nc, maps, core_ids=list(range(N_CORES)))
    return np.stack([np.asarray(r["out"], dtype=np.float32) for r in res.results], axis=0)
```

```python
import numpy as np
import concourse.bass as bass
import concourse.mybir as mybir
from concourse.bass_utils import run_bass_kernel_spmd

F32 = mybir.dt.float32
BF16 = mybir.dt.bfloat16
AF = mybir.ActivationFunctionType
ALU = mybir.AluOpType
AX = mybir.AxisListType
ISZ = {F32: 4, BF16: 2}

UNIT = 256
SB_BASE = 16512
SB_END = 229376
EPOCH = 30000
ENGS = ['pe', 'act', 'dve', 'pool', 'sp']


class Tile:
    def __init__(self, h, space, addr, shape, dtype):
        self.h, self.space, self.addr, self.shape, self.dtype = h, space, addr, shape, dtype
        self.isz = ISZ[dtype]
        n = 1
        for s in shape[1:]:
            n *= s
        self.nbytes = n * self.isz

    def __getitem__(self, k):
        return self.h[k]

    def rng(self, lo=None, hi=None):
        lo = 0 if lo is None else lo * self.isz
        hi = self.nbytes if hi is None else hi * self.isz
        return (self.space, self.addr + lo, self.addr + hi)


def _al(v):
    return ((v + UNIT - 1) // UNIT) * UNIT


def _rng(x):
    return x.rng() if isinstance(x, Tile) else x


class Sched:
    def __init__(self, nc, n_dma_slots=24):
        self.nc = nc
        self.eng = {'pe': nc.tensor, 'act': nc.scalar, 'dve': nc.vector, 'pool': nc.gpsimd, 'sp': nc.sync}
        self.q = {e: [] for e in ENGS}
        self.sem = {}
        self.epoch = {e: 0 for e in ENGS}
        self.cnt = {e: 0 for e in ENGS}
        for e in ENGS:
            self.sem[(e, 0)] = nc.alloc_semaphore(f"tl_{e}_0")
        self.known = {e: {} for e in ENGS}
        self.mem_w = {'sb': {}, 'ps': {}}
        self.mem_r = {'sb': {}, 'ps': {}}
        self.sb_off = SB_BASE
        self.ps_n = 0
        self.nslots = n_dma_slots
        self.dslot = {}
        self.dnext = {e: 0 for e in ENGS}
        self.ninstr = 0

    def sb(self, name, shape, dtype, off=None):
        t = Tile(None, 'sb', 0, shape, dtype)
        if off is None:
            al = UNIT if t.nbytes >= UNIT else 64
            off = ((self.sb_off + al - 1) // al) * al
            self.sb_off = off + ((t.nbytes + 63) // 64) * 64
        t.addr = off
        assert off >= SB_BASE and off + t.nbytes <= SB_END, (name, off, t.nbytes)
        t.h = self.nc.alloc_sbuf_tensor_at(name, list(shape), dtype, offset=off)
        return t

    def ps(self, name, shape=(128, 512), dtype=F32):
        t = Tile(None, 'ps', self.ps_n * 2048, shape, dtype)
        assert t.nbytes <= 2048
        self.ps_n += 1
        t.h = self.nc.alloc_psum_tensor(name, list(shape), dtype)
        return t

    def _units(self, r):
        sp, lo, hi = r
        if sp == 'ps':
            return sp, range((lo // 2048) * (2048 // UNIT), ((hi - 1) // 2048 + 1) * (2048 // UNIT))
        return sp, range(lo // UNIT, (hi - 1) // UNIT + 1)

    @staticmethod
    def _ps_as_writes(reads, writes):
        extra = [r for r in reads if _rng(r)[0] == 'ps']
        return (list(writes) + extra) if extra else writes

    def _deps(self, eng, reads, writes, extra):
        deps = {}

        def add(tok):
            if tok is None:
                return
            k, v = tok
            if eng == 'pe' and k[0] == 'pe':
                return
            if deps.get(k, 0) < v:
                deps[k] = v
        for r in reads:
            sp, us = self._units(_rng(r))
            W = self.mem_w[sp]
            for u in us:
                add(W.get(u))
        for r in writes:
            sp, us = self._units(_rng(r))
            W, Rr = self.mem_w[sp], self.mem_r[sp]
            for u in us:
                add(W.get(u))
                d = Rr.get(u)
                if d:
                    for k, v in d.items():
                        add((k, v))
        for tok in extra:
            add(tok)
        return deps

    def _emit_waits(self, eng, deps):
        kn = self.known[eng]
        for k, v in deps.items():
            if kn.get(k, 0) >= v:
                continue
            kn[k] = v
            sem = self.sem[k]
            self.q[eng].append(lambda E, sem=sem, v=v: E.wait_ge(sem, v))

    def _commit(self, tok, reads, writes):
        for r in writes:
            sp, us = self._units(_rng(r))
            W, Rr = self.mem_w[sp], self.mem_r[sp]
            for u in us:
                W[u] = tok
                Rr[u] = {}
        k, v = tok
        for r in reads:
            sp, us = self._units(_rng(r))
            Rr = self.mem_r[sp]
            for u in us:
                d = Rr.get(u)
                if d is None:
                    d = Rr[u] = {}
                d[k] = v

    def op(self, eng, fn, reads=(), writes=(), extra=()):
        writes = self._ps_as_writes(reads, writes)
        deps = self._deps(eng, reads, writes, extra)
        self._emit_waits(eng, deps)
        if self.cnt[eng] >= EPOCH:
            self.epoch[eng] += 1
            self.cnt[eng] = 0
            self.sem[(eng, self.epoch[eng])] = self.nc.alloc_semaphore(f"tl_{eng}_{self.epoch[eng]}")
        self.cnt[eng] += 1
        k = (eng, self.epoch[eng])
        sem = self.sem[k]
        self.q[eng].append(lambda E, fn=fn, sem=sem: fn(E).then_inc(sem, 1))
        tok = (k, self.cnt[eng])
        self._commit(tok, reads, writes)
        self.ninstr += 1
        return tok

    def dma(self, fn, reads=(), writes=(), extra=(), q='sp'):
        i = self.dnext[q]
        self.dnext[q] = (i + 1) % self.nslots
        k = ('d', q, i)
        if k not in self.sem:
            self.sem[k] = self.nc.alloc_semaphore(f"dma_{q}_{i}")
            self.dslot[k] = 0
        deps = self._deps(q, reads, writes, extra)
        if self.dslot[k] > 0:
            deps[k] = max(deps.get(k, 0), self.dslot[k])
        self._emit_waits(q, deps)
        self.dslot[k] += 16
        sem = self.sem[k]
        self.q[q].append(lambda E, fn=fn, sem=sem: fn(E).then_inc(sem, 16))
        tok = (k, self.dslot[k])
        self._commit(tok, reads, writes)
        self.ninstr += 1
        return tok

    def finish(self, q='sp'):
        for e in ENGS:
            deps = {k: v for k, v in self.dslot.items() if k[1] == e and v > 0}
            self._emit_waits(e, deps)

    def run(self):
        nc = self.nc
        with nc.Block() as block:
            @block.tensor
            def _(E):
                for f in self.q['pe']:
                    f(E)

            @block.scalar
            def _(E):
                for f in self.q['act']:
                    f(E)

            @block.vector
            def _(E):
                for f in self.q['dve']:
                    f(E)

            @block.gpsimd
            def _(E):
                for f in self.q['pool']:
                    f(E)

            @block.sync
            def _(E):
                for f in self.q['sp']:
                    f(E)


D = 2048
KT = 16
SBT = 512
TT = SBT // 128
NEXP = 32
ALPHA = float(2 ** 0.25)
EPS = 1e-5
OFF_QA, OFF_KA, OFF_VA, OFF_QG, OFF_KG, OFF_VG, OFF_RG, OFF_AL, OFF_GA, OFF_GG = (
    0, 1024, 1152, 1280, 2304, 3328, 5376, 7424, 7440, 9488)
NEG = -30000.0


class _Stop(Exception):
    pass


def build_nc(T, dbg=False, do_moe=True, stage=99):
    NSB = T // SBT
    nc = bass.Bass("TRN2", target_bir_lowering=False)

    def din(name, shape):
        return nc.dram_tensor(name, list(shape), F32, kind="ExternalInput").ap()

    x_d = din("x", (T, D))
    cfm_d = din("c_fm", (128, KT))
    wada_d = din("w_ada", (D, 6 * D))
    bada_d = din("b_ada", (6 * D,))
    win_d = din("w_in", (D, 11536))
    wup_d = din("w_gla_up", (16, 1024))
    bgate_d = din("b_gate_fm", (128, 8))
    sinks_d = din("attn_sinks", (16,))
    ggain_d = din("gla_gain", (D,))
    wpa_d = din("w_pa", (1024, D))
    wpg_d = din("w_pg", (D, D))
    wo_d = din("w_out", (D, D))
    ln1g_d, ln1b_d = din("ln1_gain", (D,)), din("ln1_bias", (D,))
    ln2g_d, ln2b_d = din("ln2_gain", (D,)), din("ln2_bias", (D,))
    wr_d = din("w_router", (D, NEXP))
    br_d = din("b_router", (NEXP,))
    if do_moe:
        wgu_d = din("w_gate_up", (NEXP, D, 2 * D))
        bgu_d = din("b_gu_fm", (128, NEXP * 16 * 2))
        wd_d = din("w_down", (NEXP, D, D))
        bd_d = din("b_down", (NEXP, D))
    ident_d = din("ident", (128, 128))
    swab_d = din("swa_bias", (128, 16 * 256))
    gmask_d = din("gla_mask", (64, 64))
    out_d = nc.dram_tensor("out", [T, D], F32, kind="ExternalOutput").ap()
    dbg_d = {}
    if dbg:
        for nm, w in (("d_h", D), ("d_yatt", 1024), ("d_ygla", D), ("d_x1", D), ("d_lg", 32 * 2)):
            dbg_d[nm] = nc.dram_tensor(nm, [T, w], F32, kind="ExternalOutput").ap()
    ada_row = nc.dram_tensor("ada_row", [6 * D], F32).ap()
    S_dram = nc.dram_tensor("S_dram", [4, 128, 1024], F32).ap()

    S = Sched(nc)
    xres = [S.sb(f"xres{t}", (128, D), F32) for t in range(TT)]
    hT = S.sb("hT", (128, KT, SBT), BF16)
    yattT = S.sb("yattT", (128, 8, SBT), BF16)
    yglaT = S.sb("yglaT", (128, KT, SBT), BF16)
    wst = [S.sb(f"wst{i}", (128, 4096), F32) for i in range(2)]
    wbf = [S.sb(f"wbf{i}", (128, 4096), BF16) for i in range(2)]
    bc = [S.sb(f"bc{i}", (128, D), F32) for i in range(2)]
    tmpA = S.sb("tmpA", (128, D), F32)
    ident = S.sb("ident", (128, 128), F32)
    silu_c = S.sb("silu_c", (128, KT), F32)
    sinks = S.sb("sinks", (128, 16), F32)
    nbgate = S.sb("nbgate", (128, 8), F32)
    wr_sb = S.sb("wr_sb", (128, KT, NEXP), F32)
    br_bc = S.sb("br_bc", (128, NEXP), F32)
    halo_k = [S.sb(f"halo_k{g}", (128, 128), BF16) for g in range(2)]
    halo_v = [S.sb(f"halo_v{g}", (128, 64), BF16) for g in range(2)]
    gmask = S.sb("gmask", (64, 64), F32)
    if do_moe:
        bgu = S.sb("bgu", (128, NEXP * 32), F32)
    NST = 6
    st_bn = [S.sb(f"st_bn{i}", (128, 4, 6), F32) for i in range(NST)]
    st_mv = [S.sb(f"st_mv{i}", (128, 2), F32) for i in range(NST)]
    st_a = [S.sb(f"st_a{i}", (128, 1), F32) for i in range(NST)]
    st_b = [S.sb(f"st_b{i}", (128, 1), F32) for i in range(NST)]
    st_c = [S.sb(f"st_c{i}", (128, 1), F32) for i in range(NST)]
    RA = ((S.sb_off + UNIT - 1) // UNIT) * UNIT
    PS = [S.ps(f"ps{i}") for i in range(8)]
    cnt = {'ps': 0, 'w': 0, 'st': 0, 'cast': 0, 'bc': 0}

    def nps():
        cnt['ps'] += 1
        return PS[cnt['ps'] % 8]

    def nst():
        cnt['st'] += 1
        return cnt['st'] % NST

    def MM(out, lhsT, rhs, start, stop, R, W):
        S.op('pe', lambda E: E.matmul(out, lhsT=lhsT, rhs=rhs, start=start, stop=stop), R, W)

    def TR(out, in_, R, W):
        n = in_.shape[0]
        S.op('pe', lambda E: E.transpose(out=out, in_=in_, identity=ident[0:n, 0:n]), list(R) + [ident], W)

    def ACT(out, in_, func, R, W, bias=0.0, scale=1.0, accum=None):
        S.op('act', lambda E: E.activation(out=out, in_=in_, func=func, bias=bias, scale=scale, accum_out=accum), R, W)

    def TS(eng, out, in0, s1, s2, op0, op1, R, W):
        if op1 is None:
            S.op(eng, lambda E: E.tensor_scalar(out=out, in0=in0, scalar1=s1, scalar2=None, op0=op0), R, W)
        else:
            S.op(eng, lambda E: E.tensor_scalar(out=out, in0=in0, scalar1=s1, scalar2=s2, op0=op0, op1=op1), R, W)

    def TTo(eng, out, in0, in1, op, R, W):
        S.op(eng, lambda E: E.tensor_tensor(out=out, in0=in0, in1=in1, op=op), R, W)

    def STT(eng, out, in0, scalar, in1, op0, op1, R, W):
        S.op(eng, lambda E: E.scalar_tensor_tensor(out=out, in0=in0, scalar=scalar, in1=in1, op0=op0, op1=op1), R, W)

    def CP(eng, out, in_, R, W):
        if eng == 'act':
            S.op('act', lambda E: E.copy(out=out, in_=in_), R, W)
        else:
            S.op(eng, lambda E: E.tensor_copy(out=out, in_=in_), R, W)

    def LD(out, in_, W, extra=(), q='sp'):
        return S.dma(lambda E: E.dma_start(out=out, in_=in_), (), W, extra, q=q)

    def ST(out, in_, R, extra=()):
        return S.dma(lambda E: E.dma_start(out=out, in_=in_), R, (), extra)

    CAST_ENGS = ['act', 'dve']
    WQ = ['sp', 'pool']

    def wload(srcs, kt, n, deint=False, ceng=None):
        cnt['w'] += 1
        i = cnt['w'] % 2
        sv = wst[i][:, 0:kt * n].rearrange("p (k n) -> p k n", k=kt)
        for (ap, c0, w) in srcs:
            LD(sv[:, :, c0:c0 + w], ap.rearrange("(k p) n -> p k n", p=128), [wst[i].rng(0, kt * n)], q=WQ[i])
        cnt['cast'] += 1
        eng = ceng or CAST_ENGS[cnt['cast'] % 2]
        if deint:
            ov = wbf[i][:, 0:kt * n].rearrange("p (k g f) -> p k g f", k=kt, g=2)
            iv = wst[i][:, 0:kt * n].rearrange("p (k f g) -> p k g f", k=kt, g=2)
            CP(eng, ov, iv, [wst[i].rng(0, kt * n)], [wbf[i].rng(0, kt * n)])
            return wbf[i], ov
        bv = wbf[i][:, 0:kt * n].rearrange("p (k n) -> p k n", k=kt)
        CP(eng, bv, sv, [wst[i].rng(0, kt * n)], [wbf[i].rng(0, kt * n)])
        return wbf[i], bv

    def bcload(src_ap, extra=(), plus1=False):
        cnt['bc'] += 1
        b = bc[cnt['bc'] % 2]
        LD(b[:], src_ap.partition_broadcast(128), [b], extra)
        if plus1:
            TS('pool', b[:], b[:], 1.0, None, ALU.add, None, [b], [b])
        return b

    def layernorm(src, dst_tile, dst_ap):
        i = nst()
        for c in range(4):
            S.op('dve', lambda E, c=c: E.bn_stats(out=st_bn[i][:, c, :], in_=src[:, c * 512:(c + 1) * 512]), [src], [st_bn[i]])
        S.op('dve', lambda E: E.bn_aggr(out=st_mv[i][:], in_=st_bn[i][:]), [st_bn[i]], [st_mv[i]])
        ACT(st_a[i][:], st_mv[i][:, 1:2], AF.Sqrt, [st_mv[i]], [st_a[i]], bias=EPS, scale=1.0)
        S.op('dve', lambda E: E.reciprocal(out=st_a[i][:], in_=st_a[i][:]), [st_a[i]], [st_a[i]])
        TS('dve', dst_ap, src[:], st_mv[i][:, 0:1], st_a[i][:, 0:1], ALU.subtract, ALU.mult, [src, st_mv[i], st_a[i]], [dst_tile])

    def transpose_tile(src, t, want_f32=None):
        for g in range(4):
            p = nps()
            for j in range(4):
                k = g * 4 + j
                TR(p[:, j * 128:(j + 1) * 128], src[:, k * 128:(k + 1) * 128], [src], [p])
            pv = p[:].rearrange("p (a b) -> p a b", a=4)
            CP('act', hT[:, g * 4:(g + 1) * 4, t * 128:(t + 1) * 128], pv, [p], [hT])
            if want_f32 is not None:
                CP('dve', want_f32[:, g * 4:(g + 1) * 4, :], pv, [p], [want_f32])

    LD(ident[:], ident_d, [ident])
    LD(silu_c[:], cfm_d, [silu_c])
    LD(sinks[:], sinks_d.partition_broadcast(128), [sinks])
    LD(nbgate[:], bgate_d, [nbgate])
    LD(wr_sb[:], wr_d.rearrange("(k p) n -> p k n", p=128), [wr_sb])
    LD(br_bc[:], br_d.partition_broadcast(128), [br_bc])
    LD(gmask[:], gmask_d, [gmask])
    if do_moe:
        LD(bgu[:], bgu_d, [bgu])
        bgu_up = bgu[:].rearrange("p (n g) -> p n g", g=2)[:, :, 1]
        TS('dve', bgu_up, bgu_up, 1.0, None, ALU.add, None, [bgu], [bgu])
    for g in range(2):
        S.op('pool', lambda E, g=g: E.memset(halo_k[g][:], 0.0), [], [halo_k[g]])
        S.op('pool', lambda E, g=g: E.memset(halo_v[g][:], 0.0), [], [halo_v[g]])
    TS('dve', nbgate[:], nbgate[:], -1.0, None, ALU.mult, None, [nbgate], [nbgate])
    ACT(silu_c[:], silu_c[:], AF.Silu, [silu_c], [silu_c])
    ada_tok = []
    arow = [S.sb(f"arow{i}", (1, 256), F32, off=RA + i * 1024) for i in range(2)]
    brow = [S.sb(f"brow{i}", (1, 256), F32, off=RA + 2048 + i * 1024) for i in range(2)]
    for gi in range(6 * D // 256):
        i = gi % 2
        c0 = gi * 256
        sv = wst[i][:].rearrange("p (k n) -> p k n", k=KT)
        LD(sv, wada_d[:, c0:c0 + 256].rearrange("(k p) n -> p k n", p=128), [wst[i]])
        LD(brow[i][:], bada_d[c0:c0 + 256].rearrange("(o n) -> o n", o=1), [brow[i]])
        p = nps()
        for k in range(KT):
            MM(p[0:1, 0:256], silu_c[:, k:k + 1], sv[:, k, :], k == 0, k == KT - 1, [silu_c, wst[i]], [p.rng(0, 256)])
        TTo('dve', arow[i][:], p[0:1, 0:256], brow[i][:], ALU.add, [p.rng(0, 256), brow[i]], [arow[i]])
        ada_tok.append(ST(ada_row[c0:c0 + 256].rearrange("(o n) -> o n", o=1), arow[i][:], [arow[i]]))

    def ada_bc(idx, plus1=False):
        return bcload(ada_row[idx * D:(idx + 1) * D], extra=ada_tok, plus1=plus1)

    S_tok = [None] * 4

    def chk(k):
        if stage <= k:
            raise _Stop()

    def sb_body(s):
        t0 = s * SBT
        sc1p = ada_bc(1, plus1=True)
        sh1 = ada_bc(0)
        for t in range(TT):
            LD(xres[t][:], x_d[t0 + t * 128:t0 + (t + 1) * 128, :], [xres[t]])
            layernorm(xres[t], tmpA, tmpA[:])
            TTo('pool', tmpA[:], tmpA[:], sc1p[:], ALU.mult, [tmpA, sc1p], [tmpA])
            TTo('dve', tmpA[:], tmpA[:], sh1[:], ALU.add, [tmpA, sh1], [tmpA])
            if dbg:
                ST(dbg_d["d_h"][t0 + t * 128:t0 + (t + 1) * 128, :], tmpA[:], [tmpA])
            transpose_tile(tmpA, t)

        chk(1)
        o = RA
        swab = S.sb(f"swab_{s}", (128, 16, 256), F32, off=o); o = _al(o + 16384)
        qT = S.sb(f"qT_{s}", (128, 4, SBT), BF16, off=o); o = _al(o + 4096)
        kT = S.sb(f"kT_{s}", (128, 128 + SBT), BF16, off=o); o = _al(o + 1280)
        vv = S.sb(f"vv_{s}", (128, 5, 64), BF16, off=o); o = _al(o + 640)
        s_sb = [S.sb(f"s_sb{i}_{s}", (128, 256), F32, off=o + i * 1024) for i in range(2)]; o = _al(o + 2048)
        p_sb = [S.sb(f"p_sb{i}_{s}", (128, 256), F32, off=o + i * 1024) for i in range(2)]; o = _al(o + 2048)
        pT = [S.sb(f"pT{i}_{s}", (128, 2, 128), BF16, off=o + i * 512) for i in range(2)]; o = _al(o + 1024)
        yatt = S.sb(f"yatt_{s}", (128, 4, 1024), F32, off=o); o = _al(o + 16384)
        LD(swab[:], swab_d.rearrange("p (h n) -> p h n", h=16), [swab])
        for g in range(2):
            wt, wv = wload([(win_d[:, OFF_KA + g * 64:OFF_KA + g * 64 + 64], 0, 64),
                            (win_d[:, OFF_KA + g * 64:OFF_KA + g * 64 + 64], 64, 64),
                            (win_d[:, OFF_VA + g * 64:OFF_VA + g * 64 + 64], 128, 64)], KT, 192)
            p = nps()
            for k in range(KT):
                MM(p[:, :], wv[:, k, 0:128], hT[:, k, :], k == 0, k == KT - 1, [wt, hT], [p])
            CP('dve', kT[:, 0:128], halo_k[g][:], [halo_k[g]], [kT.rng(0, 128)])
            CP('act', kT[:, 128:128 + SBT], p[:, :], [p], [kT.rng(128, 128 + SBT)])
            CP('pool', vv[:, 0, :], halo_v[g][:], [halo_v[g]], [vv.rng(0, 64)])
            for t in range(TT):
                p = nps()
                for k in range(KT):
                    MM(p[:, 0:64], hT[:, k, t * 128:(t + 1) * 128], wv[:, k, 128:192], k == 0, k == KT - 1, [wt, hT], [p.rng(0, 64)])
                CP('dve', vv[:, t + 1, :], p[:, 0:64], [p.rng(0, 64)], [vv.rng((t + 1) * 64, (t + 2) * 64)])
            CP('pool', halo_k[g][:], kT[:, SBT:SBT + 128], [kT], [halo_k[g]])
            CP('pool', halo_v[g][:], vv[:, 4, :], [vv], [halo_v[g]])
            for half in range(2):
                c0 = OFF_QA + g * 512 + half * 256
                wt, wv = wload([(win_d[:, c0:c0 + 256], 0, 256)], KT, 256)
                for jb in range(2):
                    p = nps()
                    for k in range(KT):
                        MM(p[:, :], wv[:, k, jb * 128:(jb + 1) * 128], hT[:, k, :], k == 0, k == KT - 1, [wt, hT], [p])
                    ACT(qT[:, half * 2 + jb, :], p[:, :], AF.Copy, [p], [qT], scale=0.125)
            for hl in range(8):
                h = g * 8 + hl
                jb, hf = hl // 2, hl % 2
                r0, r1 = hf * 64, hf * 64 + 64
                for n in range(TT):
                    first = (s == 0 and n == 0)
                    NS = 128 if first else 256
                    kc0 = 128 + n * 128 if first else n * 128
                    i2 = (hl * TT + n) % 2
                    ii = nst()
                    p = nps()
                    MM(p[:, 0:NS], qT[r0:r1, jb, n * 128:(n + 1) * 128], kT[r0:r1, kc0:kc0 + NS], True, True, [qT, kT], [p.rng(0, NS)])
                    TTo('dve', s_sb[i2][:, 0:NS], p[:, 0:NS], swab[:, h, 256 - NS:256], ALU.add, [p.rng(0, NS), swab], [s_sb[i2]])
                    S.op('dve', lambda E, i2=i2, NS=NS, ii=ii: E.tensor_reduce(out=st_a[ii][:], in_=s_sb[i2][:, 0:NS], axis=AX.X, op=ALU.max), [s_sb[i2]], [st_a[ii]])
                    TS('dve', st_a[ii][:], st_a[ii][:], sinks[:, h:h + 1], -1.0, ALU.max, ALU.mult, [st_a[ii], sinks], [st_a[ii]])
                    ACT(p_sb[i2][:, 0:NS], s_sb[i2][:, 0:NS], AF.Exp, [s_sb[i2], st_a[ii]], [p_sb[i2], st_b[ii]], bias=st_a[ii][:, 0:1], accum=st_b[ii][:, 0:1])
                    ACT(st_c[ii][:], sinks[:, h:h + 1], AF.Exp, [sinks, st_a[ii]], [st_c[ii]], bias=st_a[ii][:, 0:1])
                    TTo('dve', st_b[ii][:], st_b[ii][:], st_c[ii][:], ALU.add, [st_b[ii], st_c[ii]], [st_b[ii]])
                    S.op('dve', lambda E, ii=ii: E.reciprocal(out=st_b[ii][:], in_=st_b[ii][:]), [st_b[ii]], [st_b[ii]])
                    p2 = nps()
                    nsb = NS // 128
                    for sbk in range(nsb):
                        TR(p2[:, sbk * 128:(sbk + 1) * 128], p_sb[i2][:, sbk * 128:(sbk + 1) * 128], [p_sb[i2]], [p2.rng(0, NS)])
                    CP('act', pT[i2][:, 0:nsb, :], p2[:, 0:NS].rearrange("p (a b) -> p a b", a=nsb), [p2.rng(0, NS)], [pT[i2]])
                    p3 = nps()
                    for sbk in range(nsb):
                        vt = n + 1 if first else n + sbk
                        MM(p3[:, 0:64], pT[i2][:, sbk, :], vv[:, vt, :], sbk == 0, sbk == nsb - 1, [pT[i2], vv], [p3.rng(0, 64)])
                    TS('dve', yatt[:, n, h * 64:(h + 1) * 64], p3[:, 0:64], st_b[ii][:, 0:1], None, ALU.mult, None, [p3.rng(0, 64), st_b[ii]], [yatt.rng(n * 1024 + h * 64, n * 1024 + h * 64 + 64)])
        for n in range(TT):
            if dbg:
                ST(dbg_d["d_yatt"][t0 + n * 128:t0 + (n + 1) * 128, :], yatt[:, n, :], [yatt])
            for g2 in range(2):
                p = nps()
                for j in range(4):
                    k = g2 * 4 + j
                    TR(p[:, j * 128:(j + 1) * 128], yatt[:, n, k * 128:(k + 1) * 128], [yatt], [p])
                CP('act', yattT[:, g2 * 4:(g2 + 1) * 4, n * 128:(n + 1) * 128], p[:].rearrange("p (a b) -> p a b", a=4), [p], [yattT])

        chk(2)
        o = RA
        Ebuf = S.sb(f"Ebuf_{s}", (128, 2, SBT), F32, off=o); o = _al(o + 4096)
        L0 = S.sb(f"L0_{s}", (128, 2, SBT), F32, off=o); o = _al(o + 4096)
        L1 = S.sb(f"L1_{s}", (128, 2, SBT), F32, off=o); o = _al(o + 4096)
        qe = S.sb(f"qe_{s}", (128, 2, SBT), BF16, off=o); o = _al(o + 2048)
        ke = S.sb(f"ke_{s}", (128, 2, SBT), BF16, off=o); o = _al(o + 2048)
        kendT = S.sb(f"kendT_{s}", (128, 2, SBT), F32, off=o); o = _al(o + 4096)
        kend = S.sb(f"kend_{s}", (64, 8, 256), BF16, off=o); o = _al(o + 4096)
        vg = S.sb(f"vg_{s}", (64, 8, 512), BF16, off=o); o = _al(o + 8192)
        rs = S.sb(f"rs_{s}", (64, 8, 512), BF16, off=o); o = _al(o + 8192)
        Sst = S.sb(f"Sst_{s}", (128, 2, 512), F32, off=o); o = _al(o + 4096)
        Sb = S.sb(f"Sb_{s}", (128, 2, 512), BF16, off=o); o = _al(o + 2048)
        ytmp1 = S.sb(f"ytmp_{s}", (64, 512), F32, off=o); o = _al(o + 2048)
        ytmp = [ytmp1, ytmp1]
        gain_h = S.sb(f"gain_h_{s}", (64, 512), F32, off=o); o = _al(o + 2048)
        alrT = S.sb(f"alrT_{s}", (16, SBT), F32, off=o); o = _al(o + 2048)
        wup = S.sb(f"wup_{s}", (16, 256), F32, off=o); o = _al(o + 1024)
        AT = [S.sb(f"AT{i}_{s}", (64, 64), BF16, off=o + i * 128) for i in range(2)]; o = _al(o + 256)
        nll = S.sb(f"nll_{s}", (128, 2, 8), F32, off=o)
        dec = S.sb(f"dec_{s}", (128, 2, 8), F32, off=o + 64); o = _al(o + 128)
        assert o <= SB_END, (o, RA)
        wt, wv = wload([(win_d[:, OFF_AL:OFF_AL + 16], 0, 16)], KT, 16)
        p = nps()
        for k in range(KT):
            MM(p[0:16, :], wv[:, k, :], hT[:, k, :], k == 0, k == KT - 1, [wt, hT], [p])
        CP('act', alrT[:], p[0:16, :], [p], [alrT])
        for hd in range(4):
            LD(wup[:], wup_d[:, hd * 256:(hd + 1) * 256], [wup])
            for dc in range(2):
                p = nps()
                MM(p[:, :], wup[0:16, dc * 128:(dc + 1) * 128], alrT[0:16, :], True, True, [wup, alrT], [p])
                ACT(Ebuf[:, dc, :], p[:, :], AF.Exp, [p, nbgate], [Ebuf.rng(dc * SBT, (dc + 1) * SBT)], bias=nbgate[:, hd * 2 + dc:hd * 2 + dc + 1], scale=-1.0)
                ACT(L0[:, dc, :], Ebuf[:, dc, :], AF.Ln, [Ebuf.rng(dc * SBT, (dc + 1) * SBT)], [L0.rng(dc * SBT, (dc + 1) * SBT)], bias=1.0, scale=1.0)
            src, dst = L0, L1
            for sh in (1, 2, 4, 8, 16, 32):
                sv4 = src[:].rearrange("p d (c w) -> p d c w", w=64)
                dv4 = dst[:].rearrange("p d (c w) -> p d c w", w=64)
                TTo('dve', dv4[:, :, :, sh:64], sv4[:, :, :, sh:64], sv4[:, :, :, 0:64 - sh], ALU.add, [src], [dst])
                CP('pool', dv4[:, :, :, 0:sh], sv4[:, :, :, 0:sh], [src], [dst])
                src, dst = dst, src
            Lc = src
            Lc4 = Lc[:].rearrange("p d (c w) -> p d c w", w=64)
            TS('dve', nll[:], Lc4[:, :, :, 63], -1.0 / 16.0, None, ALU.mult, None, [Lc], [nll])
            ACT(dec[:], nll[:], AF.Exp, [nll], [dec])
            wt, wv = wload([(win_d[:, OFF_QG + hd * 256:OFF_QG + hd * 256 + 256], 0, 256)], KT, 256)
            for dc in range(2):
                p = nps()
                for k in range(KT):
                    MM(p[:, :], wv[:, k, dc * 128:(dc + 1) * 128], hT[:, k, :], k == 0, k == KT - 1, [wt, hT], [p])
                ACT(Ebuf[:, dc, :], Lc[:, dc, :], AF.Exp, [Lc], [Ebuf.rng(dc * SBT, (dc + 1) * SBT)], scale=-1.0 / 16.0)
                STT('dve', qe[:, dc, :], p[:, :], 1.0 / 16.0, Ebuf[:, dc, :], ALU.mult, ALU.mult, [p, Ebuf.rng(dc * SBT, (dc + 1) * SBT)], [qe.rng(dc * SBT, (dc + 1) * SBT)])
            wt, wv = wload([(win_d[:, OFF_KG + hd * 256:OFF_KG + hd * 256 + 256], 0, 256)], KT, 256)
            for dc in range(2):
                p = nps()
                for k in range(KT):
                    MM(p[:, :], wv[:, k, dc * 128:(dc + 1) * 128], hT[:, k, :], k == 0, k == KT - 1, [wt, hT], [p])
                er = Ebuf.rng(dc * SBT, (dc + 1) * SBT)
                ACT(Ebuf[:, dc, :], Lc[:, dc, :], AF.Exp, [Lc], [er], scale=1.0 / 16.0)
                TTo('dve', ke[:, dc, :], p[:, :], Ebuf[:, dc, :], ALU.mult, [p, er], [ke.rng(dc * SBT, (dc + 1) * SBT)])
                for c in range(8):
                    ACT(Ebuf[:, dc, c * 64:(c + 1) * 64], Lc[:, dc, c * 64:(c + 1) * 64], AF.Exp, [Lc, nll], [er], bias=nll[:, dc, c:c + 1], scale=1.0 / 16.0)
                TTo('dve', kendT[:, dc, :], p[:, :], Ebuf[:, dc, :], ALU.mult, [p, er], [kendT.rng(dc * SBT, (dc + 1) * SBT)])
            for c in range(8):
                p = nps()
                for dc in range(2):
                    TR(p[0:64, dc * 128:(dc + 1) * 128], kendT[:, dc, c * 64:(c + 1) * 64], [kendT], [p.rng(0, 256)])
                CP('act', kend[:, c, :], p[0:64, 0:256], [p.rng(0, 256)], [kend.rng(c * 256, (c + 1) * 256)])
            for (OFFX, dstt, is_r) in ((OFF_VG, vg, False), (OFF_RG, rs, True)):
                for half in range(2):
                    c0 = OFFX + hd * 512 + half * 256
                    wt, wv = wload([(win_d[:, c0:c0 + 256], 0, 256)], KT, 256)
                    for c in range(8):
                        p = nps()
                        for k in range(KT):
                            MM(p[0:64, 0:256], hT[:, k, c * 64:(c + 1) * 64], wv[:, k, :], k == 0, k == KT - 1, [wt, hT], [p.rng(0, 256)])
                        wr_ = dstt.rng(c * 512 + half * 256, c * 512 + half * 256 + 256)
                        if is_r:
                            ACT(dstt[:, c, half * 256:(half + 1) * 256], p[0:64, 0:256], AF.Silu, [p.rng(0, 256)], [wr_])
                        else:
                            CP('dve', dstt[:, c, half * 256:(half + 1) * 256], p[0:64, 0:256], [p.rng(0, 256)], [wr_])
            LD(gain_h[:], ggain_d[hd * 512:(hd + 1) * 512].partition_broadcast(64), [gain_h])
            if s == 0:
                S.op('pool', lambda E: E.memset(Sst[:], 0.0), [], [Sst])
            else:
                LD(Sst[:], S_dram[hd].rearrange("p (d v) -> p d v", d=2), [Sst], extra=[S_tok[hd]])
            CP('act', Sb[:], Sst[:], [Sst], [Sb])
            for c in range(8):
                cs = slice(c * 64, (c + 1) * 64)
                i2 = c % 2
                ii = nst()
                pa = nps()
                for dc in range(2):
                    MM(pa[0:64, 0:64], ke[:, dc, cs], qe[:, dc, cs], dc == 0, dc == 1, [ke, qe], [pa.rng(0, 64)])
                TTo('dve', AT[i2][:], pa[0:64, 0:64], gmask[:], ALU.mult, [pa.rng(0, 64), gmask], [AT[i2]])
                po = nps()
                MM(po[0:64, :], AT[i2][:], vg[:, c, :], True, False, [AT[i2], vg], [po])
                MM(po[0:64, :], qe[:, 0, cs], Sb[:, 0, :], False, False, [qe, Sb], [po])
                MM(po[0:64, :], qe[:, 1, cs], Sb[:, 1, :], False, True, [qe, Sb], [po])
                for dc in range(2):
                    pS = nps()
                    MM(pS[:, :], kend[:, c, dc * 128:(dc + 1) * 128], vg[:, c, :], True, True, [kend, vg], [pS])
                    sr = Sst.rng(dc * 512, (dc + 1) * 512)
                    STT('dve', Sst[:, dc, :], Sst[:, dc, :], dec[:, dc, c:c + 1], pS[:, :], ALU.mult, ALU.add, [sr, dec, pS], [sr])
                    CP('pool', Sb[:, dc, :], Sst[:, dc, :], [sr], [Sb.rng(dc * 512, (dc + 1) * 512)])
                ACT(ytmp[i2][:], po[0:64, :], AF.Square, [po], [ytmp[i2], st_a[ii]], accum=st_a[ii][0:64, 0:1])
                ACT(st_a[ii][0:64, :], st_a[ii][0:64, :], AF.Sqrt, [st_a[ii]], [st_a[ii]], bias=EPS, scale=1.0 / 512.0)
                S.op('dve', lambda E, ii=ii: E.reciprocal(out=st_a[ii][0:64, :], in_=st_a[ii][0:64, :]), [st_a[ii]], [st_a[ii]])
                STT('dve', ytmp[i2][:], po[0:64, :], st_a[ii][0:64, 0:1], gain_h[:], ALU.mult, ALU.mult, [po, st_a[ii], gain_h], [ytmp[i2]])
                TTo('pool', ytmp[i2][:], ytmp[i2][:], rs[:, c, :], ALU.mult, [ytmp[i2], rs], [ytmp[i2]])
                if dbg:
                    ST(dbg_d["d_ygla"][t0 + c * 64:t0 + (c + 1) * 64, hd * 512:(hd + 1) * 512], ytmp[i2][:], [ytmp[i2]])
                pt = nps()
                for k4 in range(4):
                    TR(pt[:, k4 * 64:(k4 + 1) * 64], ytmp[i2][:, k4 * 128:(k4 + 1) * 128], [ytmp[i2]], [pt.rng(0, 256)])
                CP('act', yglaT[:, hd * 4:(hd + 1) * 4, cs], pt[:, 0:256].rearrange("p (a b) -> p a b", a=4), [pt.rng(0, 256)], [yglaT])
            S_tok[hd] = ST(S_dram[hd].rearrange("p (d v) -> p d v", d=2), Sst[:], [Sst])

        chk(3)
        o = RA
        G = S.sb(f"G_{s}", (128, TT, NEXP), F32, off=o); o = _al(o + 512)
        GT = S.sb(f"GT_{s}", (32, SBT), F32, off=o); o = _al(o + 2048)
        G17 = S.sb(f"G17_{s}", (128, TT, NEXP), F32, off=o); o = _al(o + 512)
        P5O2 = o
        mrgT = S.sb(f"mrgT_{s}", (128, KT, SBT), BF16, off=o); o = _al(o + 16384)
        sga = S.sb(f"sga_{s}", (128, SBT), F32, off=o); o = _al(o + 2048)
        sgg = S.sb(f"sgg_{s}", (128, SBT), F32, off=o); o = _al(o + 2048)
        mt = [S.sb(f"mt{i}_{s}", (128, SBT), F32, off=o + i * 2048) for i in range(2)]; o = _al(o + 4096)
        hTf = S.sb(f"hTf_{s}", (128, KT, 128), F32, off=o); o = _al(o + 8192)
        lg = S.sb(f"lg_{s}", (128, NEXP), F32, off=o); o = _al(o + 128)
        ex = S.sb(f"ex_{s}", (128, NEXP), F32, off=o); o = _al(o + 128)
        m8 = S.sb(f"m8_{s}", (128, 8), F32, off=o); o = _al(o + 64)
        assert o <= SB_END, (o, RA)
        for j in range(KT):
            cs = slice(j * 128, (j + 1) * 128)
            wt, wv = wload([(wpa_d[:, cs], 0, 128)], 8, 128)
            ppa = nps()
            for k in range(8):
                MM(ppa[:, :], wv[:, k, :], yattT[:, k, :], k == 0, k == 7, [wt, yattT], [ppa])
            wt, wv = wload([(wpg_d[:, cs], 0, 128)], KT, 128)
            ppg = nps()
            for k in range(KT):
                MM(ppg[:, :], wv[:, k, :], yglaT[:, k, :], k == 0, k == KT - 1, [wt, yglaT], [ppg])
            wt, wv = wload([(win_d[:, OFF_GA + j * 128:OFF_GA + (j + 1) * 128], 0, 128),
                            (win_d[:, OFF_GG + j * 128:OFF_GG + (j + 1) * 128], 128, 128)], KT, 256)
            pga = nps()
            for k in range(KT):
                MM(pga[:, :], wv[:, k, 0:128], hT[:, k, :], k == 0, k == KT - 1, [wt, hT], [pga])
            pgg = nps()
            for k in range(KT):
                MM(pgg[:, :], wv[:, k, 128:256], hT[:, k, :], k == 0, k == KT - 1, [wt, hT], [pgg])
            ACT(sga[:], pga[:, :], AF.Sigmoid, [pga], [sga])
            ACT(sgg[:], pgg[:, :], AF.Sigmoid, [pgg], [sgg])
            TTo('dve', mt[0][:], ppa[:, :], sga[:], ALU.mult, [ppa, sga], [mt[0]])
            TTo('dve', mt[1][:], ppg[:, :], sgg[:], ALU.mult, [ppg, sgg], [mt[1]])
            TTo('pool', mrgT[:, j, :], mt[0][:], mt[1][:], ALU.add, [mt[0], mt[1]], [mrgT.rng(j * SBT, (j + 1) * SBT)])
        chk(3.2)
        gt1 = ada_bc(2)
        for c8 in range(8):
            cs = slice(c8 * 256, (c8 + 1) * 256)
            wt, wv = wload([(wo_d[:, cs], 0, 256)], KT, 256)
            for t in range(TT):
                p = nps()
                for k in range(KT):
                    MM(p[:, 0:256], mrgT[:, k, t * 128:(t + 1) * 128], wv[:, k, :], k == 0, k == KT - 1, [wt, mrgT], [p.rng(0, 256)])
                i2 = (c8 * TT + t) % 2
                xr = xres[t].rng(c8 * 256, (c8 + 1) * 256)
                TTo('dve', mt[i2][:, 0:256], p[:, 0:256], gt1[:, cs], ALU.mult, [p.rng(0, 256), gt1], [mt[i2]])
                STT('dve', xres[t][:, cs], xres[t][:, cs], ALPHA, mt[i2][:, 0:256], ALU.mult, ALU.add, [xr, mt[i2]], [xr])
        g1 = bcload(ln1g_d)
        for t in range(TT):
            layernorm(xres[t], xres[t], xres[t][:])
            TTo('pool', xres[t][:], xres[t][:], g1[:], ALU.mult, [xres[t], g1], [xres[t]])
        b1 = bcload(ln1b_d)
        for t in range(TT):
            TTo('dve', xres[t][:], xres[t][:], b1[:], ALU.add, [xres[t], b1], [xres[t]])
            if dbg:
                ST(dbg_d["d_x1"][t0 + t * 128:t0 + (t + 1) * 128, :], xres[t][:], [xres[t]])
        chk(3.5)
        sc2p = ada_bc(4, plus1=True)
        sh2 = ada_bc(3)
        for t in range(TT):
            layernorm(xres[t], tmpA, tmpA[:])
            TTo('pool', tmpA[:], tmpA[:], sc2p[:], ALU.mult, [tmpA, sc2p], [tmpA])
            TTo('dve', tmpA[:], tmpA[:], sh2[:], ALU.add, [tmpA, sh2], [tmpA])
            transpose_tile(tmpA, t, want_f32=hTf)
            p = nps()
            for k in range(KT):
                MM(p[:, 0:NEXP], hTf[:, k, :], wr_sb[:, k, :], k == 0, k == KT - 1, [hTf, wr_sb], [p.rng(0, NEXP)])
            ii = nst()
            TTo('dve', lg[:], p[:, 0:NEXP], br_bc[:], ALU.add, [p.rng(0, NEXP), br_bc], [lg])
            S.op('dve', lambda E: E.max(out=m8[:], in_=lg[:]), [lg], [m8])
            TS('dve', st_a[ii][:], m8[:, 0:1], -1.0, None, ALU.mult, None, [m8], [st_a[ii]])
            ACT(ex[:], lg[:], AF.Exp, [lg, st_a[ii]], [ex], bias=st_a[ii][:, 0:1])
            TS('dve', lg[:], lg[:], m8[:, 3:4], None, ALU.is_ge, None, [lg, m8], [lg])
            TTo('dve', ex[:], ex[:], lg[:], ALU.mult, [ex, lg], [ex])
            S.op('dve', lambda E, ii=ii: E.tensor_reduce(out=st_b[ii][:], in_=ex[:], axis=AX.X, op=ALU.add), [ex], [st_b[ii]])
            S.op('dve', lambda E, ii=ii: E.reciprocal(out=st_b[ii][:], in_=st_b[ii][:]), [st_b[ii]], [st_b[ii]])
            TS('dve', G[:, t, :], ex[:], st_b[ii][:, 0:1], None, ALU.mult, None, [ex, st_b[ii]], [G.rng(t * NEXP, (t + 1) * NEXP)])
            if dbg:
                ST(dbg_d["d_lg"][t0 + t * 128:t0 + (t + 1) * 128, 0:32], G[:, t, :], [G])
            TS('dve', G17[:, t, :], G[:, t, :], 1.0 / 1.702, None, ALU.mult, None, [G], [G17.rng(t * NEXP, (t + 1) * NEXP)])
            p = nps()
            TR(p[0:32, 0:128], G[:, t, :], [G], [p.rng(0, 128)])
            CP('act', GT[:, t * 128:(t + 1) * 128], p[0:32, 0:128], [p.rng(0, 128)], [GT.rng(t * 128, (t + 1) * 128)])

        chk(4)
        o = P5O2
        accf = [S.sb(f"accf{t}_{s}", (128, D), F32, off=o + t * 8192) for t in range(TT)]; o = _al(o + 4 * 8192)
        actT = S.sb(f"actT_{s}", (128, KT, SBT), BF16, off=o); o = _al(o + 16384)
        et = [S.sb(f"et{i}_{s}", (128, SBT), F32, off=tmpA.addr + i * 2048) for i in range(3)]
        assert o <= SB_END, o
        if do_moe:
            bdt = bc[(cnt['bc'] + 1) % 2]
            cnt['bc'] += 1
            LD(bdt[0:32, :], bd_d, [bdt])
            for t in range(TT):
                for c4 in range(4):
                    cs = slice(c4 * 512, (c4 + 1) * 512)
                    p = nps()
                    MM(p[:, :], GT[0:32, t * 128:(t + 1) * 128], bdt[0:32, cs], True, True, [GT, bdt], [p])
                    CP('act' if c4 % 2 else 'dve', accf[t][:, cs], p[:, :], [p], [accf[t].rng(c4 * 512, (c4 + 1) * 512)])
            for e in range(NEXP):
                for j in range(KT):
                    wt, wv = wload([(wgu_d[e][:, j * 256:(j + 1) * 256], 0, 256)], KT, 256, deint=True, ceng='act')
                    pg = nps()
                    for k in range(KT):
                        MM(pg[:, :], wv[:, k, 0, :], hT[:, k, :], k == 0, k == KT - 1, [wt, hT], [pg])
                    pu = nps()
                    for k in range(KT):
                        MM(pu[:, :], wv[:, k, 1, :], hT[:, k, :], k == 0, k == KT - 1, [wt, hT], [pu])
                    bcol = (e * 16 + j) * 2
                    TS('dve', et[0][:], pg[:, :], bgu[:, bcol:bcol + 1], 7.0, ALU.add, ALU.min, [pg, bgu], [et[0]])
                    ACT(et[1][:], et[0][:], AF.Silu, [et[0]], [et[1]], scale=1.702)
                    TS('dve', et[2][:], pu[:, :], bgu[:, bcol + 1:bcol + 2], 8.0, ALU.add, ALU.min, [pu, bgu], [et[2]])
                    STT('dve', actT[:, j, :], et[2][:], -6.0, et[1][:], ALU.max, ALU.mult, [et[2], et[1]], [actT.rng(j * SBT, (j + 1) * SBT)])
                for c8 in range(8):
                    cs = slice(c8 * 256, (c8 + 1) * 256)
                    wt, wv = wload([(wd_d[e][:, cs], 0, 256)], KT, 256, ceng='dve')
                    for t in range(TT):
                        p = nps()
                        for k in range(KT):
                            MM(p[:, 0:256], actT[:, k, t * 128:(t + 1) * 128], wv[:, k, :], k == 0, k == KT - 1, [wt, actT], [p.rng(0, 256)])
                        ar = accf[t].rng(c8 * 256, (c8 + 1) * 256)
                        STT('dve', accf[t][:, cs], p[:, 0:256], G17[:, t, e:e + 1], accf[t][:, cs], ALU.mult, ALU.add, [p.rng(0, 256), G17, ar], [ar])
        else:
            for t in range(TT):
                S.op('pool', lambda E, t=t: E.memset(accf[t][:], 0.0), [], [accf[t]])

        gt2 = ada_bc(5)
        for t in range(TT):
            TTo('pool', accf[t][:], accf[t][:], gt2[:], ALU.mult, [accf[t], gt2], [accf[t]])
            STT('dve', xres[t][:], xres[t][:], ALPHA, accf[t][:], ALU.mult, ALU.add, [xres[t], accf[t]], [xres[t]])
        g2 = bcload(ln2g_d)
        for t in range(TT):
            layernorm(xres[t], xres[t], xres[t][:])
            TTo('pool', xres[t][:], xres[t][:], g2[:], ALU.mult, [xres[t], g2], [xres[t]])
        b2 = bcload(ln2b_d)
        for t in range(TT):
            TTo('dve', xres[t][:], xres[t][:], b2[:], ALU.add, [xres[t], b2], [xres[t]])
            ST(out_d[t0 + t * 128:t0 + (t + 1) * 128, :], xres[t][:], [xres[t]])

    try:
        for s in range(NSB if stage > 0 else 0):
            sb_body(s)
    except _Stop:
        pass
    S.finish()
    S.run()
    return nc, S


def _consts():
    ident = np.eye(128, dtype=np.float32)
    q = np.arange(128)[:, None]
    s_ = np.arange(256)[None, :]
    dist = (q + 128 - s_).astype(np.float32)
    valid = (dist >= 0) & (dist < 128)
    slopes = (2.0 ** (-8.0 * (np.arange(16, dtype=np.float32) + 1.0) / 16)).astype(np.float32)
    swab = np.where(valid[:, None, :], -slopes[None, :, None] * dist[:, None, :], np.float32(NEG)).astype(np.float32)
    gmask = (np.arange(64)[:, None] <= np.arange(64)[None, :]).astype(np.float32)
    return ident, np.ascontiguousarray(swab.reshape(128, 16 * 256)), gmask


def make_in_maps(inputs, batches, T, do_moe=True):
    f = lambda a: np.ascontiguousarray(np.asarray(a, dtype=np.float32))
    ident, swab, gmask = _consts()
    shared = dict(
        w_ada=f(inputs["w_ada"][0]), b_ada=f(inputs["b_ada"][0]), w_in=f(inputs["w_in"][0]),
        w_gla_up=f(inputs["w_gla_gate_up"][0]),
        b_gate_fm=f(np.asarray(inputs["b_gla_gate"][0]).reshape(8, 128).T),
        attn_sinks=f(inputs["attn_sinks"][0]), gla_gain=f(inputs["gla_norm_gain"][0]),
        w_pa=f(inputs["w_branch_att"][0]), w_pg=f(inputs["w_branch_gla"][0]), w_out=f(inputs["w_out"][0]),
        ln1_gain=f(inputs["ln1_gain"][0]), ln1_bias=f(inputs["ln1_bias"][0]),
        ln2_gain=f(inputs["ln2_gain"][0]), ln2_bias=f(inputs["ln2_bias"][0]),
        w_router=f(inputs["w_router"][0]), b_router=f(inputs["b_router"][0]),
        ident=ident, swa_bias=swab, gla_mask=gmask)
    if do_moe:
        bgu = np.asarray(inputs["b_gate_up"][0]).reshape(NEXP, 16, 128, 2)
        shared.update(
            w_gate_up=f(inputs["w_gate_up"][0]), w_down=f(inputs["w_down"][0]), b_down=f(inputs["b_down"][0]),
            b_gu_fm=f(bgu.transpose(2, 0, 1, 3).reshape(128, NEXP * 32)))
    maps = []
    for b in batches:
        m = dict(shared)
        m["x"] = f(inputs["x"][b, :T])
        m["c_fm"] = f(np.asarray(inputs["c"][b]).reshape(KT, 128).T)
        maps.append(m)
    return maps


N_CORES = 4
SEQ = 4096


def kernel(**inputs):
    nc, _ = build_nc(SEQ)
    maps = make_in_maps(inputs, list(range(N_CORES)), SEQ)
    res = run_bass_kernel_spmd(nc, maps, core_ids=list(range(N_CORES)))
    return np.stack([np.asarray(r["out"], dtype=np.float32) for r in res.results], axis=0)
```

```python
import numpy as np
import concourse.bass as bass
import concourse.mybir as mybir
from concourse.bass_utils import run_bass_kernel_spmd

F32 = mybir.dt.float32
BF16 = mybir.dt.bfloat16
AF = mybir.ActivationFunctionType
ALU = mybir.AluOpType
AX = mybir.AxisListType
ISZ = {F32: 4, BF16: 2}

UNIT = 256
SB_BASE = 16512
SB_END = 229376
EPOCH = 30000
ENGS = ['pe', 'act', 'dve', 'pool', 'sp']


class Tile:
    def __init__(self, h, space, addr, shape, dtype):
        self.h, self.space, self.addr, self.shape, self.dtype = h, space, addr, shape, dtype
        self.isz = ISZ[dtype]
        n = 1
        for s in shape[1:]:
            n *= s
        self.nbytes = n * self.isz

    def __getitem__(self, k):
        return self.h[k]

    def rng(self, lo=None, hi=None):
        lo = 0 if lo is None else lo * self.isz
        hi = self.nbytes if hi is None else hi * self.isz
        return (self.space, self.addr + lo, self.addr + hi)


def _al(v):
    return ((v + UNIT - 1) // UNIT) * UNIT


def _rng(x):
    return x.rng() if isinstance(x, Tile) else x


class Sched:
    def __init__(self, nc, n_dma_slots=24):
        self.nc = nc
        self.eng = {'pe': nc.tensor, 'act': nc.scalar, 'dve': nc.vector, 'pool': nc.gpsimd, 'sp': nc.sync}
        self.q = {e: [] for e in ENGS}
        self.sem = {}
        self.epoch = {e: 0 for e in ENGS}
        self.cnt = {e: 0 for e in ENGS}
        for e in ENGS:
            self.sem[(e, 0)] = nc.alloc_semaphore(f"tl_{e}_0")
        self.known = {e: {} for e in ENGS}
        self.mem_w = {'sb': {}, 'ps': {}}
        self.mem_r = {'sb': {}, 'ps': {}}
        self.sb_off = SB_BASE
        self.ps_n = 0
        self.nslots = n_dma_slots
        self.dslot = {}
        self.dnext = {e: 0 for e in ENGS}
        self.ninstr = 0

    def sb(self, name, shape, dtype, off=None):
        t = Tile(None, 'sb', 0, shape, dtype)
        if off is None:
            al = UNIT if t.nbytes >= UNIT else 64
            off = ((self.sb_off + al - 1) // al) * al
            self.sb_off = off + ((t.nbytes + 63) // 64) * 64
        t.addr = off
        assert off >= SB_BASE and off + t.nbytes <= SB_END, (name, off, t.nbytes)
        t.h = self.nc.alloc_sbuf_tensor_at(name, list(shape), dtype, offset=off)
        return t

    def ps(self, name, shape=(128, 512), dtype=F32):
        t = Tile(None, 'ps', self.ps_n * 2048, shape, dtype)
        assert t.nbytes <= 2048
        self.ps_n += 1
        t.h = self.nc.alloc_psum_tensor(name, list(shape), dtype)
        return t

    def _units(self, r):
        sp, lo, hi = r
        if sp == 'ps':
            return sp, range((lo // 2048) * (2048 // UNIT), ((hi - 1) // 2048 + 1) * (2048 // UNIT))
        return sp, range(lo // UNIT, (hi - 1) // UNIT + 1)

    @staticmethod
    def _ps_as_writes(reads, writes):
        extra = [r for r in reads if _rng(r)[0] == 'ps']
        return (list(writes) + extra) if extra else writes

    def _deps(self, eng, reads, writes, extra):
        deps = {}

        def add(tok):
            if tok is None:
                return
            k, v = tok
            if eng == 'pe' and k[0] == 'pe':
                return
            if deps.get(k, 0) < v:
                deps[k] = v
        for r in reads:
            sp, us = self._units(_rng(r))
            W = self.mem_w[sp]
            for u in us:
                add(W.get(u))
        for r in writes:
            sp, us = self._units(_rng(r))
            W, Rr = self.mem_w[sp], self.mem_r[sp]
            for u in us:
                add(W.get(u))
                d = Rr.get(u)
                if d:
                    for k, v in d.items():
                        add((k, v))
        for tok in extra:
            add(tok)
        return deps

    def _emit_waits(self, eng, deps):
        kn = self.known[eng]
        for k, v in deps.items():
            if kn.get(k, 0) >= v:
                continue
            kn[k] = v
            sem = self.sem[k]
            self.q[eng].append(lambda E, sem=sem, v=v: E.wait_ge(sem, v))

    def _commit(self, tok, reads, writes):
        for r in writes:
            sp, us = self._units(_rng(r))
            W, Rr = self.mem_w[sp], self.mem_r[sp]
            for u in us:
                W[u] = tok
                Rr[u] = {}
        k, v = tok
        for r in reads:
            sp, us = self._units(_rng(r))
            Rr = self.mem_r[sp]
            for u in us:
                d = Rr.get(u)
                if d is None:
                    d = Rr[u] = {}
                d[k] = v

    def op(self, eng, fn, reads=(), writes=(), extra=()):
        writes = self._ps_as_writes(reads, writes)
        deps = self._deps(eng, reads, writes, extra)
        self._emit_waits(eng, deps)
        if self.cnt[eng] >= EPOCH:
            self.epoch[eng] += 1
            self.cnt[eng] = 0
            self.sem[(eng, self.epoch[eng])] = self.nc.alloc_semaphore(f"tl_{eng}_{self.epoch[eng]}")
        self.cnt[eng] += 1
        k = (eng, self.epoch[eng])
        sem = self.sem[k]
        self.q[eng].append(lambda E, fn=fn, sem=sem: fn(E).then_inc(sem, 1))
        tok = (k, self.cnt[eng])
        self._commit(tok, reads, writes)
        self.ninstr += 1
        return tok

    def dma(self, fn, reads=(), writes=(), extra=(), q='sp'):
        i = self.dnext[q]
        self.dnext[q] = (i + 1) % self.nslots
        k = ('d', q, i)
        if k not in self.sem:
            self.sem[k] = self.nc.alloc_semaphore(f"dma_{q}_{i}")
            self.dslot[k] = 0
        deps = self._deps(q, reads, writes, extra)
        if self.dslot[k] > 0:
            deps[k] = max(deps.get(k, 0), self.dslot[k])
        self._emit_waits(q, deps)
        self.dslot[k] += 16
        sem = self.sem[k]
        self.q[q].append(lambda E, fn=fn, sem=sem: fn(E).then_inc(sem, 16))
        tok = (k, self.dslot[k])
        self._commit(tok, reads, writes)
        self.ninstr += 1
        return tok

    def finish(self, q='sp'):
        for e in ENGS:
            deps = {k: v for k, v in self.dslot.items() if k[1] == e and v > 0}
            self._emit_waits(e, deps)

    def run(self):
        nc = self.nc
        with nc.Block() as block:
            @block.tensor
            def _(E):
                for f in self.q['pe']:
                    f(E)

            @block.scalar
            def _(E):
                for f in self.q['act']:
                    f(E)

            @block.vector
            def _(E):
                for f in self.q['dve']:
                    f(E)

            @block.gpsimd
            def _(E):
                for f in self.q['pool']:
                    f(E)

            @block.sync
            def _(E):
                for f in self.q['sp']:
                    f(E)


D = 2048
KT = 16
SBT = 512
TT = SBT // 128
NEXP = 32
ALPHA = float(2 ** 0.25)
EPS = 1e-5
OFF_QA, OFF_KA, OFF_VA, OFF_QG, OFF_KG, OFF_VG, OFF_RG, OFF_AL, OFF_GA, OFF_GG = (
    0, 1024, 1152, 1280, 2304, 3328, 5376, 7424, 7440, 9488)
NEG = -30000.0


class _Stop(Exception):
    pass


def build_nc(T, dbg=False, do_moe=True, stage=99, TP=0):
    NSB = T // SBT
    NSBP = TP // SBT
    nc = bass.Bass("TRN2", target_bir_lowering=False)

    def din(name, shape):
        return nc.dram_tensor(name, list(shape), F32, kind="ExternalInput").ap()

    x_d = din("x", (T, D))
    cfm_d = din("c_fm", (128, KT))
    wada_d = din("w_ada", (D, 6 * D))
    bada_d = din("b_ada", (6 * D,))
    win_d = din("w_in", (D, 11536))
    wup_d = din("w_gla_up", (16, 1024))
    bgate_d = din("b_gate_fm", (128, 8))
    sinks_d = din("attn_sinks", (16,))
    ggain_d = din("gla_gain", (D,))
    wpa_d = din("w_pa", (1024, D))
    wpg_d = din("w_pg", (D, D))
    wo_d = din("w_out", (D, D))
    ln1g_d, ln1b_d = din("ln1_gain", (D,)), din("ln1_bias", (D,))
    ln2g_d, ln2b_d = din("ln2_gain", (D,)), din("ln2_bias", (D,))
    wr_d = din("w_router", (D, NEXP))
    br_d = din("b_router", (NEXP,))
    if do_moe:
        wgu_d = din("w_gate_up", (NEXP, D, 2 * D))
        bgu_d = din("b_gu_fm", (128, NEXP * 16 * 2))
        wd_d = din("w_down", (NEXP, D, D))
        bd_d = din("b_down", (NEXP, D))
    ident_d = din("ident", (128, 128))
    swab_d = din("swa_bias", (128, 16 * 256))
    gmask_d = din("gla_mask", (64, 64))
    if TP:
        xprev_d = din("xprev", (TP, D))
        flag_d = din("flag", (128, 1))
        swab0_d = din("swa_bias0", (128, 16 * 256))
    out_d = nc.dram_tensor("out", [T, D], F32, kind="ExternalOutput").ap()
    dbg_d = {}
    if dbg:
        for nm, w in (("d_h", D), ("d_yatt", 1024), ("d_ygla", D), ("d_x1", D), ("d_lg", 32 * 2)):
            dbg_d[nm] = nc.dram_tensor(nm, [T, w], F32, kind="ExternalOutput").ap()
    ada_row = nc.dram_tensor("ada_row", [6 * D], F32).ap()
    S_dram = nc.dram_tensor("S_dram", [4, 128, 1024], F32).ap()

    S = Sched(nc)
    xres = [S.sb(f"xres{t}", (128, D), F32) for t in range(TT)]
    hT = S.sb("hT", (128, KT, SBT), BF16)
    yattT = S.sb("yattT", (128, 8, SBT), BF16)
    yglaT = S.sb("yglaT", (128, KT, SBT), BF16)
    wst = [S.sb(f"wst{i}", (128, 4096), F32) for i in range(2)]
    wbf = [S.sb(f"wbf{i}", (128, 4096), BF16) for i in range(2)]
    bc = [S.sb(f"bc{i}", (128, D), F32) for i in range(2)]
    tmpA = S.sb("tmpA", (128, D), F32)
    ident = S.sb("ident", (128, 128), F32)
    silu_c = S.sb("silu_c", (128, KT), F32)
    sinks = S.sb("sinks", (128, 16), F32)
    nbgate = S.sb("nbgate", (128, 8), F32)
    wr_sb = S.sb("wr_sb", (128, KT, NEXP), F32)
    br_bc = S.sb("br_bc", (128, NEXP), F32)
    halo_k = [S.sb(f"halo_k{g}", (128, 128), BF16) for g in range(2)]
    halo_v = [S.sb(f"halo_v{g}", (128, 64), BF16) for g in range(2)]
    gmask = S.sb("gmask", (64, 64), F32)
    flag = S.sb("flag", (128, 1), F32)
    if do_moe:
        bgu = S.sb("bgu", (128, NEXP * 32), F32)
    NST = 5
    st_bn = [S.sb(f"st_bn{i}", (128, 4, 6), F32) for i in range(NST)]
    st_mv = [S.sb(f"st_mv{i}", (128, 2), F32) for i in range(NST)]
    st_a = [S.sb(f"st_a{i}", (128, 1), F32) for i in range(NST)]
    st_b = [S.sb(f"st_b{i}", (128, 1), F32) for i in range(NST)]
    st_c = [S.sb(f"st_c{i}", (128, 1), F32) for i in range(NST)]
    RA = ((S.sb_off + UNIT - 1) // UNIT) * UNIT
    PS = [S.ps(f"ps{i}") for i in range(8)]
    cnt = {'ps': 0, 'w': 0, 'st': 0, 'cast': 0, 'bc': 0}

    def nps():
        cnt['ps'] += 1
        return PS[cnt['ps'] % 8]

    def nst():
        cnt['st'] += 1
        return cnt['st'] % NST

    def MM(out, lhsT, rhs, start, stop, R, W):
        S.op('pe', lambda E: E.matmul(out, lhsT=lhsT, rhs=rhs, start=start, stop=stop), R, W)

    def TR(out, in_, R, W):
        n = in_.shape[0]
        S.op('pe', lambda E: E.transpose(out=out, in_=in_, identity=ident[0:n, 0:n]), list(R) + [ident], W)

    def ACT(out, in_, func, R, W, bias=0.0, scale=1.0, accum=None):
        S.op('act', lambda E: E.activation(out=out, in_=in_, func=func, bias=bias, scale=scale, accum_out=accum), R, W)

    def TS(eng, out, in0, s1, s2, op0, op1, R, W):
        if op1 is None:
            S.op(eng, lambda E: E.tensor_scalar(out=out, in0=in0, scalar1=s1, scalar2=None, op0=op0), R, W)
        else:
            S.op(eng, lambda E: E.tensor_scalar(out=out, in0=in0, scalar1=s1, scalar2=s2, op0=op0, op1=op1), R, W)

    def TTo(eng, out, in0, in1, op, R, W):
        S.op(eng, lambda E: E.tensor_tensor(out=out, in0=in0, in1=in1, op=op), R, W)

    def STT(eng, out, in0, scalar, in1, op0, op1, R, W):
        S.op(eng, lambda E: E.scalar_tensor_tensor(out=out, in0=in0, scalar=scalar, in1=in1, op0=op0, op1=op1), R, W)

    def CP(eng, out, in_, R, W):
        if eng == 'act':
            S.op('act', lambda E: E.copy(out=out, in_=in_), R, W)
        else:
            S.op(eng, lambda E: E.tensor_copy(out=out, in_=in_), R, W)

    def LD(out, in_, W, extra=(), q='sp'):
        return S.dma(lambda E: E.dma_start(out=out, in_=in_), (), W, extra, q=q)

    def ST(out, in_, R, extra=()):
        return S.dma(lambda E: E.dma_start(out=out, in_=in_), R, (), extra)

    CAST_ENGS = ['act', 'dve']
    WQ = ['sp', 'pool']

    def wload(srcs, kt, n, deint=False, ceng=None):
        cnt['w'] += 1
        i = cnt['w'] % 2
        sv = wst[i][:, 0:kt * n].rearrange("p (k n) -> p k n", k=kt)
        for (ap, c0, w) in srcs:
            LD(sv[:, :, c0:c0 + w], ap.rearrange("(k p) n -> p k n", p=128), [wst[i].rng(0, kt * n)], q=WQ[i])
        cnt['cast'] += 1
        eng = ceng or CAST_ENGS[cnt['cast'] % 2]
        if deint:
            ov = wbf[i][:, 0:kt * n].rearrange("p (k g f) -> p k g f", k=kt, g=2)
            iv = wst[i][:, 0:kt * n].rearrange("p (k f g) -> p k g f", k=kt, g=2)
            CP(eng, ov, iv, [wst[i].rng(0, kt * n)], [wbf[i].rng(0, kt * n)])
            return wbf[i], ov
        bv = wbf[i][:, 0:kt * n].rearrange("p (k n) -> p k n", k=kt)
        CP(eng, bv, sv, [wst[i].rng(0, kt * n)], [wbf[i].rng(0, kt * n)])
        return wbf[i], bv

    def bcload(src_ap, extra=(), plus1=False):
        cnt['bc'] += 1
        b = bc[cnt['bc'] % 2]
        LD(b[:], src_ap.partition_broadcast(128), [b], extra)
        if plus1:
            TS('pool', b[:], b[:], 1.0, None, ALU.add, None, [b], [b])
        return b

    def layernorm(src, dst_tile, dst_ap):
        i = nst()
        for c in range(4):
            S.op('dve', lambda E, c=c: E.bn_stats(out=st_bn[i][:, c, :], in_=src[:, c * 512:(c + 1) * 512]), [src], [st_bn[i]])
        S.op('dve', lambda E: E.bn_aggr(out=st_mv[i][:], in_=st_bn[i][:]), [st_bn[i]], [st_mv[i]])
        ACT(st_a[i][:], st_mv[i][:, 1:2], AF.Sqrt, [st_mv[i]], [st_a[i]], bias=EPS, scale=1.0)
        S.op('dve', lambda E: E.reciprocal(out=st_a[i][:], in_=st_a[i][:]), [st_a[i]], [st_a[i]])
        TS('dve', dst_ap, src[:], st_mv[i][:, 0:1], st_a[i][:, 0:1], ALU.subtract, ALU.mult, [src, st_mv[i], st_a[i]], [dst_tile])

    def transpose_tile(src, t, want_f32=None):
        for g in range(4):
            p = nps()
            for j in range(4):
                k = g * 4 + j
                TR(p[:, j * 128:(j + 1) * 128], src[:, k * 128:(k + 1) * 128], [src], [p])
            pv = p[:].rearrange("p (a b) -> p a b", a=4)
            CP('act', hT[:, g * 4:(g + 1) * 4, t * 128:(t + 1) * 128], pv, [p], [hT])
            if want_f32 is not None:
                CP('dve', want_f32[:, g * 4:(g + 1) * 4, :], pv, [p], [want_f32])

    LD(ident[:], ident_d, [ident])
    LD(silu_c[:], cfm_d, [silu_c])
    LD(sinks[:], sinks_d.partition_broadcast(128), [sinks])
    LD(nbgate[:], bgate_d, [nbgate])
    LD(wr_sb[:], wr_d.rearrange("(k p) n -> p k n", p=128), [wr_sb])
    LD(br_bc[:], br_d.partition_broadcast(128), [br_bc])
    LD(gmask[:], gmask_d, [gmask])
    if TP:
        LD(flag[:], flag_d, [flag])
    if do_moe:
        LD(bgu[:], bgu_d, [bgu])
        bgu_up = bgu[:].rearrange("p (n g) -> p n g", g=2)[:, :, 1]
        TS('dve', bgu_up, bgu_up, 1.0, None, ALU.add, None, [bgu], [bgu])
    for g in range(2):
        S.op('pool', lambda E, g=g: E.memset(halo_k[g][:], 0.0), [], [halo_k[g]])
        S.op('pool', lambda E, g=g: E.memset(halo_v[g][:], 0.0), [], [halo_v[g]])
    TS('dve', nbgate[:], nbgate[:], -1.0, None, ALU.mult, None, [nbgate], [nbgate])
    ACT(silu_c[:], silu_c[:], AF.Silu, [silu_c], [silu_c])
    ada_tok = []
    arow = [S.sb(f"arow{i}", (1, 256), F32, off=RA + i * 1024) for i in range(2)]
    brow = [S.sb(f"brow{i}", (1, 256), F32, off=RA + 2048 + i * 1024) for i in range(2)]
    for gi in range(6 * D // 256):
        i = gi % 2
        c0 = gi * 256
        sv = wst[i][:].rearrange("p (k n) -> p k n", k=KT)
        LD(sv, wada_d[:, c0:c0 + 256].rearrange("(k p) n -> p k n", p=128), [wst[i]])
        LD(brow[i][:], bada_d[c0:c0 + 256].rearrange("(o n) -> o n", o=1), [brow[i]])
        p = nps()
        for k in range(KT):
            MM(p[0:1, 0:256], silu_c[:, k:k + 1], sv[:, k, :], k == 0, k == KT - 1, [silu_c, wst[i]], [p.rng(0, 256)])
        TTo('dve', arow[i][:], p[0:1, 0:256], brow[i][:], ALU.add, [p.rng(0, 256), brow[i]], [arow[i]])
        ada_tok.append(ST(ada_row[c0:c0 + 256].rearrange("(o n) -> o n", o=1), arow[i][:], [arow[i]]))

    def ada_bc(idx, plus1=False):
        return bcload(ada_row[idx * D:(idx + 1) * D], extra=ada_tok, plus1=plus1)

    S_tok = [None] * 4

    def chk(k):
        if stage <= k:
            raise _Stop()

    def sb_body(s, pre=False):
        t0 = s * SBT
        xsrc = xprev_d if pre else x_d
        last_pre = pre and s == NSBP - 1
        sc1p = ada_bc(1, plus1=True)
        sh1 = ada_bc(0)
        for t in range(TT):
            LD(xres[t][:], xsrc[t0 + t * 128:t0 + (t + 1) * 128, :], [xres[t]])
            layernorm(xres[t], tmpA, tmpA[:])
            TTo('pool', tmpA[:], tmpA[:], sc1p[:], ALU.mult, [tmpA, sc1p], [tmpA])
            TTo('dve', tmpA[:], tmpA[:], sh1[:], ALU.add, [tmpA, sh1], [tmpA])
            if dbg and not pre:
                ST(dbg_d["d_h"][t0 + t * 128:t0 + (t + 1) * 128, :], tmpA[:], [tmpA])
            transpose_tile(tmpA, t)

        chk(1)
        o = RA
        swab = S.sb(f"swab_{s}", (128, 16, 256), F32, off=o); o = _al(o + 16384)
        qT = S.sb(f"qT_{s}", (128, 4, SBT), BF16, off=o); o = _al(o + 4096)
        kT = S.sb(f"kT_{s}", (128, 128 + SBT), BF16, off=o); o = _al(o + 1280)
        vv = S.sb(f"vv_{s}", (128, 5, 64), BF16, off=o); o = _al(o + 640)
        s_sb = [S.sb(f"s_sb{i}_{s}", (128, 256), F32, off=o + i * 1024) for i in range(2)]; o = _al(o + 2048)
        p_sb = [S.sb(f"p_sb{i}_{s}", (128, 256), F32, off=o + i * 1024) for i in range(2)]; o = _al(o + 2048)
        pT = [S.sb(f"pT{i}_{s}", (128, 2, 128), BF16, off=o + i * 512) for i in range(2)]; o = _al(o + 1024)
        yatt = S.sb(f"yatt_{s}", (128, 4, 1024), F32, off=o); o = _al(o + 16384)
        sw0 = S.sb(f"sw0_{s}", (128, 256), F32, off=o); o = _al(o + 1024)
        if not pre:
            LD(swab[:], swab_d.rearrange("p (h n) -> p h n", h=16), [swab])
        for g in range(2 if (not pre or last_pre) else 0):
            wt, wv = wload([(win_d[:, OFF_KA + g * 64:OFF_KA + g * 64 + 64], 0, 64),
                            (win_d[:, OFF_KA + g * 64:OFF_KA + g * 64 + 64], 64, 64),
                            (win_d[:, OFF_VA + g * 64:OFF_VA + g * 64 + 64], 128, 64)], KT, 192)
            p = nps()
            for k in range(KT):
                MM(p[:, :], wv[:, k, 0:128], hT[:, k, :], k == 0, k == KT - 1, [wt, hT], [p])
            CP('dve', kT[:, 0:128], halo_k[g][:], [halo_k[g]], [kT.rng(0, 128)])
            CP('act', kT[:, 128:128 + SBT], p[:, :], [p], [kT.rng(128, 128 + SBT)])
            CP('pool', vv[:, 0, :], halo_v[g][:], [halo_v[g]], [vv.rng(0, 64)])
            for t in range(TT):
                p = nps()
                for k in range(KT):
                    MM(p[:, 0:64], hT[:, k, t * 128:(t + 1) * 128], wv[:, k, 128:192], k == 0, k == KT - 1, [wt, hT], [p.rng(0, 64)])
                CP('dve', vv[:, t + 1, :], p[:, 0:64], [p.rng(0, 64)], [vv.rng((t + 1) * 64, (t + 2) * 64)])
            if pre:
                TS('dve', halo_k[g][:], kT[:, SBT:SBT + 128], flag[:, 0:1], None, ALU.mult, None, [kT, flag], [halo_k[g]])
                TS('dve', halo_v[g][:], vv[:, 4, :], flag[:, 0:1], None, ALU.mult, None, [vv, flag], [halo_v[g]])
                continue
            CP('pool', halo_k[g][:], kT[:, SBT:SBT + 128], [kT], [halo_k[g]])
            CP('pool', halo_v[g][:], vv[:, 4, :], [vv], [halo_v[g]])
            for half in range(2):
                c0 = OFF_QA + g * 512 + half * 256
                wt, wv = wload([(win_d[:, c0:c0 + 256], 0, 256)], KT, 256)
                for jb in range(2):
                    p = nps()
                    for k in range(KT):
                        MM(p[:, :], wv[:, k, jb * 128:(jb + 1) * 128], hT[:, k, :], k == 0, k == KT - 1, [wt, hT], [p])
                    ACT(qT[:, half * 2 + jb, :], p[:, :], AF.Copy, [p], [qT], scale=0.125)
            for hl in range(8):
                h = g * 8 + hl
                jb, hf = hl // 2, hl % 2
                r0, r1 = hf * 64, hf * 64 + 64
                for n in range(TT):
                    first = (s == 0 and n == 0 and not TP)
                    NS = 128 if first else 256
                    kc0 = 128 + n * 128 if first else n * 128
                    if TP and s == 0 and n == 0:
                        LD(sw0[:], swab0_d[:, h * 256:(h + 1) * 256], [sw0])
                        bias_ap, bias_t = sw0[:, 0:256], sw0
                    else:
                        bias_ap, bias_t = swab[:, h, 256 - NS:256], swab
                    i2 = (hl * TT + n) % 2
                    ii = nst()
                    p = nps()
                    MM(p[:, 0:NS], qT[r0:r1, jb, n * 128:(n + 1) * 128], kT[r0:r1, kc0:kc0 + NS], True, True, [qT, kT], [p.rng(0, NS)])
                    TTo('dve', s_sb[i2][:, 0:NS], p[:, 0:NS], bias_ap, ALU.add, [p.rng(0, NS), bias_t], [s_sb[i2]])
                    S.op('dve', lambda E, i2=i2, NS=NS, ii=ii: E.tensor_reduce(out=st_a[ii][:], in_=s_sb[i2][:, 0:NS], axis=AX.X, op=ALU.max), [s_sb[i2]], [st_a[ii]])
                    TS('dve', st_a[ii][:], st_a[ii][:], sinks[:, h:h + 1], -1.0, ALU.max, ALU.mult, [st_a[ii], sinks], [st_a[ii]])
                    ACT(p_sb[i2][:, 0:NS], s_sb[i2][:, 0:NS], AF.Exp, [s_sb[i2], st_a[ii]], [p_sb[i2], st_b[ii]], bias=st_a[ii][:, 0:1], accum=st_b[ii][:, 0:1])
                    ACT(st_c[ii][:], sinks[:, h:h + 1], AF.Exp, [sinks, st_a[ii]], [st_c[ii]], bias=st_a[ii][:, 0:1])
                    TTo('dve', st_b[ii][:], st_b[ii][:], st_c[ii][:], ALU.add, [st_b[ii], st_c[ii]], [st_b[ii]])
                    S.op('dve', lambda E, ii=ii: E.reciprocal(out=st_b[ii][:], in_=st_b[ii][:]), [st_b[ii]], [st_b[ii]])
                    p2 = nps()
                    nsb = NS // 128
                    for sbk in range(nsb):
                        TR(p2[:, sbk * 128:(sbk + 1) * 128], p_sb[i2][:, sbk * 128:(sbk + 1) * 128], [p_sb[i2]], [p2.rng(0, NS)])
                    CP('act', pT[i2][:, 0:nsb, :], p2[:, 0:NS].rearrange("p (a b) -> p a b", a=nsb), [p2.rng(0, NS)], [pT[i2]])
                    p3 = nps()
                    for sbk in range(nsb):
                        vt = n + 1 if first else n + sbk
                        MM(p3[:, 0:64], pT[i2][:, sbk, :], vv[:, vt, :], sbk == 0, sbk == nsb - 1, [pT[i2], vv], [p3.rng(0, 64)])
                    TS('dve', yatt[:, n, h * 64:(h + 1) * 64], p3[:, 0:64], st_b[ii][:, 0:1], None, ALU.mult, None, [p3.rng(0, 64), st_b[ii]], [yatt.rng(n * 1024 + h * 64, n * 1024 + h * 64 + 64)])
        for n in range(0 if pre else TT):
            if dbg:
                ST(dbg_d["d_yatt"][t0 + n * 128:t0 + (n + 1) * 128, :], yatt[:, n, :], [yatt])
            for g2 in range(2):
                p = nps()
                for j in range(4):
                    k = g2 * 4 + j
                    TR(p[:, j * 128:(j + 1) * 128], yatt[:, n, k * 128:(k + 1) * 128], [yatt], [p])
                CP('act', yattT[:, g2 * 4:(g2 + 1) * 4, n * 128:(n + 1) * 128], p[:].rearrange("p (a b) -> p a b", a=4), [p], [yattT])

        chk(2)
        o = RA
        Ebuf = S.sb(f"Ebuf_{s}", (128, 2, SBT), F32, off=o); o = _al(o + 4096)
        L0 = S.sb(f"L0_{s}", (128, 2, SBT), F32, off=o); o = _al(o + 4096)
        L1 = S.sb(f"L1_{s}", (128, 2, SBT), F32, off=o); o = _al(o + 4096)
        qe = S.sb(f"qe_{s}", (128, 2, SBT), BF16, off=o); o = _al(o + 2048)
        ke = S.sb(f"ke_{s}", (128, 2, SBT), BF16, off=o); o = _al(o + 2048)
        kendT = S.sb(f"kendT_{s}", (128, 2, SBT), F32, off=o); o = _al(o + 4096)
        kend = S.sb(f"kend_{s}", (64, 8, 256), BF16, off=o); o = _al(o + 4096)
        vg = S.sb(f"vg_{s}", (64, 8, 512), BF16, off=o); o = _al(o + 8192)
        rs = S.sb(f"rs_{s}", (64, 8, 512), BF16, off=o); o = _al(o + 8192)
        Sst = S.sb(f"Sst_{s}", (128, 2, 512), F32, off=o); o = _al(o + 4096)
        Sb = S.sb(f"Sb_{s}", (128, 2, 512), BF16, off=o); o = _al(o + 2048)
        ytmp1 = S.sb(f"ytmp_{s}", (64, 512), F32, off=o); o = _al(o + 2048)
        ytmp = [ytmp1, ytmp1]
        gain_h = S.sb(f"gain_h_{s}", (64, 512), F32, off=o); o = _al(o + 2048)
        alrT = S.sb(f"alrT_{s}", (16, SBT), F32, off=o); o = _al(o + 2048)
        wup = S.sb(f"wup_{s}", (16, 256), F32, off=o); o = _al(o + 1024)
        AT = [S.sb(f"AT{i}_{s}", (64, 64), BF16, off=o + i * 128) for i in range(2)]; o = _al(o + 256)
        nll = S.sb(f"nll_{s}", (128, 2, 8), F32, off=o)
        dec = S.sb(f"dec_{s}", (128, 2, 8), F32, off=o + 64); o = _al(o + 128)
        assert o <= SB_END, (o, RA)
        wt, wv = wload([(win_d[:, OFF_AL:OFF_AL + 16], 0, 16)], KT, 16)
        p = nps()
        for k in range(KT):
            MM(p[0:16, :], wv[:, k, :], hT[:, k, :], k == 0, k == KT - 1, [wt, hT], [p])
        CP('act', alrT[:], p[0:16, :], [p], [alrT])
        for hd in range(4):
            LD(wup[:], wup_d[:, hd * 256:(hd + 1) * 256], [wup])
            for dc in range(2):
                p = nps()
                MM(p[:, :], wup[0:16, dc * 128:(dc + 1) * 128], alrT[0:16, :], True, True, [wup, alrT], [p])
                ACT(Ebuf[:, dc, :], p[:, :], AF.Exp, [p, nbgate], [Ebuf.rng(dc * SBT, (dc + 1) * SBT)], bias=nbgate[:, hd * 2 + dc:hd * 2 + dc + 1], scale=-1.0)
                ACT(L0[:, dc, :], Ebuf[:, dc, :], AF.Ln, [Ebuf.rng(dc * SBT, (dc + 1) * SBT)], [L0.rng(dc * SBT, (dc + 1) * SBT)], bias=1.0, scale=1.0)
            src, dst = L0, L1
            for sh in (1, 2, 4, 8, 16, 32):
                sv4 = src[:].rearrange("p d (c w) -> p d c w", w=64)
                dv4 = dst[:].rearrange("p d (c w) -> p d c w", w=64)
                TTo('dve', dv4[:, :, :, sh:64], sv4[:, :, :, sh:64], sv4[:, :, :, 0:64 - sh], ALU.add, [src], [dst])
                CP('pool', dv4[:, :, :, 0:sh], sv4[:, :, :, 0:sh], [src], [dst])
                src, dst = dst, src
            Lc = src
            Lc4 = Lc[:].rearrange("p d (c w) -> p d c w", w=64)
            TS('dve', nll[:], Lc4[:, :, :, 63], -1.0 / 16.0, None, ALU.mult, None, [Lc], [nll])
            ACT(dec[:], nll[:], AF.Exp, [nll], [dec])
            if not pre:
                wt, wv = wload([(win_d[:, OFF_QG + hd * 256:OFF_QG + hd * 256 + 256], 0, 256)], KT, 256)
            for dc in range(0 if pre else 2):
                p = nps()
                for k in range(KT):
                    MM(p[:, :], wv[:, k, dc * 128:(dc + 1) * 128], hT[:, k, :], k == 0, k == KT - 1, [wt, hT], [p])
                ACT(Ebuf[:, dc, :], Lc[:, dc, :], AF.Exp, [Lc], [Ebuf.rng(dc * SBT, (dc + 1) * SBT)], scale=-1.0 / 16.0)
                STT('dve', qe[:, dc, :], p[:, :], 1.0 / 16.0, Ebuf[:, dc, :], ALU.mult, ALU.mult, [p, Ebuf.rng(dc * SBT, (dc + 1) * SBT)], [qe.rng(dc * SBT, (dc + 1) * SBT)])
            wt, wv = wload([(win_d[:, OFF_KG + hd * 256:OFF_KG + hd * 256 + 256], 0, 256)], KT, 256)
            for dc in range(2):
                p = nps()
                for k in range(KT):
                    MM(p[:, :], wv[:, k, dc * 128:(dc + 1) * 128], hT[:, k, :], k == 0, k == KT - 1, [wt, hT], [p])
                er = Ebuf.rng(dc * SBT, (dc + 1) * SBT)
                ACT(Ebuf[:, dc, :], Lc[:, dc, :], AF.Exp, [Lc], [er], scale=1.0 / 16.0)
                TTo('dve', ke[:, dc, :], p[:, :], Ebuf[:, dc, :], ALU.mult, [p, er], [ke.rng(dc * SBT, (dc + 1) * SBT)])
                for c in range(8):
                    ACT(Ebuf[:, dc, c * 64:(c + 1) * 64], Lc[:, dc, c * 64:(c + 1) * 64], AF.Exp, [Lc, nll], [er], bias=nll[:, dc, c:c + 1], scale=1.0 / 16.0)
                TTo('dve', kendT[:, dc, :], p[:, :], Ebuf[:, dc, :], ALU.mult, [p, er], [kendT.rng(dc * SBT, (dc + 1) * SBT)])
            for c in range(8):
                p = nps()
                for dc in range(2):
                    TR(p[0:64, dc * 128:(dc + 1) * 128], kendT[:, dc, c * 64:(c + 1) * 64], [kendT], [p.rng(0, 256)])
                CP('act', kend[:, c, :], p[0:64, 0:256], [p.rng(0, 256)], [kend.rng(c * 256, (c + 1) * 256)])
            for (OFFX, dstt, is_r) in (((OFF_VG, vg, False),) if pre else ((OFF_VG, vg, False), (OFF_RG, rs, True))):
                for half in range(2):
                    c0 = OFFX + hd * 512 + half * 256
                    wt, wv = wload([(win_d[:, c0:c0 + 256], 0, 256)], KT, 256)
                    for c in range(8):
                        p = nps()
                        for k in range(KT):
                            MM(p[0:64, 0:256], hT[:, k, c * 64:(c + 1) * 64], wv[:, k, :], k == 0, k == KT - 1, [wt, hT], [p.rng(0, 256)])
                        wr_ = dstt.rng(c * 512 + half * 256, c * 512 + half * 256 + 256)
                        if is_r:
                            ACT(dstt[:, c, half * 256:(half + 1) * 256], p[0:64, 0:256], AF.Silu, [p.rng(0, 256)], [wr_])
                        else:
                            CP('dve', dstt[:, c, half * 256:(half + 1) * 256], p[0:64, 0:256], [p.rng(0, 256)], [wr_])
            LD(gain_h[:], ggain_d[hd * 512:(hd + 1) * 512].partition_broadcast(64), [gain_h])
            if s == 0 and (pre or not TP):
                S.op('pool', lambda E: E.memset(Sst[:], 0.0), [], [Sst])
            else:
                LD(Sst[:], S_dram[hd].rearrange("p (d v) -> p d v", d=2), [Sst], extra=[S_tok[hd]])
            CP('act', Sb[:], Sst[:], [Sst], [Sb])
            for c in range(8):
                cs = slice(c * 64, (c + 1) * 64)
                i2 = c % 2
                ii = nst()
                if not pre:
                    pa = nps()
                    for dc in range(2):
                        MM(pa[0:64, 0:64], ke[:, dc, cs], qe[:, dc, cs], dc == 0, dc == 1, [ke, qe], [pa.rng(0, 64)])
                    TTo('dve', AT[i2][:], pa[0:64, 0:64], gmask[:], ALU.mult, [pa.rng(0, 64), gmask], [AT[i2]])
                    po = nps()
                    MM(po[0:64, :], AT[i2][:], vg[:, c, :], True, False, [AT[i2], vg], [po])
                    MM(po[0:64, :], qe[:, 0, cs], Sb[:, 0, :], False, False, [qe, Sb], [po])
                    MM(po[0:64, :], qe[:, 1, cs], Sb[:, 1, :], False, True, [qe, Sb], [po])
                for dc in range(2):
                    pS = nps()
                    MM(pS[:, :], kend[:, c, dc * 128:(dc + 1) * 128], vg[:, c, :], True, True, [kend, vg], [pS])
                    sr = Sst.rng(dc * 512, (dc + 1) * 512)
                    STT('dve', Sst[:, dc, :], Sst[:, dc, :], dec[:, dc, c:c + 1], pS[:, :], ALU.mult, ALU.add, [sr, dec, pS], [sr])
                    if not pre:
                        CP('pool', Sb[:, dc, :], Sst[:, dc, :], [sr], [Sb.rng(dc * 512, (dc + 1) * 512)])
                if pre:
                    continue
                ACT(ytmp[i2][:], po[0:64, :], AF.Square, [po], [ytmp[i2], st_a[ii]], accum=st_a[ii][0:64, 0:1])
                ACT(st_a[ii][0:64, :], st_a[ii][0:64, :], AF.Sqrt, [st_a[ii]], [st_a[ii]], bias=EPS, scale=1.0 / 512.0)
                S.op('dve', lambda E, ii=ii: E.reciprocal(out=st_a[ii][0:64, :], in_=st_a[ii][0:64, :]), [st_a[ii]], [st_a[ii]])
                STT('dve', ytmp[i2][:], po[0:64, :], st_a[ii][0:64, 0:1], gain_h[:], ALU.mult, ALU.mult, [po, st_a[ii], gain_h], [ytmp[i2]])
                TTo('pool', ytmp[i2][:], ytmp[i2][:], rs[:, c, :], ALU.mult, [ytmp[i2], rs], [ytmp[i2]])
                if dbg:
                    ST(dbg_d["d_ygla"][t0 + c * 64:t0 + (c + 1) * 64, hd * 512:(hd + 1) * 512], ytmp[i2][:], [ytmp[i2]])
                pt = nps()
                for k4 in range(4):
                    TR(pt[:, k4 * 64:(k4 + 1) * 64], ytmp[i2][:, k4 * 128:(k4 + 1) * 128], [ytmp[i2]], [pt.rng(0, 256)])
                CP('act', yglaT[:, hd * 4:(hd + 1) * 4, cs], pt[:, 0:256].rearrange("p (a b) -> p a b", a=4), [pt.rng(0, 256)], [yglaT])
            if last_pre:
                TS('dve', Sst[:], Sst[:], flag[:, 0:1], None, ALU.mult, None, [Sst, flag], [Sst])
            S_tok[hd] = ST(S_dram[hd].rearrange("p (d v) -> p d v", d=2), Sst[:], [Sst])
        if pre:
            return

        chk(3)
        o = RA
        G = S.sb(f"G_{s}", (128, TT, NEXP), F32, off=o); o = _al(o + 512)
        GT = S.sb(f"GT_{s}", (32, SBT), F32, off=o); o = _al(o + 2048)
        G17 = S.sb(f"G17_{s}", (128, TT, NEXP), F32, off=o); o = _al(o + 512)
        P5O2 = o
        mrgT = S.sb(f"mrgT_{s}", (128, KT, SBT), BF16, off=o); o = _al(o + 16384)
        sga = S.sb(f"sga_{s}", (128, SBT), F32, off=o); o = _al(o + 2048)
        sgg = S.sb(f"sgg_{s}", (128, SBT), F32, off=o); o = _al(o + 2048)
        mt = [S.sb(f"mt{i}_{s}", (128, SBT), F32, off=o + i * 2048) for i in range(2)]; o = _al(o + 4096)
        hTf = S.sb(f"hTf_{s}", (128, KT, 128), F32, off=o); o = _al(o + 8192)
        lg = S.sb(f"lg_{s}", (128, NEXP), F32, off=o); o = _al(o + 128)
        ex = S.sb(f"ex_{s}", (128, NEXP), F32, off=o); o = _al(o + 128)
        m8 = S.sb(f"m8_{s}", (128, 8), F32, off=o); o = _al(o + 64)
        assert o <= SB_END, (o, RA)
        for j in range(KT):
            cs = slice(j * 128, (j + 1) * 128)
            wt, wv = wload([(wpa_d[:, cs], 0, 128)], 8, 128)
            ppa = nps()
            for k in range(8):
                MM(ppa[:, :], wv[:, k, :], yattT[:, k, :], k == 0, k == 7, [wt, yattT], [ppa])
            wt, wv = wload([(wpg_d[:, cs], 0, 128)], KT, 128)
            ppg = nps()
            for k in range(KT):
                MM(ppg[:, :], wv[:, k, :], yglaT[:, k, :], k == 0, k == KT - 1, [wt, yglaT], [ppg])
            wt, wv = wload([(win_d[:, OFF_GA + j * 128:OFF_GA + (j + 1) * 128], 0, 128),
                            (win_d[:, OFF_GG + j * 128:OFF_GG + (j + 1) * 128], 128, 128)], KT, 256)
            pga = nps()
            for k in range(KT):
                MM(pga[:, :], wv[:, k, 0:128], hT[:, k, :], k == 0, k == KT - 1, [wt, hT], [pga])
            pgg = nps()
            for k in range(KT):
                MM(pgg[:, :], wv[:, k, 128:256], hT[:, k, :], k == 0, k == KT - 1, [wt, hT], [pgg])
            ACT(sga[:], pga[:, :], AF.Sigmoid, [pga], [sga])
            ACT(sgg[:], pgg[:, :], AF.Sigmoid, [pgg], [sgg])
            TTo('dve', mt[0][:], ppa[:, :], sga[:], ALU.mult, [ppa, sga], [mt[0]])
            TTo('dve', mt[1][:], ppg[:, :], sgg[:], ALU.mult, [ppg, sgg], [mt[1]])
            TTo('pool', mrgT[:, j, :], mt[0][:], mt[1][:], ALU.add, [mt[0], mt[1]], [mrgT.rng(j * SBT, (j + 1) * SBT)])
        chk(3.2)
        gt1 = ada_bc(2)
        for c8 in range(8):
            cs = slice(c8 * 256, (c8 + 1) * 256)
            wt, wv = wload([(wo_d[:, cs], 0, 256)], KT, 256)
            for t in range(TT):
                p = nps()
                for k in range(KT):
                    MM(p[:, 0:256], mrgT[:, k, t * 128:(t + 1) * 128], wv[:, k, :], k == 0, k == KT - 1, [wt, mrgT], [p.rng(0, 256)])
                i2 = (c8 * TT + t) % 2
                xr = xres[t].rng(c8 * 256, (c8 + 1) * 256)
                TTo('dve', mt[i2][:, 0:256], p[:, 0:256], gt1[:, cs], ALU.mult, [p.rng(0, 256), gt1], [mt[i2]])
                STT('dve', xres[t][:, cs], xres[t][:, cs], ALPHA, mt[i2][:, 0:256], ALU.mult, ALU.add, [xr, mt[i2]], [xr])
        g1 = bcload(ln1g_d)
        for t in range(TT):
            layernorm(xres[t], xres[t], xres[t][:])
            TTo('pool', xres[t][:], xres[t][:], g1[:], ALU.mult, [xres[t], g1], [xres[t]])
        b1 = bcload(ln1b_d)
        for t in range(TT):
            TTo('dve', xres[t][:], xres[t][:], b1[:], ALU.add, [xres[t], b1], [xres[t]])
            if dbg:
                ST(dbg_d["d_x1"][t0 + t * 128:t0 + (t + 1) * 128, :], xres[t][:], [xres[t]])
        chk(3.5)
        sc2p = ada_bc(4, plus1=True)
        sh2 = ada_bc(3)
        for t in range(TT):
            layernorm(xres[t], tmpA, tmpA[:])
            TTo('pool', tmpA[:], tmpA[:], sc2p[:], ALU.mult, [tmpA, sc2p], [tmpA])
            TTo('dve', tmpA[:], tmpA[:], sh2[:], ALU.add, [tmpA, sh2], [tmpA])
            transpose_tile(tmpA, t, want_f32=hTf)
            p = nps()
            for k in range(KT):
                MM(p[:, 0:NEXP], hTf[:, k, :], wr_sb[:, k, :], k == 0, k == KT - 1, [hTf, wr_sb], [p.rng(0, NEXP)])
            ii = nst()
            TTo('dve', lg[:], p[:, 0:NEXP], br_bc[:], ALU.add, [p.rng(0, NEXP), br_bc], [lg])
            S.op('dve', lambda E: E.max(out=m8[:], in_=lg[:]), [lg], [m8])
            TS('dve', st_a[ii][:], m8[:, 0:1], -1.0, None, ALU.mult, None, [m8], [st_a[ii]])
            ACT(ex[:], lg[:], AF.Exp, [lg, st_a[ii]], [ex], bias=st_a[ii][:, 0:1])
            TS('dve', lg[:], lg[:], m8[:, 3:4], None, ALU.is_ge, None, [lg, m8], [lg])
            TTo('dve', ex[:], ex[:], lg[:], ALU.mult, [ex, lg], [ex])
            S.op('dve', lambda E, ii=ii: E.tensor_reduce(out=st_b[ii][:], in_=ex[:], axis=AX.X, op=ALU.add), [ex], [st_b[ii]])
            S.op('dve', lambda E, ii=ii: E.reciprocal(out=st_b[ii][:], in_=st_b[ii][:]), [st_b[ii]], [st_b[ii]])
            TS('dve', G[:, t, :], ex[:], st_b[ii][:, 0:1], None, ALU.mult, None, [ex, st_b[ii]], [G.rng(t * NEXP, (t + 1) * NEXP)])
            if dbg:
                ST(dbg_d["d_lg"][t0 + t * 128:t0 + (t + 1) * 128, 0:32], G[:, t, :], [G])
            TS('dve', G17[:, t, :], G[:, t, :], 1.0 / 1.702, None, ALU.mult, None, [G], [G17.rng(t * NEXP, (t + 1) * NEXP)])
            p = nps()
            TR(p[0:32, 0:128], G[:, t, :], [G], [p.rng(0, 128)])
            CP('act', GT[:, t * 128:(t + 1) * 128], p[0:32, 0:128], [p.rng(0, 128)], [GT.rng(t * 128, (t + 1) * 128)])

        chk(4)
        o = P5O2
        accf = [S.sb(f"accf{t}_{s}", (128, D), F32, off=o + t * 8192) for t in range(TT)]; o = _al(o + 4 * 8192)
        actT = S.sb(f"actT_{s}", (128, KT, SBT), BF16, off=o); o = _al(o + 16384)
        et = [S.sb(f"et{i}_{s}", (128, SBT), F32, off=tmpA.addr + i * 2048) for i in range(3)]
        assert o <= SB_END, o
        if do_moe:
            bdt = bc[(cnt['bc'] + 1) % 2]
            cnt['bc'] += 1
            LD(bdt[0:32, :], bd_d, [bdt])
            for t in range(TT):
                for c4 in range(4):
                    cs = slice(c4 * 512, (c4 + 1) * 512)
                    p = nps()
                    MM(p[:, :], GT[0:32, t * 128:(t + 1) * 128], bdt[0:32, cs], True, True, [GT, bdt], [p])
                    CP('act' if c4 % 2 else 'dve', accf[t][:, cs], p[:, :], [p], [accf[t].rng(c4 * 512, (c4 + 1) * 512)])
            for e in range(NEXP):
                for j in range(KT):
                    wt, wv = wload([(wgu_d[e][:, j * 256:(j + 1) * 256], 0, 256)], KT, 256, deint=True, ceng='act')
                    pg = nps()
                    for k in range(KT):
                        MM(pg[:, :], wv[:, k, 0, :], hT[:, k, :], k == 0, k == KT - 1, [wt, hT], [pg])
                    pu = nps()
                    for k in range(KT):
                        MM(pu[:, :], wv[:, k, 1, :], hT[:, k, :], k == 0, k == KT - 1, [wt, hT], [pu])
                    bcol = (e * 16 + j) * 2
                    TS('dve', et[0][:], pg[:, :], bgu[:, bcol:bcol + 1], 7.0, ALU.add, ALU.min, [pg, bgu], [et[0]])
                    ACT(et[1][:], et[0][:], AF.Silu, [et[0]], [et[1]], scale=1.702)
                    TS('dve', et[2][:], pu[:, :], bgu[:, bcol + 1:bcol + 2], 8.0, ALU.add, ALU.min, [pu, bgu], [et[2]])
                    STT('dve', actT[:, j, :], et[2][:], -6.0, et[1][:], ALU.max, ALU.mult, [et[2], et[1]], [actT.rng(j * SBT, (j + 1) * SBT)])
                for c8 in range(8):
                    cs = slice(c8 * 256, (c8 + 1) * 256)
                    wt, wv = wload([(wd_d[e][:, cs], 0, 256)], KT, 256, ceng='dve')
                    for t in range(TT):
                        p = nps()
                        for k in range(KT):
                            MM(p[:, 0:256], actT[:, k, t * 128:(t + 1) * 128], wv[:, k, :], k == 0, k == KT - 1, [wt, actT], [p.rng(0, 256)])
                        ar = accf[t].rng(c8 * 256, (c8 + 1) * 256)
                        STT('dve', accf[t][:, cs], p[:, 0:256], G17[:, t, e:e + 1], accf[t][:, cs], ALU.mult, ALU.add, [p.rng(0, 256), G17, ar], [ar])
        else:
            for t in range(TT):
                S.op('pool', lambda E, t=t: E.memset(accf[t][:], 0.0), [], [accf[t]])

        gt2 = ada_bc(5)
        for t in range(TT):
            TTo('pool', accf[t][:], accf[t][:], gt2[:], ALU.mult, [accf[t], gt2], [accf[t]])
            STT('dve', xres[t][:], xres[t][:], ALPHA, accf[t][:], ALU.mult, ALU.add, [xres[t], accf[t]], [xres[t]])
        g2 = bcload(ln2g_d)
        for t in range(TT):
            layernorm(xres[t], xres[t], xres[t][:])
            TTo('pool', xres[t][:], xres[t][:], g2[:], ALU.mult, [xres[t], g2], [xres[t]])
        b2 = bcload(ln2b_d)
        for t in range(TT):
            TTo('dve', xres[t][:], xres[t][:], b2[:], ALU.add, [xres[t], b2], [xres[t]])
            ST(out_d[t0 + t * 128:t0 + (t + 1) * 128, :], xres[t][:], [xres[t]])

    try:
        for s in range(NSBP):
            sb_body(s, pre=True)
        for s in range(NSB if stage > 0 else 0):
            sb_body(s)
    except _Stop:
        pass
    S.finish()
    S.run()
    return nc, S


def _consts():
    ident = np.eye(128, dtype=np.float32)
    q = np.arange(128)[:, None]
    s_ = np.arange(256)[None, :]
    dist = (q + 128 - s_).astype(np.float32)
    valid = (dist >= 0) & (dist < 128)
    slopes = (2.0 ** (-8.0 * (np.arange(16, dtype=np.float32) + 1.0) / 16)).astype(np.float32)
    swab = np.where(valid[:, None, :], -slopes[None, :, None] * dist[:, None, :], np.float32(NEG)).astype(np.float32)
    gmask = (np.arange(64)[:, None] <= np.arange(64)[None, :]).astype(np.float32)
    return ident, np.ascontiguousarray(swab.reshape(128, 16 * 256)), gmask


def make_in_maps(inputs, batches, T, do_moe=True):
    f = lambda a: np.ascontiguousarray(np.asarray(a, dtype=np.float32))
    ident, swab, gmask = _consts()
    shared = dict(
        w_ada=f(inputs["w_ada"][0]), b_ada=f(inputs["b_ada"][0]), w_in=f(inputs["w_in"][0]),
        w_gla_up=f(inputs["w_gla_gate_up"][0]),
        b_gate_fm=f(np.asarray(inputs["b_gla_gate"][0]).reshape(8, 128).T),
        attn_sinks=f(inputs["attn_sinks"][0]), gla_gain=f(inputs["gla_norm_gain"][0]),
        w_pa=f(inputs["w_branch_att"][0]), w_pg=f(inputs["w_branch_gla"][0]), w_out=f(inputs["w_out"][0]),
        ln1_gain=f(inputs["ln1_gain"][0]), ln1_bias=f(inputs["ln1_bias"][0]),
        ln2_gain=f(inputs["ln2_gain"][0]), ln2_bias=f(inputs["ln2_bias"][0]),
        w_router=f(inputs["w_router"][0]), b_router=f(inputs["b_router"][0]),
        ident=ident, swa_bias=swab, gla_mask=gmask)
    if do_moe:
        bgu = np.asarray(inputs["b_gate_up"][0]).reshape(NEXP, 16, 128, 2)
        shared.update(
            w_gate_up=f(inputs["w_gate_up"][0]), w_down=f(inputs["w_down"][0]), b_down=f(inputs["b_down"][0]),
            b_gu_fm=f(bgu.transpose(2, 0, 1, 3).reshape(128, NEXP * 32)))
    maps = []
    for b in batches:
        m = dict(shared)
        m["x"] = f(inputs["x"][b, :T])
        m["c_fm"] = f(np.asarray(inputs["c"][b]).reshape(KT, 128).T)
        maps.append(m)
    return maps


N_CORES = 8
SEQ = 4096
HALF = SEQ // 2


def make_in_maps8(inputs, cores, do_moe=True):
    base = make_in_maps(inputs, [0], HALF, do_moe=do_moe)[0]
    f = lambda a: np.ascontiguousarray(np.asarray(a, dtype=np.float32))
    _, swab, _ = _consts()
    swab_first = swab.reshape(128, 16, 256).copy()
    swab_first[:, :, 0:128] = NEG
    swab_first = np.ascontiguousarray(swab_first.reshape(128, 16 * 256))
    maps = []
    for c in cores:
        b, half = c // 2, c % 2
        m = dict(base)
        m["x"] = f(inputs["x"][b, half * HALF:(half + 1) * HALF])
        m["c_fm"] = f(np.asarray(inputs["c"][b]).reshape(KT, 128).T)
        if half:
            m["xprev"] = f(inputs["x"][b, 0:HALF])
            m["flag"] = np.ones((128, 1), np.float32)
            m["swa_bias0"] = swab
        else:
            m["xprev"] = np.zeros((HALF, D), np.float32)
            m["flag"] = np.zeros((128, 1), np.float32)
            m["swa_bias0"] = swab_first
        maps.append(m)
    return maps


def kernel(**inputs):
    nc, _ = build_nc(HALF, TP=HALF)
    maps = make_in_maps8(inputs, list(range(N_CORES)))
    res = run_bass_kernel_spmd(nc, maps, core_ids=list(range(N_CORES)))
    B = N_CORES // 2
    out = np.empty((B, SEQ, D), np.float32)
    for c, r in enumerate(res.results):
        out[c // 2, (c % 2) * HALF:(c % 2 + 1) * HALF] = np.asarray(r["out"], dtype=np.float32)
    return out
```

```python
import numpy as np
import concourse.bass as bass
import concourse.mybir as mybir
from concourse.bass_utils import run_bass_kernel_spmd

F32 = mybir.dt.float32
BF16 = mybir.dt.bfloat16
AF = mybir.ActivationFunctionType
ALU = mybir.AluOpType
AX = mybir.AxisListType
ISZ = {F32: 4, BF16: 2}

UNIT = 256
SB_BASE = 16512
SB_END = 229376
EPOCH = 30000
ENGS = ['pe', 'act', 'dve', 'pool', 'sp']


class Tile:
    def __init__(self, h, space, addr, shape, dtype):
        self.h, self.space, self.addr, self.shape, self.dtype = h, space, addr, shape, dtype
        self.isz = ISZ[dtype]
        n = 1
        for s in shape[1:]:
            n *= s
        self.nbytes = n * self.isz

    def __getitem__(self, k):
        return self.h[k]

    def rng(self, lo=None, hi=None):
        lo = 0 if lo is None else lo * self.isz
        hi = self.nbytes if hi is None else hi * self.isz
        return (self.space, self.addr + lo, self.addr + hi)


def _al(v):
    return ((v + UNIT - 1) // UNIT) * UNIT


def _rng(x):
    return x.rng() if isinstance(x, Tile) else x


class Sched:
    def __init__(self, nc, n_dma_slots=24):
        self.nc = nc
        self.eng = {'pe': nc.tensor, 'act': nc.scalar, 'dve': nc.vector, 'pool': nc.gpsimd, 'sp': nc.sync}
        self.q = {e: [] for e in ENGS}
        self.sem = {}
        self.epoch = {e: 0 for e in ENGS}
        self.cnt = {e: 0 for e in ENGS}
        for e in ENGS:
            self.sem[(e, 0)] = nc.alloc_semaphore(f"tl_{e}_0")
        self.known = {e: {} for e in ENGS}
        self.mem_w = {'sb': {}, 'ps': {}}
        self.mem_r = {'sb': {}, 'ps': {}}
        self.sb_off = SB_BASE
        self.ps_n = 0
        self.nslots = n_dma_slots
        self.dslot = {}
        self.dnext = {e: 0 for e in ENGS}
        self.ninstr = 0

    def sb(self, name, shape, dtype, off=None):
        t = Tile(None, 'sb', 0, shape, dtype)
        if off is None:
            al = UNIT if t.nbytes >= UNIT else 64
            off = ((self.sb_off + al - 1) // al) * al
            self.sb_off = off + ((t.nbytes + 63) // 64) * 64
        t.addr = off
        assert off >= SB_BASE and off + t.nbytes <= SB_END, (name, off, t.nbytes)
        t.h = self.nc.alloc_sbuf_tensor_at(name, list(shape), dtype, offset=off)
        return t

    def ps(self, name, shape=(128, 512), dtype=F32):
        t = Tile(None, 'ps', self.ps_n * 2048, shape, dtype)
        assert t.nbytes <= 2048
        self.ps_n += 1
        t.h = self.nc.alloc_psum_tensor(name, list(shape), dtype)
        return t

    def _units(self, r):
        sp, lo, hi = r
        if sp == 'ps':
            return sp, range((lo // 2048) * (2048 // UNIT), ((hi - 1) // 2048 + 1) * (2048 // UNIT))
        return sp, range(lo // UNIT, (hi - 1) // UNIT + 1)

    @staticmethod
    def _ps_as_writes(reads, writes):
        extra = [r for r in reads if _rng(r)[0] == 'ps']
        return (list(writes) + extra) if extra else writes

    def _deps(self, eng, reads, writes, extra):
        deps = {}

        def add(tok):
            if tok is None:
                return
            k, v = tok
            if eng == 'pe' and k[0] == 'pe':
                return
            if deps.get(k, 0) < v:
                deps[k] = v
        for r in reads:
            sp, us = self._units(_rng(r))
            W = self.mem_w[sp]
            for u in us:
                add(W.get(u))
        for r in writes:
            sp, us = self._units(_rng(r))
            W, Rr = self.mem_w[sp], self.mem_r[sp]
            for u in us:
                add(W.get(u))
                d = Rr.get(u)
                if d:
                    for k, v in d.items():
                        add((k, v))
        for tok in extra:
            add(tok)
        return deps

    def _emit_waits(self, eng, deps):
        kn = self.known[eng]
        for k, v in deps.items():
            if kn.get(k, 0) >= v:
                continue
            kn[k] = v
            sem = self.sem[k]
            self.q[eng].append(lambda E, sem=sem, v=v: E.wait_ge(sem, v))

    def _commit(self, tok, reads, writes):
        for r in writes:
            sp, us = self._units(_rng(r))
            W, Rr = self.mem_w[sp], self.mem_r[sp]
            for u in us:
                W[u] = tok
                Rr[u] = {}
        k, v = tok
        for r in reads:
            sp, us = self._units(_rng(r))
            Rr = self.mem_r[sp]
            for u in us:
                d = Rr.get(u)
                if d is None:
                    d = Rr[u] = {}
                d[k] = v

    def op(self, eng, fn, reads=(), writes=(), extra=()):
        writes = self._ps_as_writes(reads, writes)
        deps = self._deps(eng, reads, writes, extra)
        self._emit_waits(eng, deps)
        if self.cnt[eng] >= EPOCH:
            self.epoch[eng] += 1
            self.cnt[eng] = 0
            self.sem[(eng, self.epoch[eng])] = self.nc.alloc_semaphore(f"tl_{eng}_{self.epoch[eng]}")
        self.cnt[eng] += 1
        k = (eng, self.epoch[eng])
        sem = self.sem[k]
        self.q[eng].append(lambda E, fn=fn, sem=sem: fn(E).then_inc(sem, 1))
        tok = (k, self.cnt[eng])
        self._commit(tok, reads, writes)
        self.ninstr += 1
        return tok

    def dma(self, fn, reads=(), writes=(), extra=(), q='sp'):
        i = self.dnext[q]
        self.dnext[q] = (i + 1) % self.nslots
        k = ('d', q, i)
        if k not in self.sem:
            self.sem[k] = self.nc.alloc_semaphore(f"dma_{q}_{i}")
            self.dslot[k] = 0
        deps = self._deps(q, reads, writes, extra)
        if self.dslot[k] > 0:
            deps[k] = max(deps.get(k, 0), self.dslot[k])
        self._emit_waits(q, deps)
        self.dslot[k] += 16
        sem = self.sem[k]
        self.q[q].append(lambda E, fn=fn, sem=sem: fn(E).then_inc(sem, 16))
        tok = (k, self.dslot[k])
        self._commit(tok, reads, writes)
        self.ninstr += 1
        return tok

    def finish(self, q='sp'):
        for e in ENGS:
            deps = {k: v for k, v in self.dslot.items() if k[1] == e and v > 0}
            self._emit_waits(e, deps)

    def run(self):
        nc = self.nc
        with nc.Block() as block:
            @block.tensor
            def _(E):
                for f in self.q['pe']:
                    f(E)

            @block.scalar
            def _(E):
                for f in self.q['act']:
                    f(E)

            @block.vector
            def _(E):
                for f in self.q['dve']:
                    f(E)

            @block.gpsimd
            def _(E):
                for f in self.q['pool']:
                    f(E)

            @block.sync
            def _(E):
                for f in self.q['sp']:
                    f(E)


D = 2048
KT = 16
SBT = 512
TT = SBT // 128
NEXP = 32
ALPHA = float(2 ** 0.25)
EPS = 1e-5
OFF_QA, OFF_KA, OFF_VA, OFF_QG, OFF_KG, OFF_VG, OFF_RG, OFF_AL, OFF_GA, OFF_GG = (
    0, 1024, 1152, 1280, 2304, 3328, 5376, 7424, 7440, 9488)
NEG = -30000.0


class _Stop(Exception):
    pass


def build_nc(T, dbg=False, do_moe=True, stage=99, TP=0):
    NSB = T // SBT
    NSBP = TP // SBT
    nc = bass.Bass("TRN2", target_bir_lowering=False)

    def din(name, shape):
        return nc.dram_tensor(name, list(shape), F32, kind="ExternalInput").ap()

    x_d = din("x", (T, D))
    cfm_d = din("c_fm", (128, KT))
    wada_d = din("w_ada", (D, 6 * D))
    bada_d = din("b_ada", (6 * D,))
    win_d = din("w_in", (D, 11536))
    wup_d = din("w_gla_up", (16, 1024))
    bgate_d = din("b_gate_fm", (128, 8))
    sinks_d = din("attn_sinks", (16,))
    ggain_d = din("gla_gain", (D,))
    wpa_d = din("w_pa", (1024, D))
    wpg_d = din("w_pg", (D, D))
    wo_d = din("w_out", (D, D))
    ln1g_d, ln1b_d = din("ln1_gain", (D,)), din("ln1_bias", (D,))
    ln2g_d, ln2b_d = din("ln2_gain", (D,)), din("ln2_bias", (D,))
    wr_d = din("w_router", (D, NEXP))
    br_d = din("b_router", (NEXP,))
    if do_moe:
        wgu_d = din("w_gate_up", (NEXP, D, 2 * D))
        bgu_d = din("b_gu_fm", (128, NEXP * 16 * 2))
        wd_d = din("w_down", (NEXP, D, D))
        bd_d = din("b_down", (NEXP, D))
    ident_d = din("ident", (128, 128))
    swab_d = din("swa_bias", (128, 16 * 256))
    gmask_d = din("gla_mask", (64, 64))
    if TP:
        xprev_d = din("xprev", (TP, D))
        flag_d = din("flag", (128, 1))
        swab0_d = din("swa_bias0", (128, 16 * 256))
    out_d = nc.dram_tensor("out", [T, D], F32, kind="ExternalOutput").ap()
    dbg_d = {}
    if dbg:
        for nm, w in (("d_h", D), ("d_yatt", 1024), ("d_ygla", D), ("d_x1", D), ("d_lg", 32 * 2)):
            dbg_d[nm] = nc.dram_tensor(nm, [T, w], F32, kind="ExternalOutput").ap()
    ada_row = nc.dram_tensor("ada_row", [6 * D], F32).ap()
    S_dram = nc.dram_tensor("S_dram", [4, 128, 1024], F32).ap()

    S = Sched(nc)
    xres = [S.sb(f"xres{t}", (128, D), F32) for t in range(TT)]
    hT = S.sb("hT", (128, KT, SBT), BF16)
    yattT = S.sb("yattT", (128, 8, SBT), BF16)
    yglaT = S.sb("yglaT", (128, KT, SBT), BF16)
    wst = [S.sb(f"wst{i}", (128, 4096), F32) for i in range(2)]
    wbf = [S.sb(f"wbf{i}", (128, 4096), BF16) for i in range(2)]
    bc = [S.sb(f"bc{i}", (128, D), F32) for i in range(2)]
    tmpA = S.sb("tmpA", (128, D), F32)
    ident = S.sb("ident", (128, 128), F32)
    silu_c = S.sb("silu_c", (128, KT), F32)
    sinks = S.sb("sinks", (128, 16), F32)
    nbgate = S.sb("nbgate", (128, 8), F32)
    wr_sb = S.sb("wr_sb", (128, KT, NEXP), F32)
    br_bc = S.sb("br_bc", (128, NEXP), F32)
    halo_k = [S.sb(f"halo_k{g}", (128, 128), BF16) for g in range(2)]
    halo_v = [S.sb(f"halo_v{g}", (128, 64), BF16) for g in range(2)]
    gmask = S.sb("gmask", (64, 64), F32)
    flag = S.sb("flag", (128, 1), F32)
    if do_moe:
        bgu = S.sb("bgu", (128, NEXP * 32), F32)
    NST = 5
    st_bn = [S.sb(f"st_bn{i}", (128, 4, 6), F32) for i in range(NST)]
    st_mv = [S.sb(f"st_mv{i}", (128, 2), F32) for i in range(NST)]
    st_a = [S.sb(f"st_a{i}", (128, 1), F32) for i in range(NST)]
    st_b = [S.sb(f"st_b{i}", (128, 1), F32) for i in range(NST)]
    st_c = [S.sb(f"st_c{i}", (128, 1), F32) for i in range(NST)]
    RA = ((S.sb_off + UNIT - 1) // UNIT) * UNIT
    PS = [S.ps(f"ps{i}") for i in range(8)]
    cnt = {'ps': 0, 'w': 0, 'st': 0, 'cast': 0, 'bc': 0}

    def nps():
        cnt['ps'] += 1
        return PS[cnt['ps'] % 8]

    def nst():
        cnt['st'] += 1
        return cnt['st'] % NST

    def MM(out, lhsT, rhs, start, stop, R, W):
        S.op('pe', lambda E: E.matmul(out, lhsT=lhsT, rhs=rhs, start=start, stop=stop), R, W)

    def TR(out, in_, R, W):
        n = in_.shape[0]
        S.op('pe', lambda E: E.transpose(out=out, in_=in_, identity=ident[0:n, 0:n]), list(R) + [ident], W)

    def ACT(out, in_, func, R, W, bias=0.0, scale=1.0, accum=None):
        S.op('act', lambda E: E.activation(out=out, in_=in_, func=func, bias=bias, scale=scale, accum_out=accum), R, W)

    def TS(eng, out, in0, s1, s2, op0, op1, R, W):
        if op1 is None:
            S.op(eng, lambda E: E.tensor_scalar(out=out, in0=in0, scalar1=s1, scalar2=None, op0=op0), R, W)
        else:
            S.op(eng, lambda E: E.tensor_scalar(out=out, in0=in0, scalar1=s1, scalar2=s2, op0=op0, op1=op1), R, W)

    def TTo(eng, out, in0, in1, op, R, W):
        S.op(eng, lambda E: E.tensor_tensor(out=out, in0=in0, in1=in1, op=op), R, W)

    def STT(eng, out, in0, scalar, in1, op0, op1, R, W):
        S.op(eng, lambda E: E.scalar_tensor_tensor(out=out, in0=in0, scalar=scalar, in1=in1, op0=op0, op1=op1), R, W)

    def CP(eng, out, in_, R, W):
        if eng == 'act':
            S.op('act', lambda E: E.copy(out=out, in_=in_), R, W)
        else:
            S.op(eng, lambda E: E.tensor_copy(out=out, in_=in_), R, W)

    def LD(out, in_, W, extra=(), q='sp'):
        return S.dma(lambda E: E.dma_start(out=out, in_=in_), (), W, extra, q=q)

    def ST(out, in_, R, extra=()):
        return S.dma(lambda E: E.dma_start(out=out, in_=in_), R, (), extra)

    CAST_ENGS = ['act', 'dve']
    WQ = ['sp', 'pool']

    def wload(srcs, kt, n, deint=False, ceng=None):
        cnt['w'] += 1
        i = cnt['w'] % 2
        sv = wst[i][:, 0:kt * n].rearrange("p (k n) -> p k n", k=kt)
        for (ap, c0, w) in srcs:
            LD(sv[:, :, c0:c0 + w], ap.rearrange("(k p) n -> p k n", p=128), [wst[i].rng(0, kt * n)], q=WQ[i])
        cnt['cast'] += 1
        eng = ceng or CAST_ENGS[cnt['cast'] % 2]
        if deint:
            ov = wbf[i][:, 0:kt * n].rearrange("p (k g f) -> p k g f", k=kt, g=2)
            iv = wst[i][:, 0:kt * n].rearrange("p (k f g) -> p k g f", k=kt, g=2)
            CP(eng, ov, iv, [wst[i].rng(0, kt * n)], [wbf[i].rng(0, kt * n)])
            return wbf[i], ov
        bv = wbf[i][:, 0:kt * n].rearrange("p (k n) -> p k n", k=kt)
        CP(eng, bv, sv, [wst[i].rng(0, kt * n)], [wbf[i].rng(0, kt * n)])
        return wbf[i], bv

    def bcload(src_ap, extra=(), plus1=False):
        cnt['bc'] += 1
        b = bc[cnt['bc'] % 2]
        LD(b[:], src_ap.partition_broadcast(128), [b], extra)
        if plus1:
            TS('pool', b[:], b[:], 1.0, None, ALU.add, None, [b], [b])
        return b

    def layernorm(src, dst_tile, dst_ap):
        i = nst()
        for c in range(4):
            S.op('dve', lambda E, c=c: E.bn_stats(out=st_bn[i][:, c, :], in_=src[:, c * 512:(c + 1) * 512]), [src], [st_bn[i]])
        S.op('dve', lambda E: E.bn_aggr(out=st_mv[i][:], in_=st_bn[i][:]), [st_bn[i]], [st_mv[i]])
        ACT(st_a[i][:], st_mv[i][:, 1:2], AF.Sqrt, [st_mv[i]], [st_a[i]], bias=EPS, scale=1.0)
        S.op('dve', lambda E: E.reciprocal(out=st_a[i][:], in_=st_a[i][:]), [st_a[i]], [st_a[i]])
        TS('dve', dst_ap, src[:], st_mv[i][:, 0:1], st_a[i][:, 0:1], ALU.subtract, ALU.mult, [src, st_mv[i], st_a[i]], [dst_tile])

    def transpose_tile(src, t, want_f32=None):
        for g in range(4):
            p = nps()
            for j in range(4):
                k = g * 4 + j
                TR(p[:, j * 128:(j + 1) * 128], src[:, k * 128:(k + 1) * 128], [src], [p])
            pv = p[:].rearrange("p (a b) -> p a b", a=4)
            CP('act', hT[:, g * 4:(g + 1) * 4, t * 128:(t + 1) * 128], pv, [p], [hT])
            if want_f32 is not None:
                CP('dve', want_f32[:, g * 4:(g + 1) * 4, :], pv, [p], [want_f32])

    LD(ident[:], ident_d, [ident])
    LD(silu_c[:], cfm_d, [silu_c])
    LD(sinks[:], sinks_d.partition_broadcast(128), [sinks])
    LD(nbgate[:], bgate_d, [nbgate])
    LD(wr_sb[:], wr_d.rearrange("(k p) n -> p k n", p=128), [wr_sb])
    LD(br_bc[:], br_d.partition_broadcast(128), [br_bc])
    LD(gmask[:], gmask_d, [gmask])
    if TP:
        LD(flag[:], flag_d, [flag])
    if do_moe:
        LD(bgu[:], bgu_d, [bgu])
        bgu_up = bgu[:].rearrange("p (n g) -> p n g", g=2)[:, :, 1]
        TS('dve', bgu_up, bgu_up, 1.0, None, ALU.add, None, [bgu], [bgu])
    for g in range(2):
        S.op('pool', lambda E, g=g: E.memset(halo_k[g][:], 0.0), [], [halo_k[g]])
        S.op('pool', lambda E, g=g: E.memset(halo_v[g][:], 0.0), [], [halo_v[g]])
    TS('dve', nbgate[:], nbgate[:], -1.0, None, ALU.mult, None, [nbgate], [nbgate])
    ACT(silu_c[:], silu_c[:], AF.Silu, [silu_c], [silu_c])
    ada_tok = []
    arow = [S.sb(f"arow{i}", (1, 256), F32, off=RA + i * 1024) for i in range(2)]
    brow = [S.sb(f"brow{i}", (1, 256), F32, off=RA + 2048 + i * 1024) for i in range(2)]
    for gi in range(6 * D // 256):
        i = gi % 2
        c0 = gi * 256
        sv = wst[i][:].rearrange("p (k n) -> p k n", k=KT)
        LD(sv, wada_d[:, c0:c0 + 256].rearrange("(k p) n -> p k n", p=128), [wst[i]])
        LD(brow[i][:], bada_d[c0:c0 + 256].rearrange("(o n) -> o n", o=1), [brow[i]])
        p = nps()
        for k in range(KT):
            MM(p[0:1, 0:256], silu_c[:, k:k + 1], sv[:, k, :], k == 0, k == KT - 1, [silu_c, wst[i]], [p.rng(0, 256)])
        TTo('dve', arow[i][:], p[0:1, 0:256], brow[i][:], ALU.add, [p.rng(0, 256), brow[i]], [arow[i]])
        ada_tok.append(ST(ada_row[c0:c0 + 256].rearrange("(o n) -> o n", o=1), arow[i][:], [arow[i]]))

    def ada_bc(idx, plus1=False):
        return bcload(ada_row[idx * D:(idx + 1) * D], extra=ada_tok, plus1=plus1)

    S_tok = [None] * 4

    def chk(k):
        if stage <= k:
            raise _Stop()

    def sb_body(s, pre=False):
        t0 = s * SBT
        xsrc = xprev_d if pre else x_d
        last_pre = pre and s == NSBP - 1
        sc1p = ada_bc(1, plus1=True)
        sh1 = ada_bc(0)
        for t in range(TT):
            LD(xres[t][:], xsrc[t0 + t * 128:t0 + (t + 1) * 128, :], [xres[t]])
            layernorm(xres[t], tmpA, tmpA[:])
            TTo('pool', tmpA[:], tmpA[:], sc1p[:], ALU.mult, [tmpA, sc1p], [tmpA])
            TTo('dve', tmpA[:], tmpA[:], sh1[:], ALU.add, [tmpA, sh1], [tmpA])
            if dbg and not pre:
                ST(dbg_d["d_h"][t0 + t * 128:t0 + (t + 1) * 128, :], tmpA[:], [tmpA])
            transpose_tile(tmpA, t)

        chk(1)
        o = RA
        swab = S.sb(f"swab_{s}", (128, 16, 256), F32, off=o); o = _al(o + 16384)
        qT = S.sb(f"qT_{s}", (128, 4, SBT), BF16, off=o); o = _al(o + 4096)
        kT = S.sb(f"kT_{s}", (128, 128 + SBT), BF16, off=o); o = _al(o + 1280)
        vv = S.sb(f"vv_{s}", (128, 5, 64), BF16, off=o); o = _al(o + 640)
        s_sb = [S.sb(f"s_sb{i}_{s}", (128, 256), F32, off=o + i * 1024) for i in range(2)]; o = _al(o + 2048)
        p_sb = [S.sb(f"p_sb{i}_{s}", (128, 256), F32, off=o + i * 1024) for i in range(2)]; o = _al(o + 2048)
        pT = [S.sb(f"pT{i}_{s}", (128, 2, 128), BF16, off=o + i * 512) for i in range(2)]; o = _al(o + 1024)
        yatt = S.sb(f"yatt_{s}", (128, 4, 1024), F32, off=o); o = _al(o + 16384)
        sw0 = S.sb(f"sw0_{s}", (128, 256), F32, off=o); o = _al(o + 1024)
        if not pre:
            LD(swab[:], swab_d.rearrange("p (h n) -> p h n", h=16), [swab])
        for g in range(2 if (not pre or last_pre) else 0):
            wt, wv = wload([(win_d[:, OFF_KA + g * 64:OFF_KA + g * 64 + 64], 0, 64),
                            (win_d[:, OFF_KA + g * 64:OFF_KA + g * 64 + 64], 64, 64),
                            (win_d[:, OFF_VA + g * 64:OFF_VA + g * 64 + 64], 128, 64)], KT, 192)
            p = nps()
            for k in range(KT):
                MM(p[:, :], wv[:, k, 0:128], hT[:, k, :], k == 0, k == KT - 1, [wt, hT], [p])
            CP('dve', kT[:, 0:128], halo_k[g][:], [halo_k[g]], [kT.rng(0, 128)])
            CP('act', kT[:, 128:128 + SBT], p[:, :], [p], [kT.rng(128, 128 + SBT)])
            CP('pool', vv[:, 0, :], halo_v[g][:], [halo_v[g]], [vv.rng(0, 64)])
            for t in range(TT):
                p = nps()
                for k in range(KT):
                    MM(p[:, 0:64], hT[:, k, t * 128:(t + 1) * 128], wv[:, k, 128:192], k == 0, k == KT - 1, [wt, hT], [p.rng(0, 64)])
                CP('dve', vv[:, t + 1, :], p[:, 0:64], [p.rng(0, 64)], [vv.rng((t + 1) * 64, (t + 2) * 64)])
            if pre:
                TS('dve', halo_k[g][:], kT[:, SBT:SBT + 128], flag[:, 0:1], None, ALU.mult, None, [kT, flag], [halo_k[g]])
                TS('dve', halo_v[g][:], vv[:, 4, :], flag[:, 0:1], None, ALU.mult, None, [vv, flag], [halo_v[g]])
                continue
            CP('pool', halo_k[g][:], kT[:, SBT:SBT + 128], [kT], [halo_k[g]])
            CP('pool', halo_v[g][:], vv[:, 4, :], [vv], [halo_v[g]])
            for half in range(2):
                c0 = OFF_QA + g * 512 + half * 256
                wt, wv = wload([(win_d[:, c0:c0 + 256], 0, 256)], KT, 256)
                for jb in range(2):
                    p = nps()
                    for k in range(KT):
                        MM(p[:, :], wv[:, k, jb * 128:(jb + 1) * 128], hT[:, k, :], k == 0, k == KT - 1, [wt, hT], [p])
                    ACT(qT[:, half * 2 + jb, :], p[:, :], AF.Copy, [p], [qT], scale=0.125)
            for hl in range(8):
                h = g * 8 + hl
                jb, hf = hl // 2, hl % 2
                r0, r1 = hf * 64, hf * 64 + 64
                for n in range(TT):
                    first = (s == 0 and n == 0 and not TP)
                    NS = 128 if first else 256
                    kc0 = 128 + n * 128 if first else n * 128
                    if TP and s == 0 and n == 0:
                        LD(sw0[:], swab0_d[:, h * 256:(h + 1) * 256], [sw0])
                        bias_ap, bias_t = sw0[:, 0:256], sw0
                    else:
                        bias_ap, bias_t = swab[:, h, 256 - NS:256], swab
                    i2 = (hl * TT + n) % 2
                    ii = nst()
                    p = nps()
                    MM(p[:, 0:NS], qT[r0:r1, jb, n * 128:(n + 1) * 128], kT[r0:r1, kc0:kc0 + NS], True, True, [qT, kT], [p.rng(0, NS)])
                    TTo('dve', s_sb[i2][:, 0:NS], p[:, 0:NS], bias_ap, ALU.add, [p.rng(0, NS), bias_t], [s_sb[i2]])
                    S.op('dve', lambda E, i2=i2, NS=NS, ii=ii: E.tensor_reduce(out=st_a[ii][:], in_=s_sb[i2][:, 0:NS], axis=AX.X, op=ALU.max), [s_sb[i2]], [st_a[ii]])
                    TS('dve', st_a[ii][:], st_a[ii][:], sinks[:, h:h + 1], -1.0, ALU.max, ALU.mult, [st_a[ii], sinks], [st_a[ii]])
                    ACT(p_sb[i2][:, 0:NS], s_sb[i2][:, 0:NS], AF.Exp, [s_sb[i2], st_a[ii]], [p_sb[i2], st_b[ii]], bias=st_a[ii][:, 0:1], accum=st_b[ii][:, 0:1])
                    ACT(st_c[ii][:], sinks[:, h:h + 1], AF.Exp, [sinks, st_a[ii]], [st_c[ii]], bias=st_a[ii][:, 0:1])
                    TTo('dve', st_b[ii][:], st_b[ii][:], st_c[ii][:], ALU.add, [st_b[ii], st_c[ii]], [st_b[ii]])
                    S.op('dve', lambda E, ii=ii: E.reciprocal(out=st_b[ii][:], in_=st_b[ii][:]), [st_b[ii]], [st_b[ii]])
                    p2 = nps()
                    nsb = NS // 128
                    for sbk in range(nsb):
                        TR(p2[:, sbk * 128:(sbk + 1) * 128], p_sb[i2][:, sbk * 128:(sbk + 1) * 128], [p_sb[i2]], [p2.rng(0, NS)])
                    CP('act', pT[i2][:, 0:nsb, :], p2[:, 0:NS].rearrange("p (a b) -> p a b", a=nsb), [p2.rng(0, NS)], [pT[i2]])
                    p3 = nps()
                    for sbk in range(nsb):
                        vt = n + 1 if first else n + sbk
                        MM(p3[:, 0:64], pT[i2][:, sbk, :], vv[:, vt, :], sbk == 0, sbk == nsb - 1, [pT[i2], vv], [p3.rng(0, 64)])
                    TS('dve', yatt[:, n, h * 64:(h + 1) * 64], p3[:, 0:64], st_b[ii][:, 0:1], None, ALU.mult, None, [p3.rng(0, 64), st_b[ii]], [yatt.rng(n * 1024 + h * 64, n * 1024 + h * 64 + 64)])
        for n in range(0 if pre else TT):
            if dbg:
                ST(dbg_d["d_yatt"][t0 + n * 128:t0 + (n + 1) * 128, :], yatt[:, n, :], [yatt])
            for g2 in range(2):
                p = nps()
                for j in range(4):
                    k = g2 * 4 + j
                    TR(p[:, j * 128:(j + 1) * 128], yatt[:, n, k * 128:(k + 1) * 128], [yatt], [p])
                CP('act', yattT[:, g2 * 4:(g2 + 1) * 4, n * 128:(n + 1) * 128], p[:].rearrange("p (a b) -> p a b", a=4), [p], [yattT])

        chk(2)
        o = RA
        Ebuf = S.sb(f"Ebuf_{s}", (128, 2, SBT), F32, off=o); o = _al(o + 4096)
        L0 = S.sb(f"L0_{s}", (128, 2, SBT), F32, off=o); o = _al(o + 4096)
        L1 = S.sb(f"L1_{s}", (128, 2, SBT), F32, off=o); o = _al(o + 4096)
        qe = S.sb(f"qe_{s}", (128, 2, SBT), BF16, off=o); o = _al(o + 2048)
        ke = S.sb(f"ke_{s}", (128, 2, SBT), BF16, off=o); o = _al(o + 2048)
        kendT = S.sb(f"kendT_{s}", (128, 2, SBT), F32, off=o); o = _al(o + 4096)
        kend = S.sb(f"kend_{s}", (64, 8, 256), BF16, off=o); o = _al(o + 4096)
        vg = S.sb(f"vg_{s}", (64, 8, 512), BF16, off=o); o = _al(o + 8192)
        rs = S.sb(f"rs_{s}", (64, 8, 512), BF16, off=o); o = _al(o + 8192)
        Sst = S.sb(f"Sst_{s}", (128, 2, 512), F32, off=o); o = _al(o + 4096)
        Sb = S.sb(f"Sb_{s}", (128, 2, 512), BF16, off=o); o = _al(o + 2048)
        ytmp1 = S.sb(f"ytmp_{s}", (64, 512), F32, off=o); o = _al(o + 2048)
        ytmp = [ytmp1, ytmp1]
        gain_h = S.sb(f"gain_h_{s}", (64, 512), F32, off=o); o = _al(o + 2048)
        alrT = S.sb(f"alrT_{s}", (16, SBT), F32, off=o); o = _al(o + 2048)
        wup = S.sb(f"wup_{s}", (16, 256), F32, off=o); o = _al(o + 1024)
        AT = [S.sb(f"AT{i}_{s}", (64, 64), BF16, off=o + i * 128) for i in range(2)]; o = _al(o + 256)
        nll = S.sb(f"nll_{s}", (128, 2, 8), F32, off=o)
        dec = S.sb(f"dec_{s}", (128, 2, 8), F32, off=o + 64); o = _al(o + 128)
        assert o <= SB_END, (o, RA)
        wt, wv = wload([(win_d[:, OFF_AL:OFF_AL + 16], 0, 16)], KT, 16)
        p = nps()
        for k in range(KT):
            MM(p[0:16, :], wv[:, k, :], hT[:, k, :], k == 0, k == KT - 1, [wt, hT], [p])
        CP('act', alrT[:], p[0:16, :], [p], [alrT])
        for hd in range(4):
            LD(wup[:], wup_d[:, hd * 256:(hd + 1) * 256], [wup])
            for dc in range(2):
                p = nps()
                MM(p[:, :], wup[0:16, dc * 128:(dc + 1) * 128], alrT[0:16, :], True, True, [wup, alrT], [p])
                ACT(Ebuf[:, dc, :], p[:, :], AF.Exp, [p, nbgate], [Ebuf.rng(dc * SBT, (dc + 1) * SBT)], bias=nbgate[:, hd * 2 + dc:hd * 2 + dc + 1], scale=-1.0)
                ACT(L0[:, dc, :], Ebuf[:, dc, :], AF.Ln, [Ebuf.rng(dc * SBT, (dc + 1) * SBT)], [L0.rng(dc * SBT, (dc + 1) * SBT)], bias=1.0, scale=1.0)
            src, dst = L0, L1
            for sh in (1, 2, 4, 8, 16, 32):
                sv4 = src[:].rearrange("p d (c w) -> p d c w", w=64)
                dv4 = dst[:].rearrange("p d (c w) -> p d c w", w=64)
                TTo('dve', dv4[:, :, :, sh:64], sv4[:, :, :, sh:64], sv4[:, :, :, 0:64 - sh], ALU.add, [src], [dst])
                CP('pool', dv4[:, :, :, 0:sh], sv4[:, :, :, 0:sh], [src], [dst])
                src, dst = dst, src
            Lc = src
            Lc4 = Lc[:].rearrange("p d (c w) -> p d c w", w=64)
            TS('dve', nll[:], Lc4[:, :, :, 63], -1.0 / 16.0, None, ALU.mult, None, [Lc], [nll])
            ACT(dec[:], nll[:], AF.Exp, [nll], [dec])
            if not pre:
                wt, wv = wload([(win_d[:, OFF_QG + hd * 256:OFF_QG + hd * 256 + 256], 0, 256)], KT, 256)
            for dc in range(0 if pre else 2):
                p = nps()
                for k in range(KT):
                    MM(p[:, :], wv[:, k, dc * 128:(dc + 1) * 128], hT[:, k, :], k == 0, k == KT - 1, [wt, hT], [p])
                ACT(Ebuf[:, dc, :], Lc[:, dc, :], AF.Exp, [Lc], [Ebuf.rng(dc * SBT, (dc + 1) * SBT)], scale=-1.0 / 16.0)
                STT('dve', qe[:, dc, :], p[:, :], 1.0 / 16.0, Ebuf[:, dc, :], ALU.mult, ALU.mult, [p, Ebuf.rng(dc * SBT, (dc + 1) * SBT)], [qe.rng(dc * SBT, (dc + 1) * SBT)])
            wt, wv = wload([(win_d[:, OFF_KG + hd * 256:OFF_KG + hd * 256 + 256], 0, 256)], KT, 256)
            for dc in range(2):
                p = nps()
                for k in range(KT):
                    MM(p[:, :], wv[:, k, dc * 128:(dc + 1) * 128], hT[:, k, :], k == 0, k == KT - 1, [wt, hT], [p])
                er = Ebuf.rng(dc * SBT, (dc + 1) * SBT)
                ACT(Ebuf[:, dc, :], Lc[:, dc, :], AF.Exp, [Lc], [er], scale=1.0 / 16.0)
                TTo('dve', ke[:, dc, :], p[:, :], Ebuf[:, dc, :], ALU.mult, [p, er], [ke.rng(dc * SBT, (dc + 1) * SBT)])
                for c in range(8):
                    ACT(Ebuf[:, dc, c * 64:(c + 1) * 64], Lc[:, dc, c * 64:(c + 1) * 64], AF.Exp, [Lc, nll], [er], bias=nll[:, dc, c:c + 1], scale=1.0 / 16.0)
                TTo('dve', kendT[:, dc, :], p[:, :], Ebuf[:, dc, :], ALU.mult, [p, er], [kendT.rng(dc * SBT, (dc + 1) * SBT)])
            for c in range(8):
                p = nps()
                for dc in range(2):
                    TR(p[0:64, dc * 128:(dc + 1) * 128], kendT[:, dc, c * 64:(c + 1) * 64], [kendT], [p.rng(0, 256)])
                CP('act', kend[:, c, :], p[0:64, 0:256], [p.rng(0, 256)], [kend.rng(c * 256, (c + 1) * 256)])
            for (OFFX, dstt, is_r) in (((OFF_VG, vg, False),) if pre else ((OFF_VG, vg, False), (OFF_RG, rs, True))):
                for half in range(2):
                    c0 = OFFX + hd * 512 + half * 256
                    wt, wv = wload([(win_d[:, c0:c0 + 256], 0, 256)], KT, 256)
                    for c in range(8):
                        p = nps()
                        for k in range(KT):
                            MM(p[0:64, 0:256], hT[:, k, c * 64:(c + 1) * 64], wv[:, k, :], k == 0, k == KT - 1, [wt, hT], [p.rng(0, 256)])
                        wr_ = dstt.rng(c * 512 + half * 256, c * 512 + half * 256 + 256)
                        if is_r:
                            ACT(dstt[:, c, half * 256:(half + 1) * 256], p[0:64, 0:256], AF.Silu, [p.rng(0, 256)], [wr_])
                        else:
                            CP('dve', dstt[:, c, half * 256:(half + 1) * 256], p[0:64, 0:256], [p.rng(0, 256)], [wr_])
            LD(gain_h[:], ggain_d[hd * 512:(hd + 1) * 512].partition_broadcast(64), [gain_h])
            if s == 0 and (pre or not TP):
                S.op('pool', lambda E: E.memset(Sst[:], 0.0), [], [Sst])
            else:
                LD(Sst[:], S_dram[hd].rearrange("p (d v) -> p d v", d=2), [Sst], extra=[S_tok[hd]])
            CP('act', Sb[:], Sst[:], [Sst], [Sb])
            for c in range(8):
                cs = slice(c * 64, (c + 1) * 64)
                i2 = c % 2
                ii = nst()
                if not pre:
                    pa = nps()
                    for dc in range(2):
                        MM(pa[0:64, 0:64], ke[:, dc, cs], qe[:, dc, cs], dc == 0, dc == 1, [ke, qe], [pa.rng(0, 64)])
                    TTo('dve', AT[i2][:], pa[0:64, 0:64], gmask[:], ALU.mult, [pa.rng(0, 64), gmask], [AT[i2]])
                    po = nps()
                    MM(po[0:64, :], AT[i2][:], vg[:, c, :], True, False, [AT[i2], vg], [po])
                    MM(po[0:64, :], qe[:, 0, cs], Sb[:, 0, :], False, False, [qe, Sb], [po])
                    MM(po[0:64, :], qe[:, 1, cs], Sb[:, 1, :], False, True, [qe, Sb], [po])
                for dc in range(2):
                    pS = nps()
                    MM(pS[:, :], kend[:, c, dc * 128:(dc + 1) * 128], vg[:, c, :], True, True, [kend, vg], [pS])
                    sr = Sst.rng(dc * 512, (dc + 1) * 512)
                    STT('dve', Sst[:, dc, :], Sst[:, dc, :], dec[:, dc, c:c + 1], pS[:, :], ALU.mult, ALU.add, [sr, dec, pS], [sr])
                    if not pre:
                        CP('pool', Sb[:, dc, :], Sst[:, dc, :], [sr], [Sb.rng(dc * 512, (dc + 1) * 512)])
                if pre:
                    continue
                ACT(ytmp[i2][:], po[0:64, :], AF.Square, [po], [ytmp[i2], st_a[ii]], accum=st_a[ii][0:64, 0:1])
                ACT(st_a[ii][0:64, :], st_a[ii][0:64, :], AF.Sqrt, [st_a[ii]], [st_a[ii]], bias=EPS, scale=1.0 / 512.0)
                S.op('dve', lambda E, ii=ii: E.reciprocal(out=st_a[ii][0:64, :], in_=st_a[ii][0:64, :]), [st_a[ii]], [st_a[ii]])
                STT('dve', ytmp[i2][:], po[0:64, :], st_a[ii][0:64, 0:1], gain_h[:], ALU.mult, ALU.mult, [po, st_a[ii], gain_h], [ytmp[i2]])
                TTo('pool', ytmp[i2][:], ytmp[i2][:], rs[:, c, :], ALU.mult, [ytmp[i2], rs], [ytmp[i2]])
                if dbg:
                    ST(dbg_d["d_ygla"][t0 + c * 64:t0 + (c + 1) * 64, hd * 512:(hd + 1) * 512], ytmp[i2][:], [ytmp[i2]])
                pt = nps()
                for k4 in range(4):
                    TR(pt[:, k4 * 64:(k4 + 1) * 64], ytmp[i2][:, k4 * 128:(k4 + 1) * 128], [ytmp[i2]], [pt.rng(0, 256)])
                CP('act', yglaT[:, hd * 4:(hd + 1) * 4, cs], pt[:, 0:256].rearrange("p (a b) -> p a b", a=4), [pt.rng(0, 256)], [yglaT])
            if last_pre:
                TS('dve', Sst[:], Sst[:], flag[:, 0:1], None, ALU.mult, None, [Sst, flag], [Sst])
            S_tok[hd] = ST(S_dram[hd].rearrange("p (d v) -> p d v", d=2), Sst[:], [Sst])
        if pre:
            return

        chk(3)
        o = RA
        G = S.sb(f"G_{s}", (128, TT, NEXP), F32, off=o); o = _al(o + 512)
        GT = S.sb(f"GT_{s}", (32, SBT), F32, off=o); o = _al(o + 2048)
        G17 = S.sb(f"G17_{s}", (128, TT, NEXP), F32, off=o); o = _al(o + 512)
        P5O2 = o
        mrgT = S.sb(f"mrgT_{s}", (128, KT, SBT), BF16, off=o); o = _al(o + 16384)
        sga = S.sb(f"sga_{s}", (128, SBT), F32, off=o); o = _al(o + 2048)
        sgg = S.sb(f"sgg_{s}", (128, SBT), F32, off=o); o = _al(o + 2048)
        mt = [S.sb(f"mt{i}_{s}", (128, SBT), F32, off=o + i * 2048) for i in range(2)]; o = _al(o + 4096)
        hTf = S.sb(f"hTf_{s}", (128, KT, 128), F32, off=o); o = _al(o + 8192)
        lg = S.sb(f"lg_{s}", (128, NEXP), F32, off=o); o = _al(o + 128)
        ex = S.sb(f"ex_{s}", (128, NEXP), F32, off=o); o = _al(o + 128)
        m8 = S.sb(f"m8_{s}", (128, 8), F32, off=o); o = _al(o + 64)
        assert o <= SB_END, (o, RA)
        for j in range(KT):
            cs = slice(j * 128, (j + 1) * 128)
            wt, wv = wload([(wpa_d[:, cs], 0, 128)], 8, 128)
            ppa = nps()
            for k in range(8):
                MM(ppa[:, :], wv[:, k, :], yattT[:, k, :], k == 0, k == 7, [wt, yattT], [ppa])
            wt, wv = wload([(wpg_d[:, cs], 0, 128)], KT, 128)
            ppg = nps()
            for k in range(KT):
                MM(ppg[:, :], wv[:, k, :], yglaT[:, k, :], k == 0, k == KT - 1, [wt, yglaT], [ppg])
            wt, wv = wload([(win_d[:, OFF_GA + j * 128:OFF_GA + (j + 1) * 128], 0, 128),
                            (win_d[:, OFF_GG + j * 128:OFF_GG + (j + 1) * 128], 128, 128)], KT, 256)
            pga = nps()
            for k in range(KT):
                MM(pga[:, :], wv[:, k, 0:128], hT[:, k, :], k == 0, k == KT - 1, [wt, hT], [pga])
            pgg = nps()
            for k in range(KT):
                MM(pgg[:, :], wv[:, k, 128:256], hT[:, k, :], k == 0, k == KT - 1, [wt, hT], [pgg])
            ACT(sga[:], pga[:, :], AF.Sigmoid, [pga], [sga])
            ACT(sgg[:], pgg[:, :], AF.Sigmoid, [pgg], [sgg])
            TTo('dve', mt[0][:], ppa[:, :], sga[:], ALU.mult, [ppa, sga], [mt[0]])
            TTo('dve', mt[1][:], ppg[:, :], sgg[:], ALU.mult, [ppg, sgg], [mt[1]])
            TTo('pool', mrgT[:, j, :], mt[0][:], mt[1][:], ALU.add, [mt[0], mt[1]], [mrgT.rng(j * SBT, (j + 1) * SBT)])
        chk(3.2)
        gt1 = ada_bc(2)
        for c8 in range(8):
            cs = slice(c8 * 256, (c8 + 1) * 256)
            wt, wv = wload([(wo_d[:, cs], 0, 256)], KT, 256)
            for t in range(TT):
                p = nps()
                for k in range(KT):
                    MM(p[:, 0:256], mrgT[:, k, t * 128:(t + 1) * 128], wv[:, k, :], k == 0, k == KT - 1, [wt, mrgT], [p.rng(0, 256)])
                i2 = (c8 * TT + t) % 2
                xr = xres[t].rng(c8 * 256, (c8 + 1) * 256)
                TTo('dve', mt[i2][:, 0:256], p[:, 0:256], gt1[:, cs], ALU.mult, [p.rng(0, 256), gt1], [mt[i2]])
                STT('dve', xres[t][:, cs], xres[t][:, cs], ALPHA, mt[i2][:, 0:256], ALU.mult, ALU.add, [xr, mt[i2]], [xr])
        g1 = bcload(ln1g_d)
        for t in range(TT):
            layernorm(xres[t], xres[t], xres[t][:])
            TTo('pool', xres[t][:], xres[t][:], g1[:], ALU.mult, [xres[t], g1], [xres[t]])
        b1 = bcload(ln1b_d)
        for t in range(TT):
            TTo('dve', xres[t][:], xres[t][:], b1[:], ALU.add, [xres[t], b1], [xres[t]])
            if dbg:
                ST(dbg_d["d_x1"][t0 + t * 128:t0 + (t + 1) * 128, :], xres[t][:], [xres[t]])
        chk(3.5)
        sc2p = ada_bc(4, plus1=True)
        sh2 = ada_bc(3)
        for t in range(TT):
            layernorm(xres[t], tmpA, tmpA[:])
            TTo('pool', tmpA[:], tmpA[:], sc2p[:], ALU.mult, [tmpA, sc2p], [tmpA])
            TTo('dve', tmpA[:], tmpA[:], sh2[:], ALU.add, [tmpA, sh2], [tmpA])
            transpose_tile(tmpA, t, want_f32=hTf)
            p = nps()
            for k in range(KT):
                MM(p[:, 0:NEXP], hTf[:, k, :], wr_sb[:, k, :], k == 0, k == KT - 1, [hTf, wr_sb], [p.rng(0, NEXP)])
            ii = nst()
            TTo('dve', lg[:], p[:, 0:NEXP], br_bc[:], ALU.add, [p.rng(0, NEXP), br_bc], [lg])
            S.op('dve', lambda E: E.max(out=m8[:], in_=lg[:]), [lg], [m8])
            TS('dve', st_a[ii][:], m8[:, 0:1], -1.0, None, ALU.mult, None, [m8], [st_a[ii]])
            ACT(ex[:], lg[:], AF.Exp, [lg, st_a[ii]], [ex], bias=st_a[ii][:, 0:1])
            TS('dve', lg[:], lg[:], m8[:, 3:4], None, ALU.is_ge, None, [lg, m8], [lg])
            TTo('dve', ex[:], ex[:], lg[:], ALU.mult, [ex, lg], [ex])
            S.op('dve', lambda E, ii=ii: E.tensor_reduce(out=st_b[ii][:], in_=ex[:], axis=AX.X, op=ALU.add), [ex], [st_b[ii]])
            S.op('dve', lambda E, ii=ii: E.reciprocal(out=st_b[ii][:], in_=st_b[ii][:]), [st_b[ii]], [st_b[ii]])
            TS('dve', G[:, t, :], ex[:], st_b[ii][:, 0:1], None, ALU.mult, None, [ex, st_b[ii]], [G.rng(t * NEXP, (t + 1) * NEXP)])
            if dbg:
                ST(dbg_d["d_lg"][t0 + t * 128:t0 + (t + 1) * 128, 0:32], G[:, t, :], [G])
            TS('dve', G17[:, t, :], G[:, t, :], 1.0 / 1.702, None, ALU.mult, None, [G], [G17.rng(t * NEXP, (t + 1) * NEXP)])
            p = nps()
            TR(p[0:32, 0:128], G[:, t, :], [G], [p.rng(0, 128)])
            CP('act', GT[:, t * 128:(t + 1) * 128], p[0:32, 0:128], [p.rng(0, 128)], [GT.rng(t * 128, (t + 1) * 128)])

        chk(4)
        o = P5O2
        accf = [S.sb(f"accf{t}_{s}", (128, D), F32, off=o + t * 8192) for t in range(TT)]; o = _al(o + 4 * 8192)
        actT = S.sb(f"actT_{s}", (128, KT, SBT), BF16, off=o); o = _al(o + 16384)
        et = [S.sb(f"et{i}_{s}", (128, SBT), F32, off=tmpA.addr + i * 2048) for i in range(3)]
        assert o <= SB_END, o
        if do_moe:
            bdt = bc[(cnt['bc'] + 1) % 2]
            cnt['bc'] += 1
            LD(bdt[0:32, :], bd_d, [bdt])
            for t in range(TT):
                for c4 in range(4):
                    cs = slice(c4 * 512, (c4 + 1) * 512)
                    p = nps()
                    MM(p[:, :], GT[0:32, t * 128:(t + 1) * 128], bdt[0:32, cs], True, True, [GT, bdt], [p])
                    CP('act' if c4 % 2 else 'dve', accf[t][:, cs], p[:, :], [p], [accf[t].rng(c4 * 512, (c4 + 1) * 512)])
            chunks = []
            for e in range(NEXP):
                chunks += [(e, 'gu', j) for j in range(KT)] + [(e, 'dn', c8) for c8 in range(8)]
            NCH = len(chunks)

            def w_dma(k):
                e, kind, idx = chunks[k]
                i = k % 2
                src = wgu_d[e][:, idx * 256:(idx + 1) * 256] if kind == 'gu' else wd_d[e][:, idx * 256:(idx + 1) * 256]
                sv = wst[i][:].rearrange("p (k n) -> p k n", k=KT)
                LD(sv, src.rearrange("(k p) n -> p k n", p=128), [wst[i]], q=WQ[i])

            def w_cast(k):
                e, kind, idx = chunks[k]
                i = k % 2
                if kind == 'gu':
                    ov = wbf[i][:].rearrange("p (k g f) -> p k g f", k=KT, g=2)
                    iv = wst[i][:].rearrange("p (k f g) -> p k g f", k=KT, g=2)
                    CP('act', ov, iv, [wst[i]], [wbf[i]])
                    return ov
                bv = wbf[i][:].rearrange("p (k n) -> p k n", k=KT)
                CP('dve', bv, wst[i][:].rearrange("p (k n) -> p k n", k=KT), [wst[i]], [wbf[i]])
                return bv

            w_dma(0)
            w_dma(1)
            views = {0: w_cast(0)}
            for k in range(NCH):
                if k + 2 < NCH:
                    w_dma(k + 2)
                if k + 1 < NCH:
                    views[k + 1] = w_cast(k + 1)
                e, kind, idx = chunks[k]
                wt, wv = wbf[k % 2], views.pop(k)
                if kind == 'gu':
                    j = idx
                    pg = nps()
                    for kk in range(KT):
                        MM(pg[:, :], wv[:, kk, 0, :], hT[:, kk, :], kk == 0, kk == KT - 1, [wt, hT], [pg])
                    pu = nps()
                    for kk in range(KT):
                        MM(pu[:, :], wv[:, kk, 1, :], hT[:, kk, :], kk == 0, kk == KT - 1, [wt, hT], [pu])
                    bcol = (e * 16 + j) * 2
                    TS('dve', et[0][:], pg[:, :], bgu[:, bcol:bcol + 1], 7.0, ALU.add, ALU.min, [pg, bgu], [et[0]])
                    ACT(et[1][:], et[0][:], AF.Silu, [et[0]], [et[1]], scale=1.702)
                    TS('dve', et[2][:], pu[:, :], bgu[:, bcol + 1:bcol + 2], 8.0, ALU.add, ALU.min, [pu, bgu], [et[2]])
                    STT('dve', actT[:, j, :], et[2][:], -6.0, et[1][:], ALU.max, ALU.mult, [et[2], et[1]], [actT.rng(j * SBT, (j + 1) * SBT)])
                else:
                    c8 = idx
                    cs = slice(c8 * 256, (c8 + 1) * 256)
                    for t in range(TT):
                        p = nps()
                        for kk in range(KT):
                            MM(p[:, 0:256], actT[:, kk, t * 128:(t + 1) * 128], wv[:, kk, :], kk == 0, kk == KT - 1, [wt, actT], [p.rng(0, 256)])
                        ar = accf[t].rng(c8 * 256, (c8 + 1) * 256)
                        STT('dve', accf[t][:, cs], p[:, 0:256], G17[:, t, e:e + 1], accf[t][:, cs], ALU.mult, ALU.add, [p.rng(0, 256), G17, ar], [ar])
        else:
            for t in range(TT):
                S.op('pool', lambda E, t=t: E.memset(accf[t][:], 0.0), [], [accf[t]])

        gt2 = ada_bc(5)
        for t in range(TT):
            TTo('pool', accf[t][:], accf[t][:], gt2[:], ALU.mult, [accf[t], gt2], [accf[t]])
            STT('dve', xres[t][:], xres[t][:], ALPHA, accf[t][:], ALU.mult, ALU.add, [xres[t], accf[t]], [xres[t]])
        g2 = bcload(ln2g_d)
        for t in range(TT):
            layernorm(xres[t], xres[t], xres[t][:])
            TTo('pool', xres[t][:], xres[t][:], g2[:], ALU.mult, [xres[t], g2], [xres[t]])
        b2 = bcload(ln2b_d)
        for t in range(TT):
            TTo('dve', xres[t][:], xres[t][:], b2[:], ALU.add, [xres[t], b2], [xres[t]])
            ST(out_d[t0 + t * 128:t0 + (t + 1) * 128, :], xres[t][:], [xres[t]])

    try:
        for s in range(NSBP):
            sb_body(s, pre=True)
        for s in range(NSB if stage > 0 else 0):
            sb_body(s)
    except _Stop:
        pass
    S.finish()
    S.run()
    return nc, S


def _consts():
    ident = np.eye(128, dtype=np.float32)
    q = np.arange(128)[:, None]
    s_ = np.arange(256)[None, :]
    dist = (q + 128 - s_).astype(np.float32)
    valid = (dist >= 0) & (dist < 128)
    slopes = (2.0 ** (-8.0 * (np.arange(16, dtype=np.float32) + 1.0) / 16)).astype(np.float32)
    swab = np.where(valid[:, None, :], -slopes[None, :, None] * dist[:, None, :], np.float32(NEG)).astype(np.float32)
    gmask = (np.arange(64)[:, None] <= np.arange(64)[None, :]).astype(np.float32)
    return ident, np.ascontiguousarray(swab.reshape(128, 16 * 256)), gmask


def make_in_maps(inputs, batches, T, do_moe=True):
    f = lambda a: np.ascontiguousarray(np.asarray(a, dtype=np.float32))
    ident, swab, gmask = _consts()
    shared = dict(
        w_ada=f(inputs["w_ada"][0]), b_ada=f(inputs["b_ada"][0]), w_in=f(inputs["w_in"][0]),
        w_gla_up=f(inputs["w_gla_gate_up"][0]),
        b_gate_fm=f(np.asarray(inputs["b_gla_gate"][0]).reshape(8, 128).T),
        attn_sinks=f(inputs["attn_sinks"][0]), gla_gain=f(inputs["gla_norm_gain"][0]),
        w_pa=f(inputs["w_branch_att"][0]), w_pg=f(inputs["w_branch_gla"][0]), w_out=f(inputs["w_out"][0]),
        ln1_gain=f(inputs["ln1_gain"][0]), ln1_bias=f(inputs["ln1_bias"][0]),
        ln2_gain=f(inputs["ln2_gain"][0]), ln2_bias=f(inputs["ln2_bias"][0]),
        w_router=f(inputs["w_router"][0]), b_router=f(inputs["b_router"][0]),
        ident=ident, swa_bias=swab, gla_mask=gmask)
    if do_moe:
        bgu = np.asarray(inputs["b_gate_up"][0]).reshape(NEXP, 16, 128, 2)
        shared.update(
            w_gate_up=f(inputs["w_gate_up"][0]), w_down=f(inputs["w_down"][0]), b_down=f(inputs["b_down"][0]),
            b_gu_fm=f(bgu.transpose(2, 0, 1, 3).reshape(128, NEXP * 32)))
    maps = []
    for b in batches:
        m = dict(shared)
        m["x"] = f(inputs["x"][b, :T])
        m["c_fm"] = f(np.asarray(inputs["c"][b]).reshape(KT, 128).T)
        maps.append(m)
    return maps


N_CORES = 8
SEQ = 4096
HALF = SEQ // 2


def make_in_maps8(inputs, cores, do_moe=True):
    base = make_in_maps(inputs, [0], HALF, do_moe=do_moe)[0]
    f = lambda a: np.ascontiguousarray(np.asarray(a, dtype=np.float32))
    _, swab, _ = _consts()
    swab_first = swab.reshape(128, 16, 256).copy()
    swab_first[:, :, 0:128] = NEG
    swab_first = np.ascontiguousarray(swab_first.reshape(128, 16 * 256))
    maps = []
    for c in cores:
        b, half = c // 2, c % 2
        m = dict(base)
        m["x"] = f(inputs["x"][b, half * HALF:(half + 1) * HALF])
        m["c_fm"] = f(np.asarray(inputs["c"][b]).reshape(KT, 128).T)
        if half:
            m["xprev"] = f(inputs["x"][b, 0:HALF])
            m["flag"] = np.ones((128, 1), np.float32)
            m["swa_bias0"] = swab
        else:
            m["xprev"] = np.zeros((HALF, D), np.float32)
            m["flag"] = np.zeros((128, 1), np.float32)
            m["swa_bias0"] = swab_first
        maps.append(m)
    return maps


def kernel(**inputs):
    nc, _ = build_nc(HALF, TP=HALF)
    maps = make_in_maps8(inputs, list(range(N_CORES)))
    res = run_bass_kernel_spmd(nc, maps, core_ids=list(range(N_CORES)))
    B = N_CORES // 2
    out = np.empty((B, SEQ, D), np.float32)
    for c, r in enumerate(res.results):
        out[c // 2, (c % 2) * HALF:(c % 2 + 1) * HALF] = np.asarray(r["out"], dtype=np.float32)
    return out
```
